# Optimizing a Trainium2 kernel written in Bass

```python
import jax
import jax.numpy as jnp
from jax import lax
import numpy as np

D_MODEL = 1024
BATCH = 4
SEQ = 4096
DEPTH = 4

D_PLE = 256
N_SG = 8
SG_DIM = 64
D_SG = N_SG * SG_DIM
CHUNK = 128
N_HEADS = 8
QK_NOPE = 64
QK_ROPE = 32
V_DIM = 64
Q_LORA = 256
KV_LORA = 128
D_ATT = N_HEADS * V_DIM
D_MIX = D_SG + D_ATT
D_IN = 2 * D_SG + Q_LORA + KV_LORA + QK_ROPE
ROPE_THETA = 10000.0
Q_BLOCK = 128
D_FF = 2816
N_EXPERTS = 8
TOP_K = 2
D_FF_EXPERT = 3584
N_DENSE = (DEPTH + 1) // 2
N_MOE = DEPTH // 2
DN_ALPHA = (2.0 * DEPTH) ** 0.25
DN_BETA = (8.0 * DEPTH) ** -0.25
EPS = 1e-6

kernel_name = "hybrid_gmlp_mla_moe_deepnorm"


def layer_norm(x, g, b):
    xf = x.astype(jnp.float32)
    mu = jnp.mean(xf, -1, keepdims=True)
    var = jnp.mean(jnp.square(xf - mu), -1, keepdims=True)
    return ((xf - mu) * lax.rsqrt(var + EPS) * g + b).astype(x.dtype)


def rms_norm(x, g):
    xf = x.astype(jnp.float32)
    return (xf * lax.rsqrt(jnp.mean(xf * xf, -1, keepdims=True) + EPS) * g).astype(x.dtype)


def rope_angles(positions):
    inv_freq = 1.0 / (ROPE_THETA ** (jnp.arange(0, QK_ROPE, 2, dtype=jnp.float32) / QK_ROPE))
    ang = positions.astype(jnp.float32)[..., None] * inv_freq
    return jnp.cos(ang), jnp.sin(ang)


def apply_rope(x, cos, sin):
    xf = x.astype(jnp.float32)
    x1, x2 = jnp.split(xf, 2, axis=-1)
    return jnp.concatenate([x1 * cos - x2 * sin, x1 * sin + x2 * cos], -1).astype(x.dtype)


def spatial_gating(uv, v_g, v_b, w_s, b_s):
    B, S, _ = uv.shape
    u, v = jnp.split(jax.nn.gelu(uv, approximate=False), 2, axis=-1)
    v = layer_norm(v, v_g, v_b)
    v = v.reshape(B, S // CHUNK, CHUNK, N_SG, SG_DIM)
    causal = jnp.tril(jnp.ones((CHUNK, CHUNK), dtype=bool))
    w = jnp.where(causal[None], w_s, 0)
    mixed = jnp.einsum('gts,bcsgd->bctgd', w, v) + b_s.T[None, None, :, :, None]
    return u * mixed.reshape(B, S, D_SG)


def latent_attention(c_q, c_kv, k_r, q_g, kv_g, w_uq, w_ukv, cos, sin):
    B, S, _ = c_q.shape
    q = (rms_norm(c_q, q_g) @ w_uq).reshape(B, S, N_HEADS, QK_NOPE + QK_ROPE)
    q_nope = q[..., :QK_NOPE]
    q_rope = apply_rope(q[..., QK_NOPE:], cos[:, :, None, :], sin[:, :, None, :])
    kv = (rms_norm(c_kv, kv_g) @ w_ukv).reshape(B, S, N_HEADS, QK_NOPE + V_DIM)
    k_nope = kv[..., :QK_NOPE]
    v = kv[..., QK_NOPE:]
    k_rope = apply_rope(k_r, cos, sin)
    scale = (QK_NOPE + QK_ROPE) ** -0.5
    n_blk = S // Q_BLOCK
    qn_b = q_nope.reshape(B, n_blk, Q_BLOCK, N_HEADS, QK_NOPE).transpose(1, 0, 2, 3, 4)
    qr_b = q_rope.reshape(B, n_blk, Q_BLOCK, N_HEADS, QK_ROPE).transpose(1, 0, 2, 3, 4)
    k_pos = jnp.arange(S)

    def block(args):
        qn, qr, bi = args
        s = (jnp.einsum('bqhd,bkhd->bhqk', qn, k_nope)
             + jnp.einsum('bqhd,bkd->bhqk', qr, k_rope)).astype(jnp.float32) * scale
        q_pos = bi * Q_BLOCK + jnp.arange(Q_BLOCK)
        mask = k_pos[None, :] <= q_pos[:, None]
        s = jnp.where(mask[None, None], s, -jnp.inf)
        pr = jax.nn.softmax(s, axis=-1).astype(v.dtype)
        return jnp.einsum('bhqk,bkhd->bqhd', pr, v)

    o = lax.map(block, (qn_b, qr_b, jnp.arange(n_blk)))
    return o.transpose(1, 0, 2, 3, 4).reshape(B, S, D_ATT)


def swiglu(x, w1, w3, w2):
    return (jax.nn.silu(x @ w1) * (x @ w3)) @ w2


def moe(x, w_r, w1, w3, w2):
    B, S, D = x.shape
    xt = x.reshape(-1, D)
    logits = (xt @ w_r).astype(jnp.float32)
    top_l, top_i = lax.top_k(logits, TOP_K)
    top_w = jax.nn.softmax(top_l, axis=-1)
    combine = jnp.sum(jax.nn.one_hot(top_i, N_EXPERTS, dtype=jnp.float32) * top_w[..., None], axis=1)
    combine = combine.astype(x.dtype)
    out = jnp.zeros_like(xt)
    for e in range(N_EXPERTS):
        out = out + combine[:, e:e + 1] * swiglu(xt, w1[e], w3[e], w2[e])
    return out.reshape(B, S, D)


def setup_inputs(seed: int = 0) -> dict:
    key = jax.random.key(seed)
    ks = iter(jax.random.split(key, 32))
    L = DEPTH

    def nrm(shape, scale):
        return jax.random.normal(next(ks), shape, jnp.float32) * scale

    def gain(shape):
        return 1.0 + nrm(shape, 0.02)

    x = nrm((BATCH, SEQ, D_MODEL), 1.0)
    p = nrm((DEPTH, BATCH, SEQ, D_PLE), 1.0)
    offs = jax.random.randint(next(ks), (BATCH, 1), 0, 1024, dtype=jnp.int32)
    positions = offs + jnp.arange(SEQ, dtype=jnp.int32)[None, :]
    return {
        'x': x,
        'p': p,
        'positions': positions,
        'w_in': nrm((L, D_MODEL, D_IN), D_MODEL ** -0.5),
        'sg_v_g': gain((L, D_SG)),
        'sg_v_b': nrm((L, D_SG), 0.02),
        'sg_w_s': nrm((L, N_SG, CHUNK, CHUNK), 0.5 * CHUNK ** -0.5),
        'sg_b_s': 1.0 + nrm((L, N_SG, CHUNK), 0.1),
        'q_norm_g': gain((L, Q_LORA)),
        'kv_norm_g': gain((L, KV_LORA)),
        'w_uq': nrm((L, Q_LORA, N_HEADS * (QK_NOPE + QK_ROPE)), Q_LORA ** -0.5),
        'w_ukv': nrm((L, KV_LORA, N_HEADS * (QK_NOPE + V_DIM)), KV_LORA ** -0.5),
        'out_g': gain((L, D_MIX)),
        'w_o': nrm((L, D_MIX, D_MODEL), DN_BETA * D_MIX ** -0.5),
        'ln1_g': gain((L, D_MODEL)),
        'ln1_b': nrm((L, D_MODEL), 0.02),
        'ffn_w1': nrm((N_DENSE, D_MODEL, D_FF), D_MODEL ** -0.5),
        'ffn_w3': nrm((N_DENSE, D_MODEL, D_FF), D_MODEL ** -0.5),
        'ffn_w2': nrm((N_DENSE, D_FF, D_MODEL), DN_BETA * D_FF ** -0.5),
        'moe_w_r': nrm((N_MOE, D_MODEL, N_EXPERTS), D_MODEL ** -0.5),
        'moe_w1': nrm((N_MOE, N_EXPERTS, D_MODEL, D_FF_EXPERT), D_MODEL ** -0.5),
        'moe_w3': nrm((N_MOE, N_EXPERTS, D_MODEL, D_FF_EXPERT), D_MODEL ** -0.5),
        'moe_w2': nrm((N_MOE, N_EXPERTS, D_FF_EXPERT, D_MODEL), DN_BETA * D_FF_EXPERT ** -0.5),
        'ln2_g': gain((L, D_MODEL)),
        'ln2_b': nrm((L, D_MODEL), 0.02),
        'ple_w_g': nrm((L, D_MODEL, D_MODEL), D_MODEL ** -0.5),
        'ple_b_g': nrm((L, D_MODEL), 0.01),
        'ple_w_p': nrm((L, D_PLE, D_MODEL), D_PLE ** -0.5),
    }


def reference(x, p, positions, w_in, sg_v_g, sg_v_b, sg_w_s, sg_b_s, q_norm_g, kv_norm_g,
              w_uq, w_ukv, out_g, w_o, ln1_g, ln1_b, ffn_w1, ffn_w3, ffn_w2,
              moe_w_r, moe_w1, moe_w3, moe_w2, ln2_g, ln2_b, ple_w_g, ple_b_g, ple_w_p):
    cos, sin = rope_angles(positions)
    s1 = 2 * D_SG
    s2 = s1 + Q_LORA
    s3 = s2 + KV_LORA
    for i in range(DEPTH):
        h = x @ w_in[i]
        a = spatial_gating(h[..., :s1], sg_v_g[i], sg_v_b[i], sg_w_s[i], sg_b_s[i])
        m = latent_attention(h[..., s1:s2], h[..., s2:s3], h[..., s3:], q_norm_g[i], kv_norm_g[i],
                             w_uq[i], w_ukv[i], cos, sin)
        y = jnp.concatenate([rms_norm(a, out_g[i, :D_SG]), rms_norm(m, out_g[i, D_SG:])], -1) @ w_o[i]
        x = layer_norm(DN_ALPHA * x + y, ln1_g[i], ln1_b[i])
        j = i // 2
        if i % 2 == 0:
            f = swiglu(x, ffn_w1[j], ffn_w3[j], ffn_w2[j])
        else:
            f = moe(x, moe_w_r[j], moe_w1[j], moe_w3[j], moe_w2[j])
        x = layer_norm(DN_ALPHA * x + f, ln2_g[i], ln2_b[i])
        gate = jax.nn.sigmoid(x @ ple_w_g[i] + ple_b_g[i])
        x = x + gate * (p[i] @ ple_w_p[i])
    return x
```

```python
import contextlib
import math
import numpy as np
import concourse.bass as bass
import concourse.mybir as mybir
from concourse.bass_utils import run_bass_kernel_spmd

F32 = mybir.dt.float32
BF16 = mybir.dt.bfloat16
I32 = mybir.dt.int32
AF = mybir.ActivationFunctionType
ALU = mybir.AluOpType
AX = mybir.AxisListType

ENGS = ["sync", "scalar", "vector", "gpsimd", "tensor"]
NDMASEM = 40
GMAP = [[0, 3], [1, 2]]
ALPHA = (2.0 * 4) ** 0.25
EPS = 1e-6
ATT_SCALE = 96 ** -0.5
NB = 16
T = 2048


class Prog:
    def __init__(self, nc):
        self.nc = nc
        self.q = {e: [] for e in ENGS}
        self.cnt = {e: 0 for e in ENGS}
        self.seen = {e: {} for e in ENGS}
        self.lastw = {}
        self.readers = {}
        self.dma_use = [0] * NDMASEM
        self.dma_rr = 0
        self.pending_nosig = {e: False for e in ENGS}

    def _need(self, eng, tickets):
        out = []
        best = {}
        for t in tickets:
            if t is None:
                continue
            k, v = t
            if v > best.get(k, 0):
                best[k] = v
        for k, v in best.items():
            if self.seen[eng].get(k, 0) >= v:
                continue
            self.seen[eng][k] = v
            out.append((k, v))
        return out

    def _rtix(self, x):
        d = self.lastw.get(x)
        return list(d.items()) if d else []

    def _deps(self, r, w):
        ts = []
        for x in r:
            ts.extend(self._rtix(x))
        for x in w:
            ts.extend(self._rtix(x))
            ts.extend(self.readers.get(x, ()))
        return ts

    def _commit(self, ticket, r, w):
        k, v = ticket
        for x in w:
            d = self.lastw.setdefault(x, {})
            if d.get(k, 0) < v:
                d[k] = v
            self.readers[x] = []
        for x in r:
            self.readers.setdefault(x, []).append(ticket)

    def op(self, eng, fn, r=(), w=(), sig=True, pe_acc=False):
        ts = self._deps(r, w)
        if pe_acc:
            ts = [t for t in ts if t[0] != eng]
        else:
            raw = set()
            for x in r:
                raw.update(self._rtix(x))
            ts = [t for t in ts if (t[0] != eng or t in raw)]
        waits = self._need(eng, ts)
        ticket = (eng, self.cnt[eng] + 1)
        if sig:
            self.cnt[eng] += 1
            self.pending_nosig[eng] = False
        else:
            self.pending_nosig[eng] = True
        self.q[eng].append((waits, fn, ("e", eng) if sig else None))
        self._commit(ticket, r, w)
        return ticket

    def dma(self, eng, fn, r=(), w=(), inc=16):
        ts = self._deps(r, w)
        idx = self.dma_rr
        self.dma_rr = (self.dma_rr + 1) % NDMASEM
        use = self.dma_use[idx]
        if use > 0:
            ts.append((("d", idx), use))
        self.dma_use[idx] = use + inc
        ticket = (("d", idx), use + inc)
        waits = self._need(eng, ts)
        self.q[eng].append((waits, fn, ("d", idx, inc)))
        self._commit(ticket, r, w)
        return ticket

    def wait_all(self, eng, tickets):
        waits = self._need(eng, tickets)
        if waits:
            self.q[eng].append((waits, None, None))

    def barrier(self):
        for e in ENGS:
            assert not self.pending_nosig[e]
        ts = [(e, self.cnt[e]) for e in ENGS if self.cnt[e] > 0]
        ts += [(("d", i), self.dma_use[i]) for i in range(NDMASEM) if self.dma_use[i] > 0]
        for e in ENGS:
            self.wait_all(e, ts)

    def emit(self, stack):
        nc = self.nc
        for e in ENGS:
            assert not self.pending_nosig[e], e
        esem = {e: stack.enter_context(nc.semaphore("se_" + e)) for e in ENGS}
        dsem = [stack.enter_context(nc.semaphore("sd_%d" % i)) for i in range(NDMASEM)]

        def semof(k):
            return esem[k] if isinstance(k, str) else dsem[k[1]]

        block = stack.enter_context(nc.Block())

        def mk(ename):
            def body(engine):
                for waits, fn, sg in self.q[ename]:
                    for k, v in waits:
                        engine.wait_ge(semof(k), v)
                    if fn is None:
                        continue
                    ins = fn(engine)
                    if sg is None:
                        continue
                    if sg[0] == "e":
                        ins.then_inc(esem[sg[1]], 1)
                    else:
                        ins.then_inc(dsem[sg[1]], sg[2])
            return body

        for e in ENGS:
            if self.q[e]:
                getattr(block, e)(mk(e))


def build(kinds, stop_after=None):
    nl = len(kinds)
    nd = kinds.count("d")
    nm = kinds.count("m")
    nc = bass.Bass("TRN2", target_bir_lowering=False)

    def DI(name, shape, dt=F32):
        return nc.dram_tensor(name, shape, dt, kind="ExternalInput").ap()

    x_d = DI("x", [NB, 128, 1024])
    p_d = DI("p", [nl, NB, 128, 256])
    pos_d = DI("pos", [1, T], I32)
    invf_d = DI("invf", [32, 1])
    sgn_d = DI("sgn", [32, 1])
    ident_d = DI("ident", [128, 128])
    tril_d = DI("tril", [128, 128])
    mask_d = DI("maskT", [128, 1024])
    negs_d = DI("negs", [128, 66])
    w_in_d = DI("w_in", [nl, 1024, 1440])
    sg_v_g_d = DI("sg_v_g", [nl, 512])
    sg_v_b_d = DI("sg_v_b", [nl, 512])
    sg_w_s_d = DI("sg_w_s", [nl, 8, 128, 128])
    sg_b_s_d = DI("sg_b_s", [nl, 1024])
    q_norm_g_d = DI("q_norm_g", [nl, 256])
    kv_norm_g_d = DI("kv_norm_g", [nl, 128])
    w_uq_d = DI("w_uq", [nl, 256, 768])
    w_ukv_d = DI("w_ukv", [nl, 128, 1024])
    out_g_d = DI("out_g", [nl, 1024])
    w_o_d = DI("w_o", [nl, 1024, 1024])
    ln1_g_d = DI("ln1_g", [nl, 1024])
    ln1_b_d = DI("ln1_b", [nl, 1024])
    ln2_g_d = DI("ln2_g", [nl, 1024])
    ln2_b_d = DI("ln2_b", [nl, 1024])
    ple_w_g_d = DI("ple_w_g", [nl, 1024, 1024])
    ple_b_g_d = DI("ple_b_g", [nl, 1024])
    ple_w_p_d = DI("ple_w_p", [nl, 256, 1024])
    if nd:
        ffn_w1_d = DI("ffn_w1", [nd, 1024, 2816])
        ffn_w3_d = DI("ffn_w3", [nd, 1024, 2816])
        ffn_w2_d = DI("ffn_w2", [nd, 2816, 1024])
    if nm:
        moe_w_r_d = DI("moe_w_r", [nm, 1024, 8])
        moe_w1_d = DI("moe_w1", [nm, 8, 1024, 3584])
        moe_w3_d = DI("moe_w3", [nm, 8, 1024, 3584])
        moe_w2_d = DI("moe_w2", [nm, 8, 3584, 1024])
    y_d = nc.dram_tensor("y", [NB, 128, 1024], F32, kind="ExternalOutput").ap()
    dbg_d = nc.dram_tensor("dbg", [128, 8192], BF16, kind="ExternalOutput").ap() if stop_after == "mixer_raw" else None
    ag_in = [nc.dram_tensor("ag_in%d" % l, [160, T], BF16).ap() for l in range(nl)]
    ag_out = [nc.dram_tensor("ag_out%d" % l, [320, T], BF16).ap() for l in range(nl)]

    st = contextlib.ExitStack()
    with st:
        def SB(name, shape, dt=F32):
            return st.enter_context(nc.sbuf_tensor(name, shape, dt))

        ps = st.enter_context(nc.psum_tensor("ps", [128, 4096], F32))

        def bank(b, lo=0, hi=512, p0=0, p1=128):
            return ps[p0:p1, b * 512 + lo:b * 512 + hi]

        def bankb(b, lo=0, hi=512, p0=0, p1=128):
            v = ps[p0:p1, b * 512:(b + 1) * 512].bitcast(BF16)
            return v[:, lo:hi]

        X = SB("X", [128, NB, 1024])
        WB = [SB("WB0", [128, 12288], BF16), SB("WB1", [128, 12288], BF16)]
        R1 = SB("R1", [128, 20480], BF16)
        R2 = SB("R2", [128, 8192], BF16)
        CS = SB("CS", [32, 2, T], BF16)
        LNG = SB("LNG", [128, 1024]); LNB = SB("LNB", [128, 1024])
        TA = SB("TA", [128, 1024])
        ident_f = SB("ident_f", [128, 128]); ident_b = SB("ident_b", [128, 128], BF16)
        tril_f = SB("tril_f", [128, 128])
        maskT = SB("maskTs", [128, 4, 256], BF16)
        ones_b = SB("ones_b", [128, 128], BF16); ones_f = SB("ones_f", [128, 128])
        NEGS = SB("NEGS", [128, 66], BF16)
        OG = SB("OG", [128, 8]); QG = SB("QG", [128, 2]); KG = SB("KG", [128, 1])
        invf = SB("invf_s", [32, 1]); sgn = SB("sgn_s", [32, 1])
        bsr = SB("bsr", [1, 1024], BF16); bgr = SB("bgr", [1, 1024], BF16)
        wsT = SB("wsT", [128, 8, 128], BF16)
        WKpad = SB("WKpad", [128, 128], BF16)
        WQpad = SB("WQpad", [128, 2, 128], BF16); WQsw = SB("WQsw", [128, 2, 32], BF16)
        WLsw = SB("WLsw", [128, 8, 32], BF16)
        xTt0 = SB("xTt0", [128, 8, 128], BF16)
        xTt = [xTt0, xTt0]
        sm = SB("sm", [128, 64])
        mstat = SB("mstat", [128, 16])
        Mst = SB("Mst", [128, 33], BF16)
        comb = SB("comb", [128, NB, 8])
        ssq = SB("ssq", [128, NB])
        wr_f = SB("wr_f", [128, 8, 8])
        wr_h = SB("wr_h", [128, 8, 8], BF16); wr_l = SB("wr_l", [128, 8, 8], BF16)
        lgt = SB("lgt", [128, 4, 8])
        stt = SB("stt", [128, 4, 6])
        aTt = SB("aTt", [128, 4, 128], BF16)
        cqn = SB("cqn", [128, 384], BF16)
        pblk = SB("pblk", [128, 256], BF16); pTt = SB("pTt", [128, 2, 128], BF16)
        krt = SB("krt", [32, 256])

        CKV = R1[:, 0:4096]
        CQT = R1[:, 4096:8192].rearrange("p (c t) -> p c t", c=2)
        KT = R1[:, 8192:12288]
        VA = R1[:, 12288:16384].rearrange("p (k c) -> p k c", k=32)
        QTg = [R1[:, 16384:16896], R1[:, 16896:17408]]
        PT = [R1[:, 17408:17920], R1[:, 17920:18432]]
        LAT = R1[:, 12288:14336]
        KRl = R1[0:32, 14336:16384]
        sqb = R1[:, 18432:18944]
        BT = R1[:, 0:16384].rearrange("p (c t) -> p c t", c=8)
        MT = R2[:, :].rearrange("p (c t) -> p c t", c=4)
        GT = R2[:, :].rearrange("p (c t) -> p c t", c=4)
        TBr = WB[1][:, 0:2048].bitcast(F32)
        bcs = WB[1][:, 2048:3072].bitcast(F32)
        KT2 = WB[1][:, 4096:8192]
        VA2 = WB[1][:, 8192:12288].rearrange("p (k c) -> p k c", k=32)
        KTs = [KT, KT2]
        VAs = [VA, VA2]
        WKpads = [WKpad[:], WB[1][:, 320:448]]
        WQpads = [WQpad[:], WB[1][:, 0:256].rearrange("p (c f) -> p c f", c=2)]
        WQsws = [WQsw[:], WB[1][:, 256:320].rearrange("p (c f) -> p c f", c=2)]
        rl = WB[1][:, 3072:4096].bitcast(F32)
        silu_t = [TA[:, 0:512], TA[:, 512:1024]]
        SGG = R2[:, 0:1024].bitcast(F32)
        SGB = R2[:, 1024:2048].bitcast(F32)
        abf = R1[:, 18944:19456]
        vnb = R1[:, 19456:19968]
        junk = R1[:, 19968:20480]

        p = Prog(nc)

        def V(fn, r, w, **k):
            return p.op("vector", fn, r=r, w=w, **k)

        def A(fn, r, w, **k):
            return p.op("scalar", fn, r=r, w=w, **k)

        def G(fn, r, w, **k):
            return p.op("gpsimd", fn, r=r, w=w, **k)

        def MM(out, lhsT, rhs, start, stop, r, w, sig=None):
            if sig is None:
                sig = stop
            return p.op("tensor", lambda e: e.matmul(out, lhsT=lhsT, rhs=rhs, start=start, stop=stop),
                        r=r, w=w, sig=sig, pe_acc=not start)

        def TR(out, in_, idt, r, w, sig=True):
            return p.op("tensor", lambda e: e.transpose(out, in_, idt), r=r, w=w, sig=sig)

        def D(eng, out, in_, r, w, **kw):
            return p.dma(eng, lambda e: e.dma_start(out=out, in_=in_, **kw), r=r, w=w)

        D("sync", ident_f[:], ident_d, [], ["ident_f"])
        D("gpsimd", ident_b[:], ident_d, [], ["ident_b"])
        D("sync", tril_f[:], tril_d, [], ["tril_f"])
        D("gpsimd", maskT[:].rearrange("p a b -> p (a b)"), mask_d, [], ["maskT"])
        D("sync", invf[:], invf_d, [], ["invf"])
        D("gpsimd", NEGS[:], negs_d, [], ["NEGS"])
        D("sync", sgn[:], sgn_d, [], ["sgn"])
        G(lambda e: e.memset(ones_b[:], 1.0), [], ["ones_b"])
        G(lambda e: e.memset(ones_f[:], 1.0), [], ["ones_f"])
        G(lambda e: e.memset(WKpad[:], 0.0), [], [("WKpad", 0)])
        G(lambda e: e.memset(WQpad[:], 0.0), [], [("WQpad", 0)])
        G(lambda e: e.memset(Mst[:], 0.0), [], ["Mst"])
        for jb in range(4):
            D("sync", X[:, jb * 4:(jb + 1) * 4, :], x_d[jb * 4:(jb + 1) * 4].rearrange("j p d -> p j d"),
              [], [("X", j) for j in range(jb * 4, jb * 4 + 4)])
        kf = R2[0:32, 0:2048].bitcast(F32)
        tt = R2[0:32, 2048:4096].bitcast(F32)
        uu = R2[0:32, 4096:6144].bitcast(F32)
        ti = TA[0:32, :].bitcast(I32)
        tf = R2[0:32, 6144:8192].bitcast(F32)
        SK = ["setup"]
        for hh in range(2):
            D("sync", ti, pos_d[:, hh * 1024:(hh + 1) * 1024].partition_broadcast(32), SK, SK)
            V(lambda e: e.tensor_copy(out=tf, in_=ti), SK + ["invf", "sgn"], SK)
            for which in range(2):
                dst = CS[:, which, hh * 1024:(hh + 1) * 1024]
                V(lambda e, which=which: e.tensor_scalar(out=uu, in0=tf, scalar1=invf[:, 0:1], scalar2=(0.25 if which == 0 else 0.0),
                                                           op0=ALU.mult, op1=ALU.add), SK, SK)
                V(lambda e: e.tensor_copy(out=ti, in_=uu), SK, SK)
                V(lambda e: e.tensor_copy(out=kf, in_=ti), SK, SK)
                V(lambda e: e.tensor_tensor(out=uu, in0=uu, in1=kf, op=ALU.subtract), SK, SK)
                V(lambda e: e.tensor_scalar(out=tt, in0=uu, scalar1=0.5, scalar2=None, op0=ALU.is_ge), SK, SK)
                V(lambda e: e.tensor_tensor(out=uu, in0=uu, in1=tt, op=ALU.subtract), SK, SK)
                A(lambda e: e.activation(out=uu, in_=uu, func=AF.Sin, scale=2 * math.pi), SK, SK)
                if which == 1:
                    V(lambda e, dst=dst: e.tensor_scalar(out=dst, in0=uu, scalar1=sgn[:, 0:1], scalar2=None, op0=ALU.mult), SK, SK)
                else:
                    V(lambda e, dst=dst: e.tensor_copy(out=dst, in_=uu), SK, SK)
        p.barrier()

        def rstd_from(ssum_ap, n, out_ap, key):
            A(lambda e: e.activation(out=out_ap, in_=ssum_ap, func=AF.Sqrt, bias=EPS, scale=1.0 / n), [key], [key])
            V(lambda e: e.reciprocal(out=out_ap, in_=out_ap), [key], [key])

        def layernorm_block(i, src, gt, bt, keyg, tmp=None, tk="TA", pi=0):
            xk = ("X", i)
            if tmp is None:
                tmp = TA[:]
            so = 32 * pi
            sk = "stt%d" % pi
            k1, k2, k3 = "lnmv%d" % pi, "lnr%d" % pi, "lnn%d" % pi
            V(lambda e: e.bn_stats(out=stt[:, 2 * pi, :], in_=src[:, 0:512]), [xk], [sk])
            V(lambda e: e.bn_stats(out=stt[:, 2 * pi + 1, :], in_=src[:, 512:1024]), [xk], [sk])
            V(lambda e: e.bn_aggr(out=sm[:, so:so + 2], in_=stt[:, 2 * pi:2 * pi + 2, :]), [sk], [k1])
            A(lambda e: e.activation(out=sm[:, so + 2:so + 3], in_=sm[:, so + 1:so + 2], func=AF.Sqrt, bias=EPS, scale=1.0), [k1], [k2])
            V(lambda e: e.reciprocal(out=sm[:, so + 2:so + 3], in_=sm[:, so + 2:so + 3]), [k2], [k2])
            V(lambda e: e.tensor_scalar(out=sm[:, so + 3:so + 4], in0=sm[:, so:so + 1], scalar1=-1.0, scalar2=sm[:, so + 2:so + 3], op0=ALU.mult, op1=ALU.mult),
              [k1, k2], [k3])
            A(lambda e: e.activation(out=tmp, in_=src, func=AF.Identity, bias=sm[:, so + 3:so + 4], scale=sm[:, so + 2:so + 3]), [xk, k2, k3], [tk])
            V(lambda e: e.tensor_tensor(out=tmp, in0=tmp, in1=gt[:], op=ALU.mult), [tk, keyg], [tk])
            G(lambda e: e.tensor_tensor(out=X[:, i, :], in0=tmp, in1=bt[:], op=ALU.add), [tk, keyg], [xk])

        def transpose_block(i, dst, dkey, also_f32=None):
            xk = ("X", i)
            for kc in range(8):
                TR(bank(kc // 4, (kc % 4) * 128, (kc % 4 + 1) * 128), X[:, i, kc * 128:(kc + 1) * 128], ident_f[:],
                   [xk, "ident_f"], ["B%d" % (kc // 4)], sig=(kc % 4 == 3))
            A(lambda e: e.activation(out=dst, in_=ps[:, 0:1024].rearrange("p (c t) -> p c t", c=8), func=AF.Copy), ["B0", "B1"], [dkey])
            if also_f32 is not None:
                V(lambda e: e.tensor_copy(out=also_f32, in_=ps[:, 0:1024]), ["B0", "B1"], ["WB1"])

        idx_d = 0
        idx_m = 0
        for l, kind in enumerate(kinds):
            last = (l == nl - 1)
            WLAT = WB[0][:, 0:3328].rearrange("p (c f) -> p c f", c=8)
            WUQ = WB[0][:, 3328:4864].rearrange("p (c f) -> p c f", c=2)
            WUKV = WB[0][:, 4864:5888]
            WOM = WB[0][:, 5888:9984].rearrange("p (c f) -> p c f", c=4)
            WUV = WB[1][:, 0:8192].rearrange("p (c f) -> p c f", c=8)
            WOA = WB[1][:, 8192:12288].rearrange("p (c f) -> p c f", c=4)
            D("gpsimd", WLAT, w_in_d[l, :, 1024:1440].rearrange("(c p) f -> p c f", p=128), [], ["WB0"])
            for hh in range(2):
                D("gpsimd", WUV[:, hh * 4:(hh + 1) * 4, :], w_in_d[l, hh * 512:(hh + 1) * 512, 0:1024].rearrange("(c p) f -> p c f", p=128), [], ["WB1"])
            D("sync", OG[:], out_g_d[l].rearrange("(c p) -> p c", p=128), [], ["OG"], allow_slow_non_contiguous=True)
            D("sync", QG[:], q_norm_g_d[l].rearrange("(c p) -> p c", p=128), [], ["QG"], allow_slow_non_contiguous=True)
            D("sync", KG[:], kv_norm_g_d[l].rearrange("(c p) -> p c", p=128), [], ["KG"], allow_slow_non_contiguous=True)
            D("sync", LNG[:], ln1_g_d[l:l + 1, :].partition_broadcast(128), [], ["LN"])
            D("sync", LNB[:], ln1_b_d[l:l + 1, :].partition_broadcast(128), [], ["LN"])
            D("sync", SGG[:], sg_v_g_d[l:l + 1, :].partition_broadcast(128), [], ["SG"])
            D("sync", SGB[:], sg_v_b_d[l:l + 1, :].partition_broadcast(128), [], ["SG"])
            D("gpsimd", bsr[:], sg_b_s_d[l:l + 1, :], [], ["bsr"])
            D("gpsimd", bgr[:], ple_b_g_d[l:l + 1, :], [], ["bgr"])
            stg = [TA, TA]
            si = 0

            def fold(dst, src_ap, ncols, gcol, gkey, wkey):
                nonlocal si
                t_ = stg[si % 2]
                tk = "TA"
                si += 1
                D("sync", t_[:, 0:ncols], src_ap, [], [tk])
                V(lambda e: e.tensor_scalar(out=dst, in0=t_[:, 0:ncols], scalar1=gcol, scalar2=None, op0=ALU.mult), [tk, gkey], [wkey])

            for c in range(2):
                fold(WUQ[:, c, :], w_uq_d[l, c * 128:(c + 1) * 128, :], 768, QG[:, c:c + 1], "QG", "WB0")
            fold(WUKV, w_ukv_d[l], 1024, KG[:, 0:1], "KG", "WB0")
            for c in range(4):
                fold(WOA[:, c, :], w_o_d[l, c * 128:(c + 1) * 128, :], 1024, OG[:, c:c + 1], "OG", "WB1")
            for c in range(4):
                fold(WOM[:, c, :], w_o_d[l, 512 + c * 128:512 + (c + 1) * 128, :], 1024, OG[:, 4 + c:5 + c], "OG", "WB0")
            wsn = TA[:].rearrange("p (g s) -> p g s", g=8)
            D("sync", wsn, sg_w_s_d[l].rearrange("g t s -> t g s"), [], ["TA"])
            V(lambda e: e.tensor_tensor(out=wsn, in0=wsn, in1=tril_f[:].unsqueeze(1).to_broadcast([128, 8, 128]), op=ALU.mult), ["TA", "tril_f"], ["TA"])
            for g in range(8):
                TR(bank(g // 4, (g % 4) * 128, (g % 4 + 1) * 128), wsn[:, g, :], ident_f[:], ["TA", "ident_f"], ["B%d" % (g // 4)], sig=(g % 4 == 3))
            A(lambda e: e.activation(out=wsT[:].rearrange("p g t -> p (g t)"), in_=ps[:, 0:1024], func=AF.Copy), ["B0", "B1"], ["wsT"])
            V(lambda e: e.tensor_copy(out=WLsw[:, :, 0:16], in_=WLAT[:, :, 400:416]), ["WB0"], ["WLsw"])
            V(lambda e: e.tensor_copy(out=WLsw[:, :, 16:32], in_=WLAT[:, :, 384:400]), ["WB0"], ["WLsw"])

            TAp = [TA[:], R1[:, 0:2048].bitcast(F32)]
            xTp2 = [xTt0[:], R1[:, 2048:3072].rearrange("p (c t) -> p c t", c=8)]
            aTp = [aTt[:], R1[:, 3072:3584].rearrange("p (c t) -> p c t", c=4)]
            cqp = [cqn[:], R1[:, 3584:3968]]
            abp = [abf, R1[:, 8192:8704]]
            vnp = [vnb, R1[:, 8704:9216]]
            krp = [krt[:], R1[0:32, 9216:9728].bitcast(F32)]
            def a12_stage(i, which):
                xk = ("X", i)
                pi = i % 2
                so = 32 * pi
                xt = xTp2[pi]; xtk = ("xTt", pi)
                TA_ = TAp[pi]; tak = "TA" if pi == 0 else "TA1"
                cq_ = cqp[pi]; cqk = "cqn%d" % pi
                ab_ = abp[pi]; abk = "abf%d" % pi
                vn_ = vnp[pi]; vnk = "vnb%d" % pi
                aT_ = aTp[pi]; aTk = "aTt%d" % pi
                kr_ = krp[pi]
                ksq, kskv, ksa = "sq%d" % pi, "skv%d" % pi, "sa%d" % pi
                k1, k2, k3, skk = "lnmv%d" % pi, "lnr%d" % pi, "lnn%d" % pi, "stt%d" % pi
                if which == 1:
                    transpose_block(i, xt, xtk)
                    A(lambda e, i=i: e.activation(out=X[:, i, :], in_=X[:, i, :], func=AF.Copy, scale=ALPHA), [xk], [xk])
                    for kc in range(8):
                        MM(bank(2, 0, 384), xt[:, kc, :], WLAT[:, kc, 0:384], kc == 0, kc == 7, [xtk, "WB0"], ["B2"])
                    for kc in range(8):
                        MM(bank(3, 0, 128, 0, 32), WLAT[:, kc, 384:416], xt[:, kc, :], kc == 0, kc == 7, [xtk, "WB0"], ["B3"], sig=False)
                    for kc in range(8):
                        MM(bank(3, 128, 256, 0, 32), WLsw[:, kc, :], xt[:, kc, :], kc == 0, kc == 7, [xtk, "WLsw"], ["B3"])
                    for hh in range(2):
                        for kc in range(8):
                            MM(bank(5 + hh), xt[:, kc, :], WUV[:, kc, hh * 512:(hh + 1) * 512], kc == 0, kc == 7, [xtk, "WB1"], ["B%d" % (5 + hh)])
                elif which == 2:
                    A(lambda e, so=so: e.activation(out=junk[:, 0:256], in_=bank(2, 0, 256), func=AF.Square, accum_out=sm[:, so + 8:so + 9]), ["B2"], ["junk", ksq])
                    A(lambda e, so=so: e.activation(out=junk[:, 0:128], in_=bank(2, 256, 384), func=AF.Square, accum_out=sm[:, so + 9:so + 10]), ["B2"], ["junk", kskv])
                    rstd_from(sm[:, so + 8:so + 9], 256.0, sm[:, so + 8:so + 9], ksq)
                    rstd_from(sm[:, so + 9:so + 10], 128.0, sm[:, so + 9:so + 10], kskv)
                    V(lambda e, so=so, cq_=cq_: e.tensor_scalar(out=cq_[:, 0:256], in0=bank(2, 0, 256), scalar1=sm[:, so + 8:so + 9], scalar2=None, op0=ALU.mult), ["B2", ksq], [cqk])
                    V(lambda e, so=so, cq_=cq_: e.tensor_scalar(out=cq_[:, 256:384], in0=bank(2, 256, 384), scalar1=sm[:, so + 9:so + 10], scalar2=None, op0=ALU.mult), ["B2", kskv], [cqk])
                    for c in range(3):
                        TR(bankb(4, c * 128, (c + 1) * 128), cq_[:, c * 128:(c + 1) * 128], ident_b[:], [cqk, "ident_b"], ["B4"], sig=(c == 2))
                    A(lambda e, i=i: e.activation(out=CQT[:, :, i * 128:(i + 1) * 128], in_=bankb(4, 0, 256).rearrange("p (c t) -> p c t", c=2), func=AF.Copy),
                      ["B4"], [("CQT", i)])
                    V(lambda e, i=i: e.tensor_copy(out=LAT[:, i * 128:(i + 1) * 128], in_=bankb(4, 256, 384)), ["B4"], ["LAT"])
                    V(lambda e, i=i, kr_=kr_: e.tensor_tensor(out=kr_[:, 0:128], in0=bank(3, 0, 128, 0, 32), in1=CS[:, 0, i * 128:(i + 1) * 128], op=ALU.mult),
                      ["B3"], ["krt0_%d" % pi])
                    V(lambda e, i=i, kr_=kr_: e.tensor_tensor(out=kr_[:, 128:256], in0=bank(3, 128, 256, 0, 32), in1=CS[:, 1, i * 128:(i + 1) * 128], op=ALU.mult),
                      ["B3"], ["krt1_%d" % pi])
                    V(lambda e, i=i, kr_=kr_: e.tensor_tensor(out=KRl[:, i * 128:(i + 1) * 128], in0=kr_[:, 0:128], in1=kr_[:, 128:256], op=ALU.add),
                      ["krt0_%d" % pi, "krt1_%d" % pi], ["KRl"])
                    A(lambda e, TA_=TA_: e.activation(out=TA_, in_=ps[:, 5 * 512:7 * 512], func=AF.Gelu), ["B5", "B6"], [tak])
                elif which == 3:
                    V(lambda e, TA_=TA_, pi=pi: e.bn_stats(out=stt[:, 2 * pi, :], in_=TA_[:, 512:1024]), [tak], [skk])
                    V(lambda e, so=so, pi=pi: e.bn_aggr(out=sm[:, so:so + 2], in_=stt[:, 2 * pi:2 * pi + 1, :]), [skk], [k1])
                    A(lambda e, so=so: e.activation(out=sm[:, so + 2:so + 3], in_=sm[:, so + 1:so + 2], func=AF.Sqrt, bias=EPS, scale=1.0), [k1], [k2])
                    V(lambda e, so=so: e.reciprocal(out=sm[:, so + 2:so + 3], in_=sm[:, so + 2:so + 3]), [k2], [k2])
                    V(lambda e, so=so: e.tensor_scalar(out=sm[:, so + 3:so + 4], in0=sm[:, so:so + 1], scalar1=-1.0, scalar2=sm[:, so + 2:so + 3], op0=ALU.mult, op1=ALU.mult), [k1, k2], [k3])
                    A(lambda e, so=so, TA_=TA_: e.activation(out=TA_[:, 512:1024], in_=TA_[:, 512:1024], func=AF.Identity, bias=sm[:, so + 3:so + 4], scale=sm[:, so + 2:so + 3]), [tak, k2, k3], [tak])
                    V(lambda e, TA_=TA_: e.tensor_tensor(out=TA_[:, 512:1024], in0=TA_[:, 512:1024], in1=SGG[:], op=ALU.mult), [tak, "SG"], [tak])
                    V(lambda e, TA_=TA_, vn_=vn_: e.tensor_tensor(out=vn_[:], in0=TA_[:, 512:1024], in1=SGB[:], op=ALU.add), [tak, "SG"], [vnk])
                elif which == 4:
                    for g in range(8):
                        MM(bank(7, g * 64, (g + 1) * 64), wsT[:, g, :], vn_[:, g * 64:(g + 1) * 64], True, False, ["wsT", vnk], ["B7"], sig=False)
                        MM(bank(7, g * 64, (g + 1) * 64), bsr[0:1, g * 128:(g + 1) * 128], ones_b[0:1, 0:64], False, True, ["bsr", "ones_b"], ["B7"], sig=(g == 7))
                    V(lambda e, TA_=TA_, ab_=ab_: e.tensor_tensor(out=ab_[:], in0=TA_[:, 0:512], in1=bank(7), op=ALU.mult), [tak, "B7"], [abk])
                    A(lambda e, so=so, ab_=ab_: e.activation(out=junk[:], in_=ab_[:], func=AF.Square, accum_out=sm[:, so + 10:so + 11]), [abk], ["junk", ksa])
                    rstd_from(sm[:, so + 10:so + 11], 512.0, sm[:, so + 10:so + 11], ksa)
                    for c in range(4):
                        TR(bankb(4, 512 + c * 128, 512 + (c + 1) * 128), ab_[:, c * 128:(c + 1) * 128], ident_b[:], [abk, "ident_b"], ["B4b"], sig=(c == 3))
                    A(lambda e, aT_=aT_: e.activation(out=aT_, in_=bankb(4, 512, 1024).rearrange("p (c t) -> p c t", c=4), func=AF.Copy), ["B4b"], [aTk])
                    for hh in range(2):
                        for c in range(4):
                            MM(bank(5 + hh), aT_[:, c, :], WOA[:, c, hh * 512:(hh + 1) * 512], c == 0, c == 3, [aTk, "WB1"], ["B%d" % (5 + hh)])
                    V(lambda e, i=i, so=so: e.scalar_tensor_tensor(out=X[:, i, :], in0=ps[:, 5 * 512:7 * 512], scalar=sm[:, so + 10:so + 11], in1=X[:, i, :], op0=ALU.mult, op1=ALU.add),
                      ["B5", "B6", ksa, xk], [xk])


            for it in range(NB + 1):
                if it < NB:
                    a12_stage(it, 1)
                if it >= 1:
                    a12_stage(it - 1, 3)
                if it < NB:
                    a12_stage(it, 2)
                if it >= 1:
                    a12_stage(it - 1, 4)

            D("sync", ag_in[l][0:128, :], LAT, ["LAT"], ["ag_in"])
            D("sync", ag_in[l][128:160, :], KRl, ["KRl"], ["ag_in"])
            p.dma("gpsimd", lambda e, l=l: e.collective_compute("AllGather", ALU.bypass, replica_groups=[[0, 1], [2, 3], [4, 5], [6, 7]],
                                                                ins=[ag_in[l].opt()], outs=[ag_out[l].opt()]), r=["ag_in"], w=["ag_out"], inc=1)
            p.barrier()
            for hk in range(2):
                D("sync", CKV[:, hk * T:(hk + 1) * T], ag_out[l][hk * 160:hk * 160 + 128, :], ["ag_out"], [("CKV", hk)])
                D("sync", KT[0:32, hk * T:(hk + 1) * T], ag_out[l][hk * 160 + 128:hk * 160 + 160, :], ["ag_out"], [("KTr", hk)])
                D("sync", KT2[0:32, hk * T:(hk + 1) * T], ag_out[l][hk * 160 + 128:hk * 160 + 160, :], ["ag_out"], [("KTr", hk)])
            p.barrier()
            for KTx in (KT, KT2):
                G(lambda e, KTx=KTx: e.memset(KTx[32:64, :], 0.0), [], ["KTc"])
                G(lambda e, KTx=KTx: e.memset(KTx[32:33, :], 1.0), [], ["KTc"])
            G(lambda e: e.memset(WKpads[1], 0.0), [], [("WKpad", 1)])
            G(lambda e: e.memset(WQpads[1], 0.0), [], [("WQpad", 1)])
            for gq in range(2):
                G(lambda e, gq=gq: e.memset(QTg[gq][32:64, :], 0.0), [], [("QT", gq)])
            p.barrier()
            ckv_keys = [("CKV", 0), ("CKV", 1)]
            ktr_keys = [("KTr", 0), ("KTr", 1), "KTc"]

            def pro_pads(h):
                hp = h % 2
                V(lambda e: e.tensor_copy(out=WKpads[hp][:, 64:128], in_=WUKV[:, h * 128:h * 128 + 64]), ["WB0"], [("WKpad", hp)])
                V(lambda e: e.tensor_copy(out=WQpads[hp][:, :, 0:32], in_=WUQ[:, :, h * 96 + 64:h * 96 + 96]), ["WB0"], [("WQpad", hp)])
                V(lambda e: e.tensor_copy(out=WQpads[hp][:, :, 64:128], in_=WUQ[:, :, h * 96:h * 96 + 64]), ["WB0"], [("WQpad", hp)])
                V(lambda e: e.tensor_copy(out=WQsws[hp][:, :, 0:16], in_=WUQ[:, :, h * 96 + 80:h * 96 + 96]), ["WB0"], [("WQsw", hp)])
                V(lambda e: e.tensor_copy(out=WQsws[hp][:, :, 16:32], in_=WUQ[:, :, h * 96 + 64:h * 96 + 80]), ["WB0"], [("WQsw", hp)])

            def pro_k(h, c):
                hp = h % 2
                KTx = KTs[hp]
                b_ = c % 2
                MM(bank(b_), WKpads[hp], CKV[:, c * 512:(c + 1) * 512], True, True, [("WKpad", hp), ("CKV", c // 4)], ["B%d" % b_])
                if c % 2 == 0:
                    A(lambda e: e.activation(out=KTx[64:128, c * 512:(c + 1) * 512], in_=bank(b_, 0, 512, 64, 128), func=AF.Copy), ["B%d" % b_], [("KTn", hp, c)])
                else:
                    V(lambda e: e.tensor_copy(out=KTx[64:128, c * 512:(c + 1) * 512], in_=bank(b_, 0, 512, 64, 128)), ["B%d" % b_], [("KTn", hp, c)])

            def pro_v(h, q4):
                hp = h % 2
                VAx = VAs[hp]
                voff = 0 if hp == 0 else 64
                b_ = q4 % 2
                for kk in range(8):
                    kb = q4 * 8 + kk
                    MM(bank(b_, kk * 64, (kk + 1) * 64), CKV[:, kb * 128:(kb + 1) * 128], WUKV[:, h * 128 + 64:h * 128 + 128], True, True,
                       [("CKV", kb // 16), "WB0"], ["B%d" % b_], sig=(kk == 7))
                if q4 % 2 == 0:
                    A(lambda e: e.activation(out=VAx[:, q4 * 8:(q4 + 1) * 8, voff:voff + 64],
                                             in_=bank(b_).rearrange("p (k c) -> p k c", k=8), func=AF.Copy), ["B%d" % b_], [("VA", hp)])
                else:
                    V(lambda e: e.tensor_copy(out=VAx[:, q4 * 8:(q4 + 1) * 8, voff:voff + 64],
                                              in_=bank(b_).rearrange("p (k c) -> p k c", k=8)), ["B%d" % b_], [("VA", hp)])
                if q4 == 3:
                    onescol = 64 if hp == 0 else 0
                    G(lambda e: e.memset(VAx[:, :, onescol:onescol + 1], 1.0), [("VA", hp)], [("VA", hp)])

            def prologue(h):
                pro_pads(h)
                for c in range(8):
                    pro_k(h, c)
                for q4 in range(4):
                    pro_v(h, q4)

            kalls = [ktr_keys + [("KTn", hp_, c) for c in range(8)] for hp_ in range(2)]

            def qbuild(h, Gq, u):
                qbuild_a(h, Gq, u)
                qbuild_b(h, Gq, u)

            def qbuild_a(h, Gq, u):
                qb = u % 2
                QT_ = QTg[qb]
                qk = ("QT", qb)
                cqk = [("CQT", Gq * 4 + t_) for t_ in range(4)]
                for c in range(2):
                    MM(bank(0), WQpads[h % 2][:, c, :], CQT[:, c, Gq * 512:(Gq + 1) * 512], c == 0, c == 1, [("WQpad", h % 2)] + cqk, ["B0"])
                for c in range(2):
                    MM(bank(1, 0, 512, 0, 32), WQsws[h % 2][:, c, :], CQT[:, c, Gq * 512:(Gq + 1) * 512], c == 0, c == 1, [("WQsw", h % 2)] + cqk, ["B1"])
                V(lambda e: e.tensor_copy(out=QT_[64:128, :], in_=bank(0, 0, 512, 64, 128)), ["B0"], [qk])
                G(lambda e: e.memset(QT_[32:33, :], 0.0), [], [qk])
                V(lambda e: e.tensor_tensor(out=TA[0:32, 0:512], in0=bank(0, 0, 512, 0, 32), in1=CS[:, 0, Gq * 512:(Gq + 1) * 512], op=ALU.mult), ["B0"], ["TA"])
                V(lambda e: e.tensor_tensor(out=TA[0:32, 512:1024], in0=bank(1, 0, 512, 0, 32), in1=CS[:, 1, Gq * 512:(Gq + 1) * 512], op=ALU.mult), ["B1"], ["TA"])
                V(lambda e: e.tensor_tensor(out=QT_[0:32, :], in0=TA[0:32, 0:512], in1=TA[0:32, 512:1024], op=ALU.add), ["TA"], [qk])
                V(lambda e: e.tensor_tensor(out=abf[:], in0=QT_[:, :], in1=KTs[h % 2][:, Gq * 512:(Gq + 1) * 512], op=ALU.mult), [qk] + kalls[h % 2], ["abf"])
                G(lambda e: e.tensor_tensor(out=vnb[:], in0=QT_[:, :], in1=KTs[h % 2][:, T + Gq * 512:T + (Gq + 1) * 512], op=ALU.mult), [qk] + kalls[h % 2], ["vnb"])

            def qbuild_b(h, Gq, u):
                qb = u % 2
                QT_ = QTg[qb]
                qk = ("QT", qb)
                MM(bank(2, 0, 512, 0, 33), NEGS[:, 0:33], abf[:], True, False, ["NEGS", "abf"], ["B2"], sig=False)
                MM(bank(2, 0, 512, 0, 33), NEGS[:, 33:66], vnb[:], False, True, ["NEGS", "vnb"], ["B2"], sig=True)
                A(lambda e: e.activation(out=QT_[32:33, :], in_=bank(2, 0, 512, 32, 33), func=AF.Copy), ["B2"], [qk])

            def tail(h, Gq, u):
                tail_a(h, Gq, u)
                tail_b(h, Gq, u)

            def tail_a(h, Gq, u):
                par = h % 2
                bo = 7 if u % 2 == 0 else 3
                lrow = 64 if par == 0 else 0
                V(lambda e: e.reciprocal(out=rl[lrow:lrow + 1, :], in_=bank(bo, 0, 512, lrow, lrow + 1)), ["B%d" % bo], ["WB1"])

            def tail_b(h, Gq, u):
                par = h % 2
                bo = 7 if u % 2 == 0 else 3
                bok = "B%d" % bo
                Mv = 65 if par == 0 else 128
                orow0 = 0 if par == 0 else 64
                lrow = 64 if par == 0 else 0
                MM(bank(2, 0, 512, 0, Mv if par else 64), ones_f[lrow:lrow + 1, 0:(128 if par else 64)], rl[lrow:lrow + 1, :], True, True, ["WB1", "ones_f"], ["B2"])
                V(lambda e: e.tensor_copy(out=bcs[orow0:orow0 + 64, :], in_=bank(2, 0, 512, orow0, orow0 + 64)), ["B2"], ["WB1"])
                V(lambda e: e.tensor_tensor(out=MT[orow0:orow0 + 64, h // 2, Gq * 512:(Gq + 1) * 512],
                                            in0=bank(bo, 0, 512, orow0, orow0 + 64), in1=bcs[orow0:orow0 + 64, :], op=ALU.mult),
                  [bok, "WB1"], [("MT", Gq)])
                V(lambda e: e.tensor_tensor(out=sqb[orow0:orow0 + 64, :], in0=MT[orow0:orow0 + 64, h // 2, Gq * 512:(Gq + 1) * 512],
                                            in1=MT[orow0:orow0 + 64, h // 2, Gq * 512:(Gq + 1) * 512], op=ALU.mult),
                  [("MT", Gq)], ["sqb"])
                for tb in range(4):
                    col = Gq * 4 + tb
                    MM(bank(4, h * 16 + col, h * 16 + col + 1), sqb[orow0:orow0 + 64, tb * 128:(tb + 1) * 128], ones_b[orow0:orow0 + 64, 0:1], True, True,
                       ["sqb", "ones_b"], ["B4s"], sig=(tb == 3))

            def pass2(h, Gq, u, hooks):
                par = h % 2
                qb = u % 2
                QT_ = QTg[qb]
                qk = ("QT", qb)
                bo = 7 if u % 2 == 0 else 3
                bok = "B%d" % bo
                Mv = 65 if par == 0 else 128
                kbl = []
                for hk in range(2):
                    for jb in range(4 * Gq):
                        kbl.append((hk * 16 + jb, 0, None))
                for hk in range(2):
                    for d_ in range(2):
                        kbl.append((hk * 16 + 4 * Gq + d_, 0, (0, hk * 2 + d_)))
                for hk in range(2):
                    for d_ in range(2):
                        kbl.append((hk * 16 + 4 * Gq + 2 + d_, 256, (256, hk * 2 + d_)))
                nk = len(kbl)

                def issue_S(ki):
                    kb, qlo, msk = kbl[ki]
                    b_ = 5 + (ki % 2)
                    MM(bank(b_, qlo, 512), KTs[par][:, kb * 128:(kb + 1) * 128], QT_[:, qlo:512], True, msk is None, [qk] + kalls[par], ["B%d" % b_])
                    if msk is not None:
                        mlo, kbq = msk
                        MM(bank(b_, mlo, mlo + 256), ident_b[:], maskT[:, kbq, :], False, True, ["ident_b", "maskT"], ["B%d" % b_])

                issue_S(0)
                for ki, (kb, qlo, msk) in enumerate(kbl):
                    b_ = 5 + (ki % 2)
                    pt = PT[ki % 2]
                    ptk = ("PT", ki % 2)
                    if ki + 1 < nk:
                        issue_S(ki + 1)
                    A(lambda e, b_=b_, qlo=qlo, pt=pt: e.activation(out=pt[:, qlo:512], in_=bank(b_, qlo, 512), func=AF.Exp, scale=ATT_SCALE), ["B%d" % b_], [ptk])
                    if qlo == 0:
                        MM(bank(bo, 0, 512, 0, Mv), VAs[par][:, kb, 0:Mv], pt[:, 0:512], ki == 0, ki == nk - 1, [("VA", par), ptk], [bok], sig=True)
                    else:
                        MM(bank(bo, 256, 512, 0, Mv), VAs[par][:, kb, 0:Mv], pt[:, 256:512], False, ki == nk - 1, [("VA", par), ptk], [bok], sig=True)
                    for fn in hooks.get(ki, ()):
                        fn()

            units = [(h, Gq) for h in range(8) for Gq in range(4)]
            prev = None
            for u, (h, Gq) in enumerate(units):
                hooks = {}
                if Gq == 0:
                    if h == 0:
                        prologue(0)
                    qbuild(h, 0, u)
                if h + 1 < 8 and Gq == 2:
                    hooks.setdefault(7, []).append(lambda h=h: pro_pads(h + 1))
                    for c in range(8):
                        hooks.setdefault(8 + c, []).append(lambda h=h, c=c: pro_k(h + 1, c))
                if h + 1 < 8 and Gq == 3:
                    for q4 in range(4):
                        hooks.setdefault(8 + 4 * q4, []).append(lambda h=h, q4=q4: pro_v(h + 1, q4))
                if Gq < 3:
                    hooks.setdefault(0, []).append(lambda h=h, Gq=Gq, u=u: qbuild_a(h, Gq + 1, u + 1))
                    hooks.setdefault(4, []).append(lambda h=h, Gq=Gq, u=u: qbuild_b(h, Gq + 1, u + 1))
                if prev is not None:
                    ph, pg, pu = prev
                    hooks.setdefault(1, []).append(lambda ph=ph, pg=pg, pu=pu: tail_a(ph, pg, pu))
                    hooks.setdefault(6, []).append(lambda ph=ph, pg=pg, pu=pu: tail_b(ph, pg, pu))
                pass2(h, Gq, u, hooks)
                prev = (h, Gq, u)
            tail(*prev)
            V(lambda e: e.reduce_sum(out=ssq[:], in_=bank(4, 0, 128).rearrange("p (h c) -> p c h", h=8), axis=AX.X), ["B4s"], ["ssq"])
            p.barrier()
            if dbg_d is not None:
                D("sync", dbg_d, R2[:, :], [], ["dbg"])
                p.barrier()

            if kind == "m":
                D("sync", wr_f[:], moe_w_r_d[idx_m].rearrange("(c p) e -> p c e", p=128), [], ["wr_f"])
                V(lambda e: e.tensor_copy(out=wr_h[:], in_=wr_f[:]), ["wr_f"], ["wr_h"])
                V(lambda e: e.tensor_tensor(out=wr_l[:], in0=wr_f[:], in1=wr_h[:], op=ALU.subtract), ["wr_f", "wr_h"], ["wr_l"])
            LN4t = [TA[:], R1[:, 16384:18432].bitcast(F32)]
            LN4k = ["TA", "TA1"]
            xlop = [xTt0[:], R1[:, 18432:19456].rearrange("p (c t) -> p c t", c=8)]
            def a4_stage(i, which):
                xk = ("X", i)
                pi = i % 2
                y0 = 2 if pi == 0 else 6
                smc = 11 + 32 * pi
                smk = "sm_%d" % pi
                if which == 1:
                    A(lambda e, i=i, smc=smc: e.activation(out=sm[:, smc:smc + 1], in_=ssq[:, i:i + 1], func=AF.Sqrt, bias=EPS, scale=1.0 / 512.0), ["ssq"], [smk])
                    V(lambda e, smc=smc: e.reciprocal(out=sm[:, smc:smc + 1], in_=sm[:, smc:smc + 1]), [smk], [smk])
                    for hh in range(2):
                        for c in range(4):
                            MM(bank(y0 + hh), MT[:, c, i * 128:(i + 1) * 128], WOM[:, c, hh * 512:(hh + 1) * 512], c == 0, c == 3, [("MT", i // 4), "WB0"], ["B%d" % (y0 + hh)])
                    if stop_after != "mixer_a":
                        V(lambda e, i=i, y0=y0, smc=smc: e.scalar_tensor_tensor(out=X[:, i, :], in0=ps[:, y0 * 512:(y0 + 2) * 512], scalar=sm[:, smc:smc + 1], in1=X[:, i, :], op0=ALU.mult, op1=ALU.add),
                          ["B%d" % y0, "B%d" % (y0 + 1), smk, xk], [xk])
                elif which == 2:
                    layernorm_block(i, X[:, i, :], LNG, LNB, "LN", tmp=LN4t[pi], tk=LN4k[pi], pi=pi)
                else:
                    transpose_block(i, BT[:, :, i * 128:(i + 1) * 128], ("BT", i), also_f32=None)
                    A(lambda e, i=i: e.activation(out=X[:, i, :], in_=X[:, i, :], func=AF.Copy, scale=ALPHA), [xk], [xk])
                    if kind == "m":
                        xlo = xlop[pi]
                        V(lambda e, i=i, xlo=xlo: e.tensor_tensor(out=xlo, in0=ps[:, 0:1024].rearrange("p (c t) -> p c t", c=8), in1=BT[:, :, i * 128:(i + 1) * 128], op=ALU.subtract),
                          ["B0", "B1", ("BT", i)], [("xTt", pi)])
                        for pi_, (lt, lk, wt, wk) in enumerate([("hi", None, wr_h, "wr_h"), ("lo", None, wr_h, "wr_h"), ("hi", None, wr_l, "wr_l")]):
                            for kc in range(8):
                                lhs = BT[:, kc, i * 128:(i + 1) * 128] if lt == "hi" else xlo[:, kc, :]
                                lkey = ("BT", i) if lt == "hi" else ("xTt", pi)
                                MM(bank(4, 0, 8), lhs, wt[:, kc, :], (pi_ == 0 and kc == 0), (pi_ == 2 and kc == 7), [lkey, wk], ["B4"])
                        V(lambda e: e.tensor_copy(out=lgt[:, 0, :], in_=bank(4, 0, 8)), ["B4"], ["lg0"])
                        V(lambda e: e.reduce_max(out=sm[:, 20:21], in_=lgt[:, 0, :], axis=AX.X), ["lg0"], ["m1"])
                        V(lambda e: e.tensor_scalar(out=lgt[:, 1, :], in0=lgt[:, 0, :], scalar1=sm[:, 20:21], scalar2=None, op0=ALU.is_ge), ["lg0", "m1"], ["lg1"])
                        V(lambda e: e.scalar_tensor_tensor(out=lgt[:, 2, :], in0=lgt[:, 1, :], scalar=-1e30, in1=lgt[:, 0, :], op0=ALU.mult, op1=ALU.add), ["lg1", "lg0"], ["lg2"])
                        V(lambda e: e.reduce_max(out=sm[:, 21:22], in_=lgt[:, 2, :], axis=AX.X), ["lg2"], ["m2"])
                        V(lambda e: e.tensor_scalar(out=lgt[:, 1, :], in0=lgt[:, 0, :], scalar1=sm[:, 21:22], scalar2=None, op0=ALU.is_ge), ["lg0", "m2", "lg2"], ["lg1b"])
                        V(lambda e: e.tensor_scalar(out=sm[:, 22:23], in0=sm[:, 20:21], scalar1=-1.0, scalar2=None, op0=ALU.mult), ["m1"], ["nm1"])
                        A(lambda e: e.activation(out=lgt[:, 3, :], in_=lgt[:, 0, :], func=AF.Exp, bias=sm[:, 22:23], scale=1.0), ["lg0", "nm1"], ["lg3"])
                        V(lambda e: e.tensor_tensor(out=lgt[:, 3, :], in0=lgt[:, 3, :], in1=lgt[:, 1, :], op=ALU.mult), ["lg3", "lg1b"], ["lg3b"])
                        V(lambda e: e.reduce_sum(out=sm[:, 23:24], in_=lgt[:, 3, :], axis=AX.X), ["lg3b"], ["sw"])
                        V(lambda e: e.reciprocal(out=sm[:, 23:24], in_=sm[:, 23:24]), ["sw"], ["sw"])
                        V(lambda e, i=i: e.tensor_scalar(out=comb[:, i, :], in0=lgt[:, 3, :], scalar1=sm[:, 23:24], scalar2=None, op0=ALU.mult), ["lg3b", "sw"], [("comb", i)])


            if stop_after in ("mixer_a", "mixer_raw"):
                for i in range(NB):
                    a4_stage(i, 1)
            elif stop_after == "mixer":
                for i in range(NB):
                    a4_stage(i, 1)
                    a4_stage(i, 2)
            else:
                for it in range(NB + 1):
                    if it < NB:
                        a4_stage(it, 1)
                    if it >= 1:
                        a4_stage(it - 1, 2)
                        a4_stage(it - 1, 3)

            if stop_after not in ("mixer", "mixer_a", "mixer_raw"):
                p.barrier()
                stages = []
                if kind == "d":
                    f0 = 0
                    while f0 < 2816:
                        fw = min(512, 2816 - f0)
                        stages.append((None, f0, fw))
                        f0 += fw
                else:
                    for e_ in range(8):
                        for f0 in range(0, 3584, 512):
                            stages.append((e_, f0, 512))

                def load_stage(si_):
                    e_, f0, fw = stages[si_]
                    slot = WB[si_ % 2]
                    sk = "WB%d" % (si_ % 2)
                    W1s = slot[:, 0:4096].rearrange("p (c f) -> p c f", c=8)
                    W3s = slot[:, 4096:8192].rearrange("p (c f) -> p c f", c=8)
                    W2s = slot[:, 8192:12288].rearrange("p (c f) -> p c f", c=4)
                    if e_ is None:
                        s1 = ffn_w1_d[idx_d]; s3 = ffn_w3_d[idx_d]; s2 = ffn_w2_d[idx_d]
                    else:
                        s1 = moe_w1_d[idx_m, e_]; s3 = moe_w3_d[idx_m, e_]; s2 = moe_w2_d[idx_m, e_]
                    for hh in range(2):
                        D("gpsimd", W1s[:, hh * 4:(hh + 1) * 4, 0:fw], s1[hh * 512:(hh + 1) * 512, f0:f0 + fw].rearrange("(c p) f -> p c f", p=128), [], [sk])
                        D("gpsimd", W3s[:, hh * 4:(hh + 1) * 4, 0:fw], s3[hh * 512:(hh + 1) * 512, f0:f0 + fw].rearrange("(c p) f -> p c f", p=128), [], [sk])
                    D("gpsimd", W2s[:, 0:fw // 128, :], s2[f0:f0 + fw, :].rearrange("(c p) d -> p c d", p=128), [], [sk])

                load_stage(0)
                for si_ in range(len(stages)):
                    if si_ + 1 < len(stages):
                        load_stage(si_ + 1)
                    if si_ == len(stages) - 1:
                        assert si_ % 2 == 1
                        D("sync", LNG[:], ln2_g_d[l:l + 1, :].partition_broadcast(128), [], ["LN"])
                        D("sync", LNB[:], ln2_b_d[l:l + 1, :].partition_broadcast(128), [], ["LN"])
                        WGp = WB[0][:, 0:8192].rearrange("p (c f) -> p c f", c=8)
                        WPp = WB[0][:, 8192:10240].rearrange("p (c f) -> p c f", c=2)
                        for hh in range(2):
                            D("gpsimd", WGp[:, hh * 4:(hh + 1) * 4, :], ple_w_g_d[l, hh * 512:(hh + 1) * 512, :].rearrange("(c p) f -> p c f", p=128), [], ["WB0"])
                        D("gpsimd", WPp, ple_w_p_d[l].rearrange("(c p) f -> p c f", p=128), [], ["WB0"])
                    e_, f0, fw = stages[si_]
                    nfc = fw // 128
                    slot = WB[si_ % 2]
                    sk = "WB%d" % (si_ % 2)
                    W1s = slot[:, 0:4096].rearrange("p (c f) -> p c f", c=8)
                    W3s = slot[:, 4096:8192].rearrange("p (c f) -> p c f", c=8)
                    W2s = slot[:, 8192:12288].rearrange("p (c f) -> p c f", c=4)
                    def ffn_up(tg):
                        btk = [("BT", tg * 4 + t_) for t_ in range(4)]
                        for fc in range(nfc):
                            u_ = (tg * nfc + fc) % 2
                            b1, b3 = 2 * u_, 2 * u_ + 1
                            for kc in range(8):
                                MM(bank(b1), W1s[:, kc, fc * 128:(fc + 1) * 128], BT[:, kc, tg * 512:(tg + 1) * 512], kc == 0, kc == 7, [sk] + btk, ["B%d" % b1])
                            for kc in range(8):
                                MM(bank(b3), W3s[:, kc, fc * 128:(fc + 1) * 128], BT[:, kc, tg * 512:(tg + 1) * 512], kc == 0, kc == 7, [sk] + btk, ["B%d" % b3])
                            stl = silu_t[u_]
                            A(lambda e, stl=stl, b1=b1: e.activation(out=stl[:], in_=bank(b1), func=AF.Silu), ["B%d" % b1], [("silu", u_)])
                            V(lambda e, stl=stl, b3=b3, fc=fc, tg=tg: e.tensor_tensor(out=GT[:, fc, tg * 512:(tg + 1) * 512], in0=stl[:], in1=bank(b3), op=ALU.mult),
                              [("silu", u_), "B%d" % b3], [("GT", tg)])

                    def ffn_down(tg):
                        for t_ in range(4):
                            tb = tg * 4 + t_
                            bo = 4 + 2 * (tb % 2)
                            for hh in range(2):
                                for fc in range(nfc):
                                    MM(bank(bo + hh), GT[:, fc, tb * 128:(tb + 1) * 128], W2s[:, fc, hh * 512:(hh + 1) * 512], fc == 0, fc == nfc - 1,
                                       [("GT", tg), sk], ["B%d" % (bo + hh)])
                            xk = ("X", tb)
                            if e_ is None:
                                V(lambda e, tb=tb, bo=bo: e.tensor_tensor(out=X[:, tb, :], in0=X[:, tb, :], in1=ps[:, bo * 512:(bo + 2) * 512], op=ALU.add),
                                  [xk, "B%d" % bo, "B%d" % (bo + 1)], [xk])
                            else:
                                V(lambda e, tb=tb, bo=bo, e_=e_: e.scalar_tensor_tensor(out=X[:, tb, :], in0=ps[:, bo * 512:(bo + 2) * 512], scalar=comb[:, tb, e_:e_ + 1],
                                                                                        in1=X[:, tb, :], op0=ALU.mult, op1=ALU.add),
                                  [xk, "B%d" % bo, "B%d" % (bo + 1), ("comb", tb)], [xk])

                    ffn_up(0)
                    for tg in range(4):
                        if tg + 1 < 4:
                            ffn_up(tg + 1)
                        ffn_down(tg)
                p.barrier()

                WG = WB[0][:, 0:8192].rearrange("p (c f) -> p c f", c=8)
                WP = WB[0][:, 8192:10240].rearrange("p (c f) -> p c f", c=2)
                LNt = [TA[:], R1[:, 0:2048].bitcast(F32)]
                LNk = ["TA", "TA1"]
                GTt = [R1[:, 2048:4096].bitcast(F32), R1[:, 4096:6144].bitcast(F32)]
                xTp = [xTt0[:], R1[:, 6144:7168].rearrange("p (c t) -> p c t", c=8)]
                pbp = [pblk[:], R1[:, 7168:7424]]
                pTp = [pTt[:], R1[:, 7424:7680].rearrange("p (c t) -> p c t", c=2)]
                def ple_stage(i, which):
                    xk = ("X", i)
                    pi = i % 2
                    xt = xTp[pi]
                    xtk = ("xTt", pi)
                    gt_ = GTt[pi]
                    gk = "GTt%d" % pi
                    pb_ = pbp[pi]
                    pT_ = pTp[pi]
                    g0 = 2 if pi == 0 else 6
                    if which == 1:
                        layernorm_block(i, X[:, i, :], LNG, LNB, "LN", tmp=LNt[pi], tk=LNk[pi], pi=pi)
                        D("gpsimd", pb_, p_d[l, i], [], ["pblk%d" % pi])
                    elif which == 2:
                        transpose_block(i, xt, xtk)
                        for c in range(2):
                            TR(bankb(4, c * 128, (c + 1) * 128), pb_[:, c * 128:(c + 1) * 128], ident_b[:], ["pblk%d" % pi, "ident_b"], ["B4"], sig=(c == 1))
                        V(lambda e, pT_=pT_: e.tensor_copy(out=pT_, in_=bankb(4, 0, 256).rearrange("p (c t) -> p c t", c=2)), ["B4"], ["pTt%d" % pi])
                        for hh in range(2):
                            for kc in range(8):
                                MM(bank(g0 + hh), xt[:, kc, :], WG[:, kc, hh * 512:(hh + 1) * 512], kc == 0, False, [xtk, "WB0"], ["B%d" % (g0 + hh)], sig=False)
                            MM(bank(g0 + hh), ones_b[0:1, 0:128], bgr[0:1, hh * 512:(hh + 1) * 512], False, True, ["ones_b", "bgr"], ["B%d" % (g0 + hh)], sig=True)
                        for hh in range(2):
                            for c in range(2):
                                MM(bank(4 + hh), pT_[:, c, :], WP[:, c, hh * 512:(hh + 1) * 512], c == 0, c == 1, ["pTt%d" % pi, "WB0"], ["B%d" % (4 + hh)])
                    else:
                        A(lambda e, gt_=gt_, g0=g0: e.activation(out=gt_, in_=ps[:, g0 * 512:(g0 + 2) * 512], func=AF.Sigmoid), ["B%d" % g0, "B%d" % (g0 + 1)], [gk])
                        V(lambda e, gt_=gt_: e.tensor_tensor(out=gt_, in0=gt_, in1=ps[:, 4 * 512:6 * 512], op=ALU.mult), [gk, "B4", "B5"], [gk])
                        G(lambda e, i=i, gt_=gt_: e.tensor_tensor(out=X[:, i, :], in0=X[:, i, :], in1=gt_, op=ALU.add), [gk, xk], [xk])


                if stop_after == "ffn":
                    for i in range(NB):
                        layernorm_block(i, X[:, i, :], LNG, LNB, "LN", tmp=LNt[i % 2], tk=LNk[i % 2], pi=i % 2)
                else:
                    for it in range(NB + 1):
                        if it < NB:
                            ple_stage(it, 1)
                        if it >= 1:
                            ple_stage(it - 1, 2)
                            ple_stage(it - 1, 3)
                p.barrier()
            if kind == "d":
                idx_d += 1
            else:
                idx_m += 1

        outk = []
        for jb in range(4):
            t_ = D("sync", y_d[jb * 4:(jb + 1) * 4].rearrange("j p d -> p j d"), X[:, jb * 4:(jb + 1) * 4, :],
                   [("X", j) for j in range(jb * 4, jb * 4 + 4)], [("y", jb)])
            outk.append(t_)
        p.wait_all("sync", outk)
        p.emit(st)
    return nc


_W_LAYER = ["w_in", "sg_v_g", "sg_v_b", "sg_w_s", "sg_b_s", "q_norm_g", "kv_norm_g", "w_uq", "w_ukv", "out_g", "w_o",
            "ln1_g", "ln1_b", "ln2_g", "ln2_b", "ple_w_g", "ple_b_g", "ple_w_p"]
_W_DENSE = ["ffn_w1", "ffn_w3", "ffn_w2"]
_W_MOE = ["moe_w_r", "moe_w1", "moe_w3", "moe_w2"]


def _gb(hf, j):
    return 4 * (j // 2) + GMAP[hf][j % 2]


def _consts(hf):
    inv_freq = 1.0 / (10000.0 ** (np.arange(0, 32, 2, dtype=np.float32) / 32.0))
    invf = (np.concatenate([inv_freq, inv_freq]) / (2 * np.pi)).astype(np.float32).reshape(32, 1)
    sgn = np.concatenate([-np.ones(16, np.float32), np.ones(16, np.float32)]).reshape(32, 1)
    ident = np.eye(128, dtype=np.float32)
    tril = np.tril(np.ones((128, 128), np.float32))
    m = np.full((128, 4, 2, 128), -30000.0, np.float32)
    ii = np.arange(128)
    diag = (ii[:, None] <= ii[None, :]).astype(np.float32)
    for r in range(2):
        qg = GMAP[hf][r]
        for kb in range(4):
            kg = GMAP[kb // 2][kb % 2]
            if kg < qg:
                m[:, kb, r, :] = 0.0
            elif kg == qg:
                m[:, kb, r, :] = (diag - 1.0) * 30000.0
    negs = np.zeros((128, 66), np.float32)
    negs[:, 32 + 33 * hf] = -1.0
    return {"invf": invf, "sgn": sgn, "ident": ident, "tril": tril, "maskT": m.reshape(128, 1024), "negs": negs}


def _run(kinds, layer_ids, xin, inputs, stop_after=None):
    nc = build(kinds, stop_after=stop_after)
    p = inputs["p"]
    positions = inputs["positions"]
    d_ids = [li // 2 for li in layer_ids if li % 2 == 0]
    m_ids = [li // 2 for li in layer_ids if li % 2 == 1]
    shared = {}
    for k in _W_LAYER:
        a = np.ascontiguousarray(inputs[k][layer_ids])
        if k == "sg_b_s":
            a = a.reshape(len(layer_ids), 1024)
        shared[k] = a
    if d_ids:
        for k in _W_DENSE:
            shared[k] = np.ascontiguousarray(inputs[k][d_ids])
    if m_ids:
        for k in _W_MOE:
            shared[k] = np.ascontiguousarray(inputs[k][m_ids])
    maps = []
    for c in range(8):
        b, hf = c // 2, c % 2
        blks = [_gb(hf, j) for j in range(NB)]
        pc = np.stack([np.stack([p[li, b, g * 128:(g + 1) * 128, :] for g in blks]) for li in layer_ids])
        pos = np.concatenate([positions[b, g * 128:(g + 1) * 128] for g in blks]).astype(np.int32).reshape(1, T)
        m = {"x": xin[c], "p": np.ascontiguousarray(pc), "pos": pos}
        m.update(_consts(hf))
        m.update(shared)
        maps.append(m)
    res = run_bass_kernel_spmd(nc, maps, core_ids=list(range(8)))
    global DBG
    DBG = [res.results[c].get("dbg") for c in range(8)]
    return [np.asarray(res.results[c]["y"]) for c in range(8)]


def _shard_x(x):
    out = []
    for c in range(8):
        b, hf = c // 2, c % 2
        out.append(np.ascontiguousarray(np.stack([x[b, _gb(hf, j) * 128:(_gb(hf, j) + 1) * 128, :] for j in range(NB)])))
    return out


def _unshard(ys, shape):
    out = np.zeros(shape, np.float32)
    for c in range(8):
        b, hf = c // 2, c % 2
        for j in range(NB):
            g = _gb(hf, j)
            out[b, g * 128:(g + 1) * 128, :] = ys[c][j]
    return out


FUSED = True
DBG = None


def kernel(**inputs):
    inputs = {k: np.asarray(v) for k, v in inputs.items()}
    x = inputs["x"].astype(np.float32)
    xs = _shard_x(x)
    if FUSED:
        ys = _run(["d", "m", "d", "m"], [0, 1, 2, 3], xs, inputs)
    else:
        ys = xs
        for li in range(4):
            ys = _run(["d" if li % 2 == 0 else "m"], [li], ys, inputs)
    return _unshard(ys, x.shape)
```

```python
import contextlib
import math
import numpy as np
import concourse.bass as bass
import concourse.mybir as mybir
from concourse.bass_utils import run_bass_kernel_spmd

F32 = mybir.dt.float32
BF16 = mybir.dt.bfloat16
I32 = mybir.dt.int32
AF = mybir.ActivationFunctionType
ALU = mybir.AluOpType
AX = mybir.AxisListType

ENGS = ["sync", "scalar", "vector", "gpsimd", "tensor"]
NDMASEM = 40
GMAP = [[0, 3], [1, 2]]
ALPHA = (2.0 * 4) ** 0.25
EPS = 1e-6
ATT_SCALE = 96 ** -0.5
NB = 16
T = 2048


class Prog:
    def __init__(self, nc):
        self.nc = nc
        self.q = {e: [] for e in ENGS}
        self.cnt = {e: 0 for e in ENGS}
        self.seen = {e: {} for e in ENGS}
        self.lastw = {}
        self.readers = {}
        self.dma_use = [0] * NDMASEM
        self.dma_rr = 0
        self.pending_nosig = {e: False for e in ENGS}

    def _need(self, eng, tickets):
        out = []
        best = {}
        for t in tickets:
            if t is None:
                continue
            k, v = t
            if v > best.get(k, 0):
                best[k] = v
        for k, v in best.items():
            if self.seen[eng].get(k, 0) >= v:
                continue
            self.seen[eng][k] = v
            out.append((k, v))
        return out

    def _rtix(self, x):
        d = self.lastw.get(x)
        return list(d.items()) if d else []

    def _deps(self, r, w):
        ts = []
        for x in r:
            ts.extend(self._rtix(x))
        for x in w:
            ts.extend(self._rtix(x))
            ts.extend(self.readers.get(x, ()))
        return ts

    def _commit(self, ticket, r, w):
        k, v = ticket
        for x in w:
            d = self.lastw.setdefault(x, {})
            if d.get(k, 0) < v:
                d[k] = v
            self.readers[x] = []
        for x in r:
            self.readers.setdefault(x, []).append(ticket)

    def op(self, eng, fn, r=(), w=(), sig=True, pe_acc=False):
        ts = self._deps(r, w)
        if pe_acc:
            ts = [t for t in ts if t[0] != eng]
        else:
            raw = set()
            for x in r:
                raw.update(self._rtix(x))
            ts = [t for t in ts if (t[0] != eng or t in raw)]
        waits = self._need(eng, ts)
        ticket = (eng, self.cnt[eng] + 1)
        if sig:
            self.cnt[eng] += 1
            self.pending_nosig[eng] = False
        else:
            self.pending_nosig[eng] = True
        self.q[eng].append((waits, fn, ("e", eng) if sig else None))
        self._commit(ticket, r, w)
        return ticket

    def dma(self, eng, fn, r=(), w=(), inc=16):
        ts = self._deps(r, w)
        idx = self.dma_rr
        self.dma_rr = (self.dma_rr + 1) % NDMASEM
        use = self.dma_use[idx]
        if use > 0:
            ts.append((("d", idx), use))
        self.dma_use[idx] = use + inc
        ticket = (("d", idx), use + inc)
        waits = self._need(eng, ts)
        self.q[eng].append((waits, fn, ("d", idx, inc)))
        self._commit(ticket, r, w)
        return ticket

    def wait_all(self, eng, tickets):
        waits = self._need(eng, tickets)
        if waits:
            self.q[eng].append((waits, None, None))

    def barrier(self):
        for e in ENGS:
            assert not self.pending_nosig[e]
        ts = [(e, self.cnt[e]) for e in ENGS if self.cnt[e] > 0]
        ts += [(("d", i), self.dma_use[i]) for i in range(NDMASEM) if self.dma_use[i] > 0]
        for e in ENGS:
            self.wait_all(e, ts)

    def emit(self, stack):
        nc = self.nc
        for e in ENGS:
            assert not self.pending_nosig[e], e
        esem = {e: stack.enter_context(nc.semaphore("se_" + e)) for e in ENGS}
        dsem = [stack.enter_context(nc.semaphore("sd_%d" % i)) for i in range(NDMASEM)]

        def semof(k):
            return esem[k] if isinstance(k, str) else dsem[k[1]]

        block = stack.enter_context(nc.Block())

        def mk(ename):
            def body(engine):
                for waits, fn, sg in self.q[ename]:
                    for k, v in waits:
                        engine.wait_ge(semof(k), v)
                    if fn is None:
                        continue
                    ins = fn(engine)
                    if sg is None:
                        continue
                    if sg[0] == "e":
                        ins.then_inc(esem[sg[1]], 1)
                    else:
                        ins.then_inc(dsem[sg[1]], sg[2])
            return body

        for e in ENGS:
            if self.q[e]:
                getattr(block, e)(mk(e))


def build(kinds, stop_after=None):
    nl = len(kinds)
    nd = kinds.count("d")
    nm = kinds.count("m")
    nc = bass.Bass("TRN2", target_bir_lowering=False)

    def DI(name, shape, dt=F32):
        return nc.dram_tensor(name, shape, dt, kind="ExternalInput").ap()

    x_d = DI("x", [NB, 128, 1024])
    p_d = DI("p", [nl, NB, 128, 256])
    pos_d = DI("pos", [1, T], I32)
    invf_d = DI("invf", [32, 1])
    sgn_d = DI("sgn", [32, 1])
    ident_d = DI("ident", [128, 128])
    tril_d = DI("tril", [128, 128])
    mask_d = DI("maskT", [128, 1024])
    negs_d = DI("negs", [128, 66])
    w_in_d = DI("w_in", [nl, 1024, 1440])
    sg_v_g_d = DI("sg_v_g", [nl, 512])
    sg_v_b_d = DI("sg_v_b", [nl, 512])
    sg_w_s_d = DI("sg_w_s", [nl, 8, 128, 128])
    sg_b_s_d = DI("sg_b_s", [nl, 1024])
    q_norm_g_d = DI("q_norm_g", [nl, 256])
    kv_norm_g_d = DI("kv_norm_g", [nl, 128])
    w_uq_d = DI("w_uq", [nl, 256, 768])
    w_ukv_d = DI("w_ukv", [nl, 128, 1024])
    out_g_d = DI("out_g", [nl, 1024])
    w_o_d = DI("w_o", [nl, 1024, 1024])
    ln1_g_d = DI("ln1_g", [nl, 1024])
    ln1_b_d = DI("ln1_b", [nl, 1024])
    ln2_g_d = DI("ln2_g", [nl, 1024])
    ln2_b_d = DI("ln2_b", [nl, 1024])
    ple_w_g_d = DI("ple_w_g", [nl, 1024, 1024])
    ple_b_g_d = DI("ple_b_g", [nl, 1024])
    ple_w_p_d = DI("ple_w_p", [nl, 256, 1024])
    if nd:
        ffn_w1_d = DI("ffn_w1", [nd, 1024, 2816])
        ffn_w3_d = DI("ffn_w3", [nd, 1024, 2816])
        ffn_w2_d = DI("ffn_w2", [nd, 2816, 1024])
    if nm:
        moe_w_r_d = DI("moe_w_r", [nm, 1024, 8])
        moe_w1_d = DI("moe_w1", [nm, 8, 1024, 3584])
        moe_w3_d = DI("moe_w3", [nm, 8, 1024, 3584])
        moe_w2_d = DI("moe_w2", [nm, 8, 3584, 1024])
    y_d = nc.dram_tensor("y", [NB, 128, 1024], F32, kind="ExternalOutput").ap()
    dbg_d = nc.dram_tensor("dbg", [128, 8192], BF16, kind="ExternalOutput").ap() if stop_after == "mixer_raw" else None
    ag_in = [nc.dram_tensor("ag_in%d" % l, [160, T], BF16).ap() for l in range(nl)]
    ag_out = [nc.dram_tensor("ag_out%d" % l, [320, T], BF16).ap() for l in range(nl)]

    st = contextlib.ExitStack()
    with st:
        def SB(name, shape, dt=F32):
            return st.enter_context(nc.sbuf_tensor(name, shape, dt))

        ps = st.enter_context(nc.psum_tensor("ps", [128, 4096], F32))

        def bank(b, lo=0, hi=512, p0=0, p1=128):
            return ps[p0:p1, b * 512 + lo:b * 512 + hi]

        def bankb(b, lo=0, hi=512, p0=0, p1=128):
            v = ps[p0:p1, b * 512:(b + 1) * 512].bitcast(BF16)
            return v[:, lo:hi]

        X = SB("X", [128, NB, 1024])
        WB = [SB("WB0", [128, 12288], BF16), SB("WB1", [128, 12288], BF16)]
        R1 = SB("R1", [128, 20480], BF16)
        R2 = SB("R2", [128, 8192], BF16)
        CS = SB("CS", [32, 2, T], BF16)
        LNG = SB("LNG", [128, 1024]); LNB = SB("LNB", [128, 1024])
        TA = SB("TA", [128, 1024])
        ident_f = SB("ident_f", [128, 128]); ident_b = SB("ident_b", [128, 128], BF16)
        tril_f = SB("tril_f", [128, 128])
        maskT = SB("maskTs", [128, 4, 256], BF16)
        ones_b = SB("ones_b", [128, 128], BF16); ones_f = SB("ones_f", [128, 128])
        NEGS = SB("NEGS", [128, 66], BF16)
        OG = SB("OG", [128, 8]); QG = SB("QG", [128, 2]); KG = SB("KG", [128, 1])
        invf = SB("invf_s", [32, 1]); sgn = SB("sgn_s", [32, 1])
        bsr = SB("bsr", [1, 1024], BF16); bgr = SB("bgr", [1, 1024], BF16)
        wsT = SB("wsT", [128, 8, 128], BF16)
        WKpad = SB("WKpad", [128, 128], BF16)
        WQpad = SB("WQpad", [128, 2, 128], BF16); WQsw = SB("WQsw", [128, 2, 32], BF16)
        WLsw = SB("WLsw", [128, 8, 32], BF16)
        xTt0 = SB("xTt0", [128, 8, 128], BF16)
        xTt = [xTt0, xTt0]
        sm = SB("sm", [128, 64])
        mstat = SB("mstat", [128, 16])
        Mst = SB("Mst", [128, 33], BF16)
        comb = SB("comb", [128, NB, 8])
        ssq = SB("ssq", [128, NB])
        wr_f = SB("wr_f", [128, 8, 8])
        wr_h = SB("wr_h", [128, 8, 8], BF16); wr_l = SB("wr_l", [128, 8, 8], BF16)
        lgt = SB("lgt", [128, 4, 8])
        stt = SB("stt", [128, 4, 6])
        aTt = SB("aTt", [128, 4, 128], BF16)
        cqn = SB("cqn", [128, 384], BF16)
        pblk = SB("pblk", [128, 256], BF16); pTt = SB("pTt", [128, 2, 128], BF16)
        krt = SB("krt", [32, 256])

        CKV = R1[:, 0:4096]
        CQT = R1[:, 4096:8192].rearrange("p (c t) -> p c t", c=2)
        KT = R1[:, 8192:12288]
        VA = R1[:, 12288:16384].rearrange("p (k c) -> p k c", k=32)
        QTg = [R1[:, 16384:16896], R1[:, 16896:17408]]
        PT = [R1[:, 17408:17920], R1[:, 17920:18432], R1[:, 19968:20480]]
        LAT = R1[:, 12288:14336]
        KRl = R1[0:32, 14336:16384]
        sqb = R1[:, 18432:18944]
        BT = R1[:, 0:16384].rearrange("p (c t) -> p c t", c=8)
        MT = R2[:, :].rearrange("p (c t) -> p c t", c=4)
        GT = R2[:, :].rearrange("p (c t) -> p c t", c=4)
        TBr = WB[1][:, 0:2048].bitcast(F32)
        bcs = WB[1][:, 2048:3072].bitcast(F32)
        rl = WB[1][:, 3072:4096].bitcast(F32)
        silu_t = [TA[:, 0:512], TA[:, 512:1024]]
        SGG = R2[:, 0:1024].bitcast(F32)
        SGB = R2[:, 1024:2048].bitcast(F32)
        abf = R1[:, 18944:19456]
        vnb = R1[:, 19456:19968]
        junk = R1[:, 19968:20480]

        p = Prog(nc)

        def V(fn, r, w, **k):
            return p.op("vector", fn, r=r, w=w, **k)

        def A(fn, r, w, **k):
            return p.op("scalar", fn, r=r, w=w, **k)

        def G(fn, r, w, **k):
            return p.op("gpsimd", fn, r=r, w=w, **k)

        def MM(out, lhsT, rhs, start, stop, r, w, sig=None):
            if sig is None:
                sig = stop
            return p.op("tensor", lambda e: e.matmul(out, lhsT=lhsT, rhs=rhs, start=start, stop=stop),
                        r=r, w=w, sig=sig, pe_acc=not start)

        def TR(out, in_, idt, r, w, sig=True):
            return p.op("tensor", lambda e: e.transpose(out, in_, idt), r=r, w=w, sig=sig)

        def D(eng, out, in_, r, w, **kw):
            return p.dma(eng, lambda e: e.dma_start(out=out, in_=in_, **kw), r=r, w=w)

        D("sync", ident_f[:], ident_d, [], ["ident_f"])
        D("gpsimd", ident_b[:], ident_d, [], ["ident_b"])
        D("sync", tril_f[:], tril_d, [], ["tril_f"])
        D("gpsimd", maskT[:].rearrange("p a b -> p (a b)"), mask_d, [], ["maskT"])
        D("sync", invf[:], invf_d, [], ["invf"])
        D("gpsimd", NEGS[:], negs_d, [], ["NEGS"])
        D("sync", sgn[:], sgn_d, [], ["sgn"])
        G(lambda e: e.memset(ones_b[:], 1.0), [], ["ones_b"])
        G(lambda e: e.memset(ones_f[:], 1.0), [], ["ones_f"])
        G(lambda e: e.memset(WKpad[:], 0.0), [], ["WKpad"])
        G(lambda e: e.memset(WQpad[:], 0.0), [], ["WQpad"])
        G(lambda e: e.memset(Mst[:], 0.0), [], ["Mst"])
        for jb in range(4):
            D("sync", X[:, jb * 4:(jb + 1) * 4, :], x_d[jb * 4:(jb + 1) * 4].rearrange("j p d -> p j d"),
              [], [("X", j) for j in range(jb * 4, jb * 4 + 4)])
        kf = R2[0:32, 0:2048].bitcast(F32)
        tt = R2[0:32, 2048:4096].bitcast(F32)
        uu = R2[0:32, 4096:6144].bitcast(F32)
        ti = TA[0:32, :].bitcast(I32)
        tf = R2[0:32, 6144:8192].bitcast(F32)
        SK = ["setup"]
        for hh in range(2):
            D("sync", ti, pos_d[:, hh * 1024:(hh + 1) * 1024].partition_broadcast(32), SK, SK)
            V(lambda e: e.tensor_copy(out=tf, in_=ti), SK + ["invf", "sgn"], SK)
            for which in range(2):
                dst = CS[:, which, hh * 1024:(hh + 1) * 1024]
                V(lambda e, which=which: e.tensor_scalar(out=uu, in0=tf, scalar1=invf[:, 0:1], scalar2=(0.25 if which == 0 else 0.0),
                                                           op0=ALU.mult, op1=ALU.add), SK, SK)
                V(lambda e: e.tensor_copy(out=ti, in_=uu), SK, SK)
                V(lambda e: e.tensor_copy(out=kf, in_=ti), SK, SK)
                V(lambda e: e.tensor_tensor(out=uu, in0=uu, in1=kf, op=ALU.subtract), SK, SK)
                V(lambda e: e.tensor_scalar(out=tt, in0=uu, scalar1=0.5, scalar2=None, op0=ALU.is_ge), SK, SK)
                V(lambda e: e.tensor_tensor(out=uu, in0=uu, in1=tt, op=ALU.subtract), SK, SK)
                A(lambda e: e.activation(out=uu, in_=uu, func=AF.Sin, scale=2 * math.pi), SK, SK)
                if which == 1:
                    V(lambda e, dst=dst: e.tensor_scalar(out=dst, in0=uu, scalar1=sgn[:, 0:1], scalar2=None, op0=ALU.mult), SK, SK)
                else:
                    V(lambda e, dst=dst: e.tensor_copy(out=dst, in_=uu), SK, SK)
        p.barrier()

        def rstd_from(ssum_ap, n, out_ap, key):
            A(lambda e: e.activation(out=out_ap, in_=ssum_ap, func=AF.Sqrt, bias=EPS, scale=1.0 / n), [key], [key])
            V(lambda e: e.reciprocal(out=out_ap, in_=out_ap), [key], [key])

        def layernorm_block(i, src, gt, bt, keyg, tmp=None, tk="TA", pi=0):
            xk = ("X", i)
            if tmp is None:
                tmp = TA[:]
            so = 32 * pi
            sk = "stt%d" % pi
            k1, k2, k3 = "lnmv%d" % pi, "lnr%d" % pi, "lnn%d" % pi
            V(lambda e: e.bn_stats(out=stt[:, 2 * pi, :], in_=src[:, 0:512]), [xk], [sk])
            V(lambda e: e.bn_stats(out=stt[:, 2 * pi + 1, :], in_=src[:, 512:1024]), [xk], [sk])
            V(lambda e: e.bn_aggr(out=sm[:, so:so + 2], in_=stt[:, 2 * pi:2 * pi + 2, :]), [sk], [k1])
            A(lambda e: e.activation(out=sm[:, so + 2:so + 3], in_=sm[:, so + 1:so + 2], func=AF.Sqrt, bias=EPS, scale=1.0), [k1], [k2])
            V(lambda e: e.reciprocal(out=sm[:, so + 2:so + 3], in_=sm[:, so + 2:so + 3]), [k2], [k2])
            V(lambda e: e.tensor_scalar(out=sm[:, so + 3:so + 4], in0=sm[:, so:so + 1], scalar1=-1.0, scalar2=sm[:, so + 2:so + 3], op0=ALU.mult, op1=ALU.mult),
              [k1, k2], [k3])
            A(lambda e: e.activation(out=tmp, in_=src, func=AF.Identity, bias=sm[:, so + 3:so + 4], scale=sm[:, so + 2:so + 3]), [xk, k2, k3], [tk])
            V(lambda e: e.tensor_tensor(out=tmp, in0=tmp, in1=gt[:], op=ALU.mult), [tk, keyg], [tk])
            G(lambda e: e.tensor_tensor(out=X[:, i, :], in0=tmp, in1=bt[:], op=ALU.add), [tk, keyg], [xk])

        def transpose_block(i, dst, dkey, also_f32=None):
            xk = ("X", i)
            for kc in range(8):
                TR(bank(kc // 4, (kc % 4) * 128, (kc % 4 + 1) * 128), X[:, i, kc * 128:(kc + 1) * 128], ident_f[:],
                   [xk, "ident_f"], ["B%d" % (kc // 4)], sig=(kc % 4 == 3))
            A(lambda e: e.activation(out=dst, in_=ps[:, 0:1024].rearrange("p (c t) -> p c t", c=8), func=AF.Copy), ["B0", "B1"], [dkey])
            if also_f32 is not None:
                V(lambda e: e.tensor_copy(out=also_f32, in_=ps[:, 0:1024]), ["B0", "B1"], ["WB1"])

        idx_d = 0
        idx_m = 0
        for l, kind in enumerate(kinds):
            last = (l == nl - 1)
            WLAT = WB[0][:, 0:3328].rearrange("p (c f) -> p c f", c=8)
            WUQ = WB[0][:, 3328:4864].rearrange("p (c f) -> p c f", c=2)
            WUKV = WB[0][:, 4864:5888]
            WOM = WB[0][:, 5888:9984].rearrange("p (c f) -> p c f", c=4)
            WUV = WB[1][:, 0:8192].rearrange("p (c f) -> p c f", c=8)
            WOA = WB[1][:, 8192:12288].rearrange("p (c f) -> p c f", c=4)
            D("gpsimd", WLAT, w_in_d[l, :, 1024:1440].rearrange("(c p) f -> p c f", p=128), [], ["WB0"])
            for hh in range(2):
                D("gpsimd", WUV[:, hh * 4:(hh + 1) * 4, :], w_in_d[l, hh * 512:(hh + 1) * 512, 0:1024].rearrange("(c p) f -> p c f", p=128), [], ["WB1"])
            D("sync", OG[:], out_g_d[l].rearrange("(c p) -> p c", p=128), [], ["OG"], allow_slow_non_contiguous=True)
            D("sync", QG[:], q_norm_g_d[l].rearrange("(c p) -> p c", p=128), [], ["QG"], allow_slow_non_contiguous=True)
            D("sync", KG[:], kv_norm_g_d[l].rearrange("(c p) -> p c", p=128), [], ["KG"], allow_slow_non_contiguous=True)
            D("sync", LNG[:], ln1_g_d[l:l + 1, :].partition_broadcast(128), [], ["LN"])
            D("sync", LNB[:], ln1_b_d[l:l + 1, :].partition_broadcast(128), [], ["LN"])
            D("sync", SGG[:], sg_v_g_d[l:l + 1, :].partition_broadcast(128), [], ["SG"])
            D("sync", SGB[:], sg_v_b_d[l:l + 1, :].partition_broadcast(128), [], ["SG"])
            D("gpsimd", bsr[:], sg_b_s_d[l:l + 1, :], [], ["bsr"])
            D("gpsimd", bgr[:], ple_b_g_d[l:l + 1, :], [], ["bgr"])
            stg = [TA, TA]
            si = 0

            def fold(dst, src_ap, ncols, gcol, gkey, wkey):
                nonlocal si
                t_ = stg[si % 2]
                tk = "TA"
                si += 1
                D("sync", t_[:, 0:ncols], src_ap, [], [tk])
                V(lambda e: e.tensor_scalar(out=dst, in0=t_[:, 0:ncols], scalar1=gcol, scalar2=None, op0=ALU.mult), [tk, gkey], [wkey])

            for c in range(2):
                fold(WUQ[:, c, :], w_uq_d[l, c * 128:(c + 1) * 128, :], 768, QG[:, c:c + 1], "QG", "WB0")
            fold(WUKV, w_ukv_d[l], 1024, KG[:, 0:1], "KG", "WB0")
            for c in range(4):
                fold(WOA[:, c, :], w_o_d[l, c * 128:(c + 1) * 128, :], 1024, OG[:, c:c + 1], "OG", "WB1")
            for c in range(4):
                fold(WOM[:, c, :], w_o_d[l, 512 + c * 128:512 + (c + 1) * 128, :], 1024, OG[:, 4 + c:5 + c], "OG", "WB0")
            wsn = TA[:].rearrange("p (g s) -> p g s", g=8)
            D("sync", wsn, sg_w_s_d[l].rearrange("g t s -> t g s"), [], ["TA"])
            V(lambda e: e.tensor_tensor(out=wsn, in0=wsn, in1=tril_f[:].unsqueeze(1).to_broadcast([128, 8, 128]), op=ALU.mult), ["TA", "tril_f"], ["TA"])
            for g in range(8):
                TR(bank(g // 4, (g % 4) * 128, (g % 4 + 1) * 128), wsn[:, g, :], ident_f[:], ["TA", "ident_f"], ["B%d" % (g // 4)], sig=(g % 4 == 3))
            A(lambda e: e.activation(out=wsT[:].rearrange("p g t -> p (g t)"), in_=ps[:, 0:1024], func=AF.Copy), ["B0", "B1"], ["wsT"])
            V(lambda e: e.tensor_copy(out=WLsw[:, :, 0:16], in_=WLAT[:, :, 400:416]), ["WB0"], ["WLsw"])
            V(lambda e: e.tensor_copy(out=WLsw[:, :, 16:32], in_=WLAT[:, :, 384:400]), ["WB0"], ["WLsw"])

            TAp = [TA[:], R1[:, 0:2048].bitcast(F32)]
            xTp2 = [xTt0[:], R1[:, 2048:3072].rearrange("p (c t) -> p c t", c=8)]
            aTp = [aTt[:], R1[:, 3072:3584].rearrange("p (c t) -> p c t", c=4)]
            cqp = [cqn[:], R1[:, 3584:3968]]
            abp = [abf, R1[:, 8192:8704]]
            vnp = [vnb, R1[:, 8704:9216]]
            krp = [krt[:], R1[0:32, 9216:9728].bitcast(F32)]
            def a12_stage(i, which):
                xk = ("X", i)
                pi = i % 2
                so = 32 * pi
                xt = xTp2[pi]; xtk = ("xTt", pi)
                TA_ = TAp[pi]; tak = "TA" if pi == 0 else "TA1"
                cq_ = cqp[pi]; cqk = "cqn%d" % pi
                ab_ = abp[pi]; abk = "abf%d" % pi
                vn_ = vnp[pi]; vnk = "vnb%d" % pi
                aT_ = aTp[pi]; aTk = "aTt%d" % pi
                kr_ = krp[pi]
                ksq, kskv, ksa = "sq%d" % pi, "skv%d" % pi, "sa%d" % pi
                k1, k2, k3, skk = "lnmv%d" % pi, "lnr%d" % pi, "lnn%d" % pi, "stt%d" % pi
                if which == 1:
                    transpose_block(i, xt, xtk)
                    A(lambda e, i=i: e.activation(out=X[:, i, :], in_=X[:, i, :], func=AF.Copy, scale=ALPHA), [xk], [xk])
                    for kc in range(8):
                        MM(bank(2, 0, 384), xt[:, kc, :], WLAT[:, kc, 0:384], kc == 0, kc == 7, [xtk, "WB0"], ["B2"])
                    for kc in range(8):
                        MM(bank(3, 0, 128, 0, 32), WLAT[:, kc, 384:416], xt[:, kc, :], kc == 0, kc == 7, [xtk, "WB0"], ["B3"], sig=False)
                    for kc in range(8):
                        MM(bank(3, 128, 256, 0, 32), WLsw[:, kc, :], xt[:, kc, :], kc == 0, kc == 7, [xtk, "WLsw"], ["B3"])
                    for hh in range(2):
                        for kc in range(8):
                            MM(bank(5 + hh), xt[:, kc, :], WUV[:, kc, hh * 512:(hh + 1) * 512], kc == 0, kc == 7, [xtk, "WB1"], ["B%d" % (5 + hh)])
                elif which == 2:
                    A(lambda e, so=so: e.activation(out=junk[:, 0:256], in_=bank(2, 0, 256), func=AF.Square, accum_out=sm[:, so + 8:so + 9]), ["B2"], ["junk", ksq])
                    A(lambda e, so=so: e.activation(out=junk[:, 0:128], in_=bank(2, 256, 384), func=AF.Square, accum_out=sm[:, so + 9:so + 10]), ["B2"], ["junk", kskv])
                    rstd_from(sm[:, so + 8:so + 9], 256.0, sm[:, so + 8:so + 9], ksq)
                    rstd_from(sm[:, so + 9:so + 10], 128.0, sm[:, so + 9:so + 10], kskv)
                    V(lambda e, so=so, cq_=cq_: e.tensor_scalar(out=cq_[:, 0:256], in0=bank(2, 0, 256), scalar1=sm[:, so + 8:so + 9], scalar2=None, op0=ALU.mult), ["B2", ksq], [cqk])
                    V(lambda e, so=so, cq_=cq_: e.tensor_scalar(out=cq_[:, 256:384], in0=bank(2, 256, 384), scalar1=sm[:, so + 9:so + 10], scalar2=None, op0=ALU.mult), ["B2", kskv], [cqk])
                    for c in range(3):
                        TR(bankb(4, c * 128, (c + 1) * 128), cq_[:, c * 128:(c + 1) * 128], ident_b[:], [cqk, "ident_b"], ["B4"], sig=(c == 2))
                    A(lambda e, i=i: e.activation(out=CQT[:, :, i * 128:(i + 1) * 128], in_=bankb(4, 0, 256).rearrange("p (c t) -> p c t", c=2), func=AF.Copy),
                      ["B4"], [("CQT", i)])
                    V(lambda e, i=i: e.tensor_copy(out=LAT[:, i * 128:(i + 1) * 128], in_=bankb(4, 256, 384)), ["B4"], ["LAT"])
                    V(lambda e, i=i, kr_=kr_: e.tensor_tensor(out=kr_[:, 0:128], in0=bank(3, 0, 128, 0, 32), in1=CS[:, 0, i * 128:(i + 1) * 128], op=ALU.mult),
                      ["B3"], ["krt0_%d" % pi])
                    V(lambda e, i=i, kr_=kr_: e.tensor_tensor(out=kr_[:, 128:256], in0=bank(3, 128, 256, 0, 32), in1=CS[:, 1, i * 128:(i + 1) * 128], op=ALU.mult),
                      ["B3"], ["krt1_%d" % pi])
                    V(lambda e, i=i, kr_=kr_: e.tensor_tensor(out=KRl[:, i * 128:(i + 1) * 128], in0=kr_[:, 0:128], in1=kr_[:, 128:256], op=ALU.add),
                      ["krt0_%d" % pi, "krt1_%d" % pi], ["KRl"])
                    A(lambda e, TA_=TA_: e.activation(out=TA_, in_=ps[:, 5 * 512:7 * 512], func=AF.Gelu), ["B5", "B6"], [tak])
                elif which == 3:
                    V(lambda e, TA_=TA_, pi=pi: e.bn_stats(out=stt[:, 2 * pi, :], in_=TA_[:, 512:1024]), [tak], [skk])
                    V(lambda e, so=so, pi=pi: e.bn_aggr(out=sm[:, so:so + 2], in_=stt[:, 2 * pi:2 * pi + 1, :]), [skk], [k1])
                    A(lambda e, so=so: e.activation(out=sm[:, so + 2:so + 3], in_=sm[:, so + 1:so + 2], func=AF.Sqrt, bias=EPS, scale=1.0), [k1], [k2])
                    V(lambda e, so=so: e.reciprocal(out=sm[:, so + 2:so + 3], in_=sm[:, so + 2:so + 3]), [k2], [k2])
                    V(lambda e, so=so: e.tensor_scalar(out=sm[:, so + 3:so + 4], in0=sm[:, so:so + 1], scalar1=-1.0, scalar2=sm[:, so + 2:so + 3], op0=ALU.mult, op1=ALU.mult), [k1, k2], [k3])
                    A(lambda e, so=so, TA_=TA_: e.activation(out=TA_[:, 512:1024], in_=TA_[:, 512:1024], func=AF.Identity, bias=sm[:, so + 3:so + 4], scale=sm[:, so + 2:so + 3]), [tak, k2, k3], [tak])
                    V(lambda e, TA_=TA_: e.tensor_tensor(out=TA_[:, 512:1024], in0=TA_[:, 512:1024], in1=SGG[:], op=ALU.mult), [tak, "SG"], [tak])
                    V(lambda e, TA_=TA_, vn_=vn_: e.tensor_tensor(out=vn_[:], in0=TA_[:, 512:1024], in1=SGB[:], op=ALU.add), [tak, "SG"], [vnk])
                elif which == 4:
                    for g in range(8):
                        MM(bank(7, g * 64, (g + 1) * 64), wsT[:, g, :], vn_[:, g * 64:(g + 1) * 64], True, False, ["wsT", vnk], ["B7"], sig=False)
                        MM(bank(7, g * 64, (g + 1) * 64), bsr[0:1, g * 128:(g + 1) * 128], ones_b[0:1, 0:64], False, True, ["bsr", "ones_b"], ["B7"], sig=(g == 7))
                    V(lambda e, TA_=TA_, ab_=ab_: e.tensor_tensor(out=ab_[:], in0=TA_[:, 0:512], in1=bank(7), op=ALU.mult), [tak, "B7"], [abk])
                    A(lambda e, so=so, ab_=ab_: e.activation(out=junk[:], in_=ab_[:], func=AF.Square, accum_out=sm[:, so + 10:so + 11]), [abk], ["junk", ksa])
                    rstd_from(sm[:, so + 10:so + 11], 512.0, sm[:, so + 10:so + 11], ksa)
                    for c in range(4):
                        TR(bankb(4, 512 + c * 128, 512 + (c + 1) * 128), ab_[:, c * 128:(c + 1) * 128], ident_b[:], [abk, "ident_b"], ["B4b"], sig=(c == 3))
                    A(lambda e, aT_=aT_: e.activation(out=aT_, in_=bankb(4, 512, 1024).rearrange("p (c t) -> p c t", c=4), func=AF.Copy), ["B4b"], [aTk])
                    for hh in range(2):
                        for c in range(4):
                            MM(bank(5 + hh), aT_[:, c, :], WOA[:, c, hh * 512:(hh + 1) * 512], c == 0, c == 3, [aTk, "WB1"], ["B%d" % (5 + hh)])
                    V(lambda e, i=i, so=so: e.scalar_tensor_tensor(out=X[:, i, :], in0=ps[:, 5 * 512:7 * 512], scalar=sm[:, so + 10:so + 11], in1=X[:, i, :], op0=ALU.mult, op1=ALU.add),
                      ["B5", "B6", ksa, xk], [xk])


            for it in range(NB + 1):
                if it < NB:
                    a12_stage(it, 1)
                if it >= 1:
                    a12_stage(it - 1, 3)
                if it < NB:
                    a12_stage(it, 2)
                if it >= 1:
                    a12_stage(it - 1, 4)

            D("sync", ag_in[l][0:128, :], LAT, ["LAT"], ["ag_in"])
            D("sync", ag_in[l][128:160, :], KRl, ["KRl"], ["ag_in"])
            p.dma("gpsimd", lambda e, l=l: e.collective_compute("AllGather", ALU.bypass, replica_groups=[[0, 1], [2, 3], [4, 5], [6, 7]],
                                                                ins=[ag_in[l].opt()], outs=[ag_out[l].opt()]), r=["ag_in"], w=["ag_out"], inc=1)
            p.barrier()
            for hk in range(2):
                D("sync", CKV[:, hk * T:(hk + 1) * T], ag_out[l][hk * 160:hk * 160 + 128, :], ["ag_out"], [("CKV", hk)])
                D("sync", KT[0:32, hk * T:(hk + 1) * T], ag_out[l][hk * 160 + 128:hk * 160 + 160, :], ["ag_out"], [("KTr", hk)])
            p.barrier()
            G(lambda e: e.memset(KT[32:64, :], 0.0), [], ["KTc"])
            G(lambda e: e.memset(KT[32:33, :], 1.0), [], ["KTc"])
            for gq in range(2):
                G(lambda e, gq=gq: e.memset(QTg[gq][32:64, :], 0.0), [], [("QT", gq)])
            p.barrier()
            ckv_keys = [("CKV", 0), ("CKV", 1)]
            ktr_keys = [("KTr", 0), ("KTr", 1), "KTc"]

            def prologue(h):
                par = h % 2
                V(lambda e: e.tensor_copy(out=WKpad[:, 64:128], in_=WUKV[:, h * 128:h * 128 + 64]), ["WB0"], ["WKpad"])
                V(lambda e: e.tensor_copy(out=WQpad[:, :, 0:32], in_=WUQ[:, :, h * 96 + 64:h * 96 + 96]), ["WB0"], ["WQpad"])
                V(lambda e: e.tensor_copy(out=WQpad[:, :, 64:128], in_=WUQ[:, :, h * 96:h * 96 + 64]), ["WB0"], ["WQpad"])
                V(lambda e: e.tensor_copy(out=WQsw[:, :, 0:16], in_=WUQ[:, :, h * 96 + 80:h * 96 + 96]), ["WB0"], ["WQsw"])
                V(lambda e: e.tensor_copy(out=WQsw[:, :, 16:32], in_=WUQ[:, :, h * 96 + 64:h * 96 + 80]), ["WB0"], ["WQsw"])
                for c in range(8):
                    b_ = c % 2
                    MM(bank(b_), WKpad[:], CKV[:, c * 512:(c + 1) * 512], True, True, ["WKpad", ("CKV", c // 4)], ["B%d" % b_])
                    if c % 2 == 0:
                        A(lambda e, c=c, b_=b_: e.activation(out=KT[64:128, c * 512:(c + 1) * 512], in_=bank(b_, 0, 512, 64, 128), func=AF.Copy), ["B%d" % b_], [("KTn", c)])
                    else:
                        V(lambda e, c=c, b_=b_: e.tensor_copy(out=KT[64:128, c * 512:(c + 1) * 512], in_=bank(b_, 0, 512, 64, 128)), ["B%d" % b_], [("KTn", c)])
                voff = 0 if par == 0 else 64
                onescol = 64 if par == 0 else 0
                for q4 in range(4):
                    b_ = q4 % 2
                    for kk in range(8):
                        kb = q4 * 8 + kk
                        MM(bank(b_, kk * 64, (kk + 1) * 64), CKV[:, kb * 128:(kb + 1) * 128], WUKV[:, h * 128 + 64:h * 128 + 128], True, True,
                           [("CKV", kb // 16), "WB0"], ["B%d" % b_], sig=(kk == 7))
                    if q4 % 2 == 0:
                        A(lambda e, q4=q4, b_=b_: e.activation(out=VA[:, q4 * 8:(q4 + 1) * 8, voff:voff + 64],
                                                               in_=bank(b_).rearrange("p (k c) -> p k c", k=8), func=AF.Copy), ["B%d" % b_], ["VA"])
                    else:
                        V(lambda e, q4=q4, b_=b_: e.tensor_copy(out=VA[:, q4 * 8:(q4 + 1) * 8, voff:voff + 64],
                                                                in_=bank(b_).rearrange("p (k c) -> p k c", k=8)), ["B%d" % b_], ["VA"])
                G(lambda e: e.memset(VA[:, :, onescol:onescol + 1], 1.0), ["VA"], ["VA"])

            ktn_keys = [("KTn", c) for c in range(8)]
            kall = ktr_keys + ktn_keys

            def qbuild(h, Gq, u):
                qbuild_a(h, Gq, u)
                qbuild_b(h, Gq, u)

            def qbuild_a(h, Gq, u):
                qb = u % 2
                QT_ = QTg[qb]
                qk = ("QT", qb)
                cqk = [("CQT", Gq * 4 + t_) for t_ in range(4)]
                for c in range(2):
                    MM(bank(0), WQpad[:, c, :], CQT[:, c, Gq * 512:(Gq + 1) * 512], c == 0, c == 1, ["WQpad"] + cqk, ["B0"])
                for c in range(2):
                    MM(bank(1, 0, 512, 0, 32), WQsw[:, c, :], CQT[:, c, Gq * 512:(Gq + 1) * 512], c == 0, c == 1, ["WQsw"] + cqk, ["B1"])
                V(lambda e: e.tensor_copy(out=QT_[64:128, :], in_=bank(0, 0, 512, 64, 128)), ["B0"], [qk])
                G(lambda e: e.memset(QT_[32:33, :], 0.0), [], [qk])
                V(lambda e: e.tensor_tensor(out=TA[0:32, 0:512], in0=bank(0, 0, 512, 0, 32), in1=CS[:, 0, Gq * 512:(Gq + 1) * 512], op=ALU.mult), ["B0"], ["TA"])
                V(lambda e: e.tensor_tensor(out=TA[0:32, 512:1024], in0=bank(1, 0, 512, 0, 32), in1=CS[:, 1, Gq * 512:(Gq + 1) * 512], op=ALU.mult), ["B1"], ["TA"])
                V(lambda e: e.tensor_tensor(out=QT_[0:32, :], in0=TA[0:32, 0:512], in1=TA[0:32, 512:1024], op=ALU.add), ["TA"], [qk])
                V(lambda e: e.tensor_tensor(out=abf[:], in0=QT_[:, :], in1=KT[:, Gq * 512:(Gq + 1) * 512], op=ALU.mult), [qk] + kall, ["abf"])
                G(lambda e: e.tensor_tensor(out=vnb[:], in0=QT_[:, :], in1=KT[:, T + Gq * 512:T + (Gq + 1) * 512], op=ALU.mult), [qk] + kall, ["vnb"])

            def qbuild_b(h, Gq, u):
                qb = u % 2
                QT_ = QTg[qb]
                qk = ("QT", qb)
                MM(bank(2, 0, 512, 0, 33), NEGS[:, 0:33], abf[:], True, False, ["NEGS", "abf"], ["B2"], sig=False)
                MM(bank(2, 0, 512, 0, 33), NEGS[:, 33:66], vnb[:], False, True, ["NEGS", "vnb"], ["B2"], sig=True)
                A(lambda e: e.activation(out=QT_[32:33, :], in_=bank(2, 0, 512, 32, 33), func=AF.Copy), ["B2"], [qk])

            def tail(h, Gq, u):
                tail_a(h, Gq, u)
                tail_b(h, Gq, u)

            def tail_a(h, Gq, u):
                par = h % 2
                bo = 7 if u % 2 == 0 else 3
                lrow = 64 if par == 0 else 0
                V(lambda e: e.reciprocal(out=rl[lrow:lrow + 1, :], in_=bank(bo, 0, 512, lrow, lrow + 1)), ["B%d" % bo], ["WB1"])

            def tail_b(h, Gq, u):
                par = h % 2
                bo = 7 if u % 2 == 0 else 3
                bok = "B%d" % bo
                Mv = 65 if par == 0 else 128
                orow0 = 0 if par == 0 else 64
                lrow = 64 if par == 0 else 0
                MM(bank(2, 0, 512, 0, Mv if par else 64), ones_f[lrow:lrow + 1, 0:(128 if par else 64)], rl[lrow:lrow + 1, :], True, True, ["WB1", "ones_f"], ["B2"])
                V(lambda e: e.tensor_copy(out=bcs[orow0:orow0 + 64, :], in_=bank(2, 0, 512, orow0, orow0 + 64)), ["B2"], ["WB1"])
                V(lambda e: e.tensor_tensor(out=MT[orow0:orow0 + 64, h // 2, Gq * 512:(Gq + 1) * 512],
                                            in0=bank(bo, 0, 512, orow0, orow0 + 64), in1=bcs[orow0:orow0 + 64, :], op=ALU.mult),
                  [bok, "WB1"], [("MT", Gq)])
                V(lambda e: e.tensor_tensor(out=sqb[orow0:orow0 + 64, :], in0=MT[orow0:orow0 + 64, h // 2, Gq * 512:(Gq + 1) * 512],
                                            in1=MT[orow0:orow0 + 64, h // 2, Gq * 512:(Gq + 1) * 512], op=ALU.mult),
                  [("MT", Gq)], ["sqb"])
                for tb in range(4):
                    col = Gq * 4 + tb
                    MM(bank(4, h * 16 + col, h * 16 + col + 1), sqb[orow0:orow0 + 64, tb * 128:(tb + 1) * 128], ones_b[orow0:orow0 + 64, 0:1], True, True,
                       ["sqb", "ones_b"], ["B4s"], sig=(tb == 3))

            def pass2(h, Gq, u, hooks):
                par = h % 2
                qb = u % 2
                QT_ = QTg[qb]
                qk = ("QT", qb)
                bo = 7 if u % 2 == 0 else 3
                bok = "B%d" % bo
                Mv = 65 if par == 0 else 128
                kbl = []
                for hk in range(2):
                    for jb in range(4 * Gq):
                        kbl.append((hk * 16 + jb, 0, None))
                for hk in range(2):
                    for d_ in range(2):
                        kbl.append((hk * 16 + 4 * Gq + d_, 0, (0, hk * 2 + d_)))
                for hk in range(2):
                    for d_ in range(2):
                        kbl.append((hk * 16 + 4 * Gq + 2 + d_, 256, (256, hk * 2 + d_)))
                nk = len(kbl)

                def issue_S(ki):
                    kb, qlo, msk = kbl[ki]
                    b_ = 5 + (ki % 2)
                    MM(bank(b_, qlo, 512), KT[:, kb * 128:(kb + 1) * 128], QT_[:, qlo:512], True, msk is None, [qk] + kall, ["B%d" % b_])
                    if msk is not None:
                        mlo, kbq = msk
                        MM(bank(b_, mlo, mlo + 256), ident_b[:], maskT[:, kbq, :], False, True, ["ident_b", "maskT"], ["B%d" % b_])

                issue_S(0)
                issue_S(1)
                for ki, (kb, qlo, msk) in enumerate(kbl):
                    b_ = 5 + (ki % 2)
                    pt = PT[ki % 3]
                    ptk = ("PT", ki % 3)
                    A(lambda e, b_=b_, qlo=qlo, pt=pt: e.activation(out=pt[:, qlo:512], in_=bank(b_, qlo, 512), func=AF.Exp, scale=ATT_SCALE), ["B%d" % b_], [ptk])
                    if ki + 2 < nk:
                        issue_S(ki + 2)
                    if qlo == 0:
                        MM(bank(bo, 0, 512, 0, Mv), VA[:, kb, 0:Mv], pt[:, 0:512], ki == 0, ki == nk - 1, ["VA", ptk], [bok], sig=True)
                    else:
                        MM(bank(bo, 256, 512, 0, Mv), VA[:, kb, 0:Mv], pt[:, 256:512], False, ki == nk - 1, ["VA", ptk], [bok], sig=True)
                    for fn in hooks.get(ki, ()):
                        fn()

            units = [(h, Gq) for h in range(8) for Gq in range(4)]
            prev = None
            for u, (h, Gq) in enumerate(units):
                hooks = {}
                if Gq == 0:
                    prologue(h)
                    qbuild(h, 0, u)
                if Gq < 3:
                    hooks.setdefault(0, []).append(lambda h=h, Gq=Gq, u=u: qbuild_a(h, Gq + 1, u + 1))
                    hooks.setdefault(4, []).append(lambda h=h, Gq=Gq, u=u: qbuild_b(h, Gq + 1, u + 1))
                if prev is not None:
                    ph, pg, pu = prev
                    hooks.setdefault(1, []).append(lambda ph=ph, pg=pg, pu=pu: tail_a(ph, pg, pu))
                    hooks.setdefault(6, []).append(lambda ph=ph, pg=pg, pu=pu: tail_b(ph, pg, pu))
                pass2(h, Gq, u, hooks)
                prev = (h, Gq, u)
            tail(*prev)
            V(lambda e: e.reduce_sum(out=ssq[:], in_=bank(4, 0, 128).rearrange("p (h c) -> p c h", h=8), axis=AX.X), ["B4s"], ["ssq"])
            p.barrier()
            if dbg_d is not None:
                D("sync", dbg_d, R2[:, :], [], ["dbg"])
                p.barrier()

            if kind == "m":
                D("sync", wr_f[:], moe_w_r_d[idx_m].rearrange("(c p) e -> p c e", p=128), [], ["wr_f"])
                V(lambda e: e.tensor_copy(out=wr_h[:], in_=wr_f[:]), ["wr_f"], ["wr_h"])
                V(lambda e: e.tensor_tensor(out=wr_l[:], in0=wr_f[:], in1=wr_h[:], op=ALU.subtract), ["wr_f", "wr_h"], ["wr_l"])
            LN4t = [TA[:], R1[:, 16384:18432].bitcast(F32)]
            LN4k = ["TA", "TA1"]
            xlop = [xTt0[:], R1[:, 18432:19456].rearrange("p (c t) -> p c t", c=8)]
            def a4_stage(i, which):
                xk = ("X", i)
                pi = i % 2
                y0 = 2 if pi == 0 else 6
                smc = 11 + 32 * pi
                smk = "sm_%d" % pi
                if which == 1:
                    A(lambda e, i=i, smc=smc: e.activation(out=sm[:, smc:smc + 1], in_=ssq[:, i:i + 1], func=AF.Sqrt, bias=EPS, scale=1.0 / 512.0), ["ssq"], [smk])
                    V(lambda e, smc=smc: e.reciprocal(out=sm[:, smc:smc + 1], in_=sm[:, smc:smc + 1]), [smk], [smk])
                    for hh in range(2):
                        for c in range(4):
                            MM(bank(y0 + hh), MT[:, c, i * 128:(i + 1) * 128], WOM[:, c, hh * 512:(hh + 1) * 512], c == 0, c == 3, [("MT", i // 4), "WB0"], ["B%d" % (y0 + hh)])
                    if stop_after != "mixer_a":
                        V(lambda e, i=i, y0=y0, smc=smc: e.scalar_tensor_tensor(out=X[:, i, :], in0=ps[:, y0 * 512:(y0 + 2) * 512], scalar=sm[:, smc:smc + 1], in1=X[:, i, :], op0=ALU.mult, op1=ALU.add),
                          ["B%d" % y0, "B%d" % (y0 + 1), smk, xk], [xk])
                elif which == 2:
                    layernorm_block(i, X[:, i, :], LNG, LNB, "LN", tmp=LN4t[pi], tk=LN4k[pi], pi=pi)
                else:
                    transpose_block(i, BT[:, :, i * 128:(i + 1) * 128], ("BT", i), also_f32=None)
                    A(lambda e, i=i: e.activation(out=X[:, i, :], in_=X[:, i, :], func=AF.Copy, scale=ALPHA), [xk], [xk])
                    if kind == "m":
                        xlo = xlop[pi]
                        V(lambda e, i=i, xlo=xlo: e.tensor_tensor(out=xlo, in0=ps[:, 0:1024].rearrange("p (c t) -> p c t", c=8), in1=BT[:, :, i * 128:(i + 1) * 128], op=ALU.subtract),
                          ["B0", "B1", ("BT", i)], [("xTt", pi)])
                        for pi_, (lt, lk, wt, wk) in enumerate([("hi", None, wr_h, "wr_h"), ("lo", None, wr_h, "wr_h"), ("hi", None, wr_l, "wr_l")]):
                            for kc in range(8):
                                lhs = BT[:, kc, i * 128:(i + 1) * 128] if lt == "hi" else xlo[:, kc, :]
                                lkey = ("BT", i) if lt == "hi" else ("xTt", pi)
                                MM(bank(4, 0, 8), lhs, wt[:, kc, :], (pi_ == 0 and kc == 0), (pi_ == 2 and kc == 7), [lkey, wk], ["B4"])
                        V(lambda e: e.tensor_copy(out=lgt[:, 0, :], in_=bank(4, 0, 8)), ["B4"], ["lg0"])
                        V(lambda e: e.reduce_max(out=sm[:, 20:21], in_=lgt[:, 0, :], axis=AX.X), ["lg0"], ["m1"])
                        V(lambda e: e.tensor_scalar(out=lgt[:, 1, :], in0=lgt[:, 0, :], scalar1=sm[:, 20:21], scalar2=None, op0=ALU.is_ge), ["lg0", "m1"], ["lg1"])
                        V(lambda e: e.scalar_tensor_tensor(out=lgt[:, 2, :], in0=lgt[:, 1, :], scalar=-1e30, in1=lgt[:, 0, :], op0=ALU.mult, op1=ALU.add), ["lg1", "lg0"], ["lg2"])
                        V(lambda e: e.reduce_max(out=sm[:, 21:22], in_=lgt[:, 2, :], axis=AX.X), ["lg2"], ["m2"])
                        V(lambda e: e.tensor_scalar(out=lgt[:, 1, :], in0=lgt[:, 0, :], scalar1=sm[:, 21:22], scalar2=None, op0=ALU.is_ge), ["lg0", "m2", "lg2"], ["lg1b"])
                        V(lambda e: e.tensor_scalar(out=sm[:, 22:23], in0=sm[:, 20:21], scalar1=-1.0, scalar2=None, op0=ALU.mult), ["m1"], ["nm1"])
                        A(lambda e: e.activation(out=lgt[:, 3, :], in_=lgt[:, 0, :], func=AF.Exp, bias=sm[:, 22:23], scale=1.0), ["lg0", "nm1"], ["lg3"])
                        V(lambda e: e.tensor_tensor(out=lgt[:, 3, :], in0=lgt[:, 3, :], in1=lgt[:, 1, :], op=ALU.mult), ["lg3", "lg1b"], ["lg3b"])
                        V(lambda e: e.reduce_sum(out=sm[:, 23:24], in_=lgt[:, 3, :], axis=AX.X), ["lg3b"], ["sw"])
                        V(lambda e: e.reciprocal(out=sm[:, 23:24], in_=sm[:, 23:24]), ["sw"], ["sw"])
                        V(lambda e, i=i: e.tensor_scalar(out=comb[:, i, :], in0=lgt[:, 3, :], scalar1=sm[:, 23:24], scalar2=None, op0=ALU.mult), ["lg3b", "sw"], [("comb", i)])


            if stop_after in ("mixer_a", "mixer_raw"):
                for i in range(NB):
                    a4_stage(i, 1)
            elif stop_after == "mixer":
                for i in range(NB):
                    a4_stage(i, 1)
                    a4_stage(i, 2)
            else:
                for it in range(NB + 1):
                    if it < NB:
                        a4_stage(it, 1)
                    if it >= 1:
                        a4_stage(it - 1, 2)
                        a4_stage(it - 1, 3)

            if stop_after not in ("mixer", "mixer_a", "mixer_raw"):
                p.barrier()
                stages = []
                if kind == "d":
                    f0 = 0
                    while f0 < 2816:
                        fw = min(512, 2816 - f0)
                        stages.append((None, f0, fw))
                        f0 += fw
                else:
                    for e_ in range(8):
                        for f0 in range(0, 3584, 512):
                            stages.append((e_, f0, 512))

                def load_stage(si_):
                    e_, f0, fw = stages[si_]
                    slot = WB[si_ % 2]
                    sk = "WB%d" % (si_ % 2)
                    W1s = slot[:, 0:4096].rearrange("p (c f) -> p c f", c=8)
                    W3s = slot[:, 4096:8192].rearrange("p (c f) -> p c f", c=8)
                    W2s = slot[:, 8192:12288].rearrange("p (c f) -> p c f", c=4)
                    if e_ is None:
                        s1 = ffn_w1_d[idx_d]; s3 = ffn_w3_d[idx_d]; s2 = ffn_w2_d[idx_d]
                    else:
                        s1 = moe_w1_d[idx_m, e_]; s3 = moe_w3_d[idx_m, e_]; s2 = moe_w2_d[idx_m, e_]
                    for hh in range(2):
                        D("gpsimd", W1s[:, hh * 4:(hh + 1) * 4, 0:fw], s1[hh * 512:(hh + 1) * 512, f0:f0 + fw].rearrange("(c p) f -> p c f", p=128), [], [sk])
                        D("gpsimd", W3s[:, hh * 4:(hh + 1) * 4, 0:fw], s3[hh * 512:(hh + 1) * 512, f0:f0 + fw].rearrange("(c p) f -> p c f", p=128), [], [sk])
                    D("gpsimd", W2s[:, 0:fw // 128, :], s2[f0:f0 + fw, :].rearrange("(c p) d -> p c d", p=128), [], [sk])

                load_stage(0)
                for si_ in range(len(stages)):
                    if si_ + 1 < len(stages):
                        load_stage(si_ + 1)
                    if si_ == len(stages) - 1:
                        assert si_ % 2 == 1
                        D("sync", LNG[:], ln2_g_d[l:l + 1, :].partition_broadcast(128), [], ["LN"])
                        D("sync", LNB[:], ln2_b_d[l:l + 1, :].partition_broadcast(128), [], ["LN"])
                        WGp = WB[0][:, 0:8192].rearrange("p (c f) -> p c f", c=8)
                        WPp = WB[0][:, 8192:10240].rearrange("p (c f) -> p c f", c=2)
                        for hh in range(2):
                            D("gpsimd", WGp[:, hh * 4:(hh + 1) * 4, :], ple_w_g_d[l, hh * 512:(hh + 1) * 512, :].rearrange("(c p) f -> p c f", p=128), [], ["WB0"])
                        D("gpsimd", WPp, ple_w_p_d[l].rearrange("(c p) f -> p c f", p=128), [], ["WB0"])
                    e_, f0, fw = stages[si_]
                    nfc = fw // 128
                    slot = WB[si_ % 2]
                    sk = "WB%d" % (si_ % 2)
                    W1s = slot[:, 0:4096].rearrange("p (c f) -> p c f", c=8)
                    W3s = slot[:, 4096:8192].rearrange("p (c f) -> p c f", c=8)
                    W2s = slot[:, 8192:12288].rearrange("p (c f) -> p c f", c=4)
                    def ffn_up(tg):
                        btk = [("BT", tg * 4 + t_) for t_ in range(4)]
                        for fc in range(nfc):
                            u_ = (tg * nfc + fc) % 2
                            b1, b3 = 2 * u_, 2 * u_ + 1
                            for kc in range(8):
                                MM(bank(b1), W1s[:, kc, fc * 128:(fc + 1) * 128], BT[:, kc, tg * 512:(tg + 1) * 512], kc == 0, kc == 7, [sk] + btk, ["B%d" % b1])
                            for kc in range(8):
                                MM(bank(b3), W3s[:, kc, fc * 128:(fc + 1) * 128], BT[:, kc, tg * 512:(tg + 1) * 512], kc == 0, kc == 7, [sk] + btk, ["B%d" % b3])
                            stl = silu_t[u_]
                            A(lambda e, stl=stl, b1=b1: e.activation(out=stl[:], in_=bank(b1), func=AF.Silu), ["B%d" % b1], [("silu", u_)])
                            V(lambda e, stl=stl, b3=b3, fc=fc, tg=tg: e.tensor_tensor(out=GT[:, fc, tg * 512:(tg + 1) * 512], in0=stl[:], in1=bank(b3), op=ALU.mult),
                              [("silu", u_), "B%d" % b3], [("GT", tg)])

                    def ffn_down(tg):
                        for t_ in range(4):
                            tb = tg * 4 + t_
                            bo = 4 + 2 * (tb % 2)
                            for hh in range(2):
                                for fc in range(nfc):
                                    MM(bank(bo + hh), GT[:, fc, tb * 128:(tb + 1) * 128], W2s[:, fc, hh * 512:(hh + 1) * 512], fc == 0, fc == nfc - 1,
                                       [("GT", tg), sk], ["B%d" % (bo + hh)])
                            xk = ("X", tb)
                            if e_ is None:
                                V(lambda e, tb=tb, bo=bo: e.tensor_tensor(out=X[:, tb, :], in0=X[:, tb, :], in1=ps[:, bo * 512:(bo + 2) * 512], op=ALU.add),
                                  [xk, "B%d" % bo, "B%d" % (bo + 1)], [xk])
                            else:
                                V(lambda e, tb=tb, bo=bo, e_=e_: e.scalar_tensor_tensor(out=X[:, tb, :], in0=ps[:, bo * 512:(bo + 2) * 512], scalar=comb[:, tb, e_:e_ + 1],
                                                                                        in1=X[:, tb, :], op0=ALU.mult, op1=ALU.add),
                                  [xk, "B%d" % bo, "B%d" % (bo + 1), ("comb", tb)], [xk])

                    ffn_up(0)
                    for tg in range(4):
                        if tg + 1 < 4:
                            ffn_up(tg + 1)
                        ffn_down(tg)
                p.barrier()

                WG = WB[0][:, 0:8192].rearrange("p (c f) -> p c f", c=8)
                WP = WB[0][:, 8192:10240].rearrange("p (c f) -> p c f", c=2)
                LNt = [TA[:], R1[:, 0:2048].bitcast(F32)]
                LNk = ["TA", "TA1"]
                GTt = [R1[:, 2048:4096].bitcast(F32), R1[:, 4096:6144].bitcast(F32)]
                xTp = [xTt0[:], R1[:, 6144:7168].rearrange("p (c t) -> p c t", c=8)]
                pbp = [pblk[:], R1[:, 7168:7424]]
                pTp = [pTt[:], R1[:, 7424:7680].rearrange("p (c t) -> p c t", c=2)]
                def ple_stage(i, which):
                    xk = ("X", i)
                    pi = i % 2
                    xt = xTp[pi]
                    xtk = ("xTt", pi)
                    gt_ = GTt[pi]
                    gk = "GTt%d" % pi
                    pb_ = pbp[pi]
                    pT_ = pTp[pi]
                    g0 = 2 if pi == 0 else 6
                    if which == 1:
                        layernorm_block(i, X[:, i, :], LNG, LNB, "LN", tmp=LNt[pi], tk=LNk[pi], pi=pi)
                        D("gpsimd", pb_, p_d[l, i], [], ["pblk%d" % pi])
                    elif which == 2:
                        transpose_block(i, xt, xtk)
                        for c in range(2):
                            TR(bankb(4, c * 128, (c + 1) * 128), pb_[:, c * 128:(c + 1) * 128], ident_b[:], ["pblk%d" % pi, "ident_b"], ["B4"], sig=(c == 1))
                        V(lambda e, pT_=pT_: e.tensor_copy(out=pT_, in_=bankb(4, 0, 256).rearrange("p (c t) -> p c t", c=2)), ["B4"], ["pTt%d" % pi])
                        for hh in range(2):
                            for kc in range(8):
                                MM(bank(g0 + hh), xt[:, kc, :], WG[:, kc, hh * 512:(hh + 1) * 512], kc == 0, False, [xtk, "WB0"], ["B%d" % (g0 + hh)], sig=False)
                            MM(bank(g0 + hh), ones_b[0:1, 0:128], bgr[0:1, hh * 512:(hh + 1) * 512], False, True, ["ones_b", "bgr"], ["B%d" % (g0 + hh)], sig=True)
                        for hh in range(2):
                            for c in range(2):
                                MM(bank(4 + hh), pT_[:, c, :], WP[:, c, hh * 512:(hh + 1) * 512], c == 0, c == 1, ["pTt%d" % pi, "WB0"], ["B%d" % (4 + hh)])
                    else:
                        A(lambda e, gt_=gt_, g0=g0: e.activation(out=gt_, in_=ps[:, g0 * 512:(g0 + 2) * 512], func=AF.Sigmoid), ["B%d" % g0, "B%d" % (g0 + 1)], [gk])
                        V(lambda e, gt_=gt_: e.tensor_tensor(out=gt_, in0=gt_, in1=ps[:, 4 * 512:6 * 512], op=ALU.mult), [gk, "B4", "B5"], [gk])
                        G(lambda e, i=i, gt_=gt_: e.tensor_tensor(out=X[:, i, :], in0=X[:, i, :], in1=gt_, op=ALU.add), [gk, xk], [xk])


                if stop_after == "ffn":
                    for i in range(NB):
                        layernorm_block(i, X[:, i, :], LNG, LNB, "LN", tmp=LNt[i % 2], tk=LNk[i % 2], pi=i % 2)
                else:
                    for it in range(NB + 1):
                        if it < NB:
                            ple_stage(it, 1)
                        if it >= 1:
                            ple_stage(it - 1, 2)
                            ple_stage(it - 1, 3)
                p.barrier()
            if kind == "d":
                idx_d += 1
            else:
                idx_m += 1

        outk = []
        for jb in range(4):
            t_ = D("sync", y_d[jb * 4:(jb + 1) * 4].rearrange("j p d -> p j d"), X[:, jb * 4:(jb + 1) * 4, :],
                   [("X", j) for j in range(jb * 4, jb * 4 + 4)], [("y", jb)])
            outk.append(t_)
        p.wait_all("sync", outk)
        p.emit(st)
    return nc


_W_LAYER = ["w_in", "sg_v_g", "sg_v_b", "sg_w_s", "sg_b_s", "q_norm_g", "kv_norm_g", "w_uq", "w_ukv", "out_g", "w_o",
            "ln1_g", "ln1_b", "ln2_g", "ln2_b", "ple_w_g", "ple_b_g", "ple_w_p"]
_W_DENSE = ["ffn_w1", "ffn_w3", "ffn_w2"]
_W_MOE = ["moe_w_r", "moe_w1", "moe_w3", "moe_w2"]


def _gb(hf, j):
    return 4 * (j // 2) + GMAP[hf][j % 2]


def _consts(hf):
    inv_freq = 1.0 / (10000.0 ** (np.arange(0, 32, 2, dtype=np.float32) / 32.0))
    invf = (np.concatenate([inv_freq, inv_freq]) / (2 * np.pi)).astype(np.float32).reshape(32, 1)
    sgn = np.concatenate([-np.ones(16, np.float32), np.ones(16, np.float32)]).reshape(32, 1)
    ident = np.eye(128, dtype=np.float32)
    tril = np.tril(np.ones((128, 128), np.float32))
    m = np.full((128, 4, 2, 128), -30000.0, np.float32)
    ii = np.arange(128)
    diag = (ii[:, None] <= ii[None, :]).astype(np.float32)
    for r in range(2):
        qg = GMAP[hf][r]
        for kb in range(4):
            kg = GMAP[kb // 2][kb % 2]
            if kg < qg:
                m[:, kb, r, :] = 0.0
            elif kg == qg:
                m[:, kb, r, :] = (diag - 1.0) * 30000.0
    negs = np.zeros((128, 66), np.float32)
    negs[:, 32 + 33 * hf] = -1.0
    return {"invf": invf, "sgn": sgn, "ident": ident, "tril": tril, "maskT": m.reshape(128, 1024), "negs": negs}


def _run(kinds, layer_ids, xin, inputs, stop_after=None):
    nc = build(kinds, stop_after=stop_after)
    p = inputs["p"]
    positions = inputs["positions"]
    d_ids = [li // 2 for li in layer_ids if li % 2 == 0]
    m_ids = [li // 2 for li in layer_ids if li % 2 == 1]
    shared = {}
    for k in _W_LAYER:
        a = np.ascontiguousarray(inputs[k][layer_ids])
        if k == "sg_b_s":
            a = a.reshape(len(layer_ids), 1024)
        shared[k] = a
    if d_ids:
        for k in _W_DENSE:
            shared[k] = np.ascontiguousarray(inputs[k][d_ids])
    if m_ids:
        for k in _W_MOE:
            shared[k] = np.ascontiguousarray(inputs[k][m_ids])
    maps = []
    for c in range(8):
        b, hf = c // 2, c % 2
        blks = [_gb(hf, j) for j in range(NB)]
        pc = np.stack([np.stack([p[li, b, g * 128:(g + 1) * 128, :] for g in blks]) for li in layer_ids])
        pos = np.concatenate([positions[b, g * 128:(g + 1) * 128] for g in blks]).astype(np.int32).reshape(1, T)
        m = {"x": xin[c], "p": np.ascontiguousarray(pc), "pos": pos}
        m.update(_consts(hf))
        m.update(shared)
        maps.append(m)
    res = run_bass_kernel_spmd(nc, maps, core_ids=list(range(8)))
    global DBG
    DBG = [res.results[c].get("dbg") for c in range(8)]
    return [np.asarray(res.results[c]["y"]) for c in range(8)]


def _shard_x(x):
    out = []
    for c in range(8):
        b, hf = c // 2, c % 2
        out.append(np.ascontiguousarray(np.stack([x[b, _gb(hf, j) * 128:(_gb(hf, j) + 1) * 128, :] for j in range(NB)])))
    return out


def _unshard(ys, shape):
    out = np.zeros(shape, np.float32)
    for c in range(8):
        b, hf = c // 2, c % 2
        for j in range(NB):
            g = _gb(hf, j)
            out[b, g * 128:(g + 1) * 128, :] = ys[c][j]
    return out


FUSED = True
DBG = None


def kernel(**inputs):
    inputs = {k: np.asarray(v) for k, v in inputs.items()}
    x = inputs["x"].astype(np.float32)
    xs = _shard_x(x)
    if FUSED:
        ys = _run(["d", "m", "d", "m"], [0, 1, 2, 3], xs, inputs)
    else:
        ys = xs
        for li in range(4):
            ys = _run(["d" if li % 2 == 0 else "m"], [li], ys, inputs)
    return _unshard(ys, x.shape)
```

```python
import contextlib
import math
import numpy as np
import concourse.bass as bass
import concourse.mybir as mybir
from concourse.bass_utils import run_bass_kernel_spmd

F32 = mybir.dt.float32
BF16 = mybir.dt.bfloat16
I32 = mybir.dt.int32
AF = mybir.ActivationFunctionType
ALU = mybir.AluOpType
AX = mybir.AxisListType

ENGS = ["sync", "scalar", "vector", "gpsimd", "tensor"]
NDMASEM = 40
GMAP = [[0, 3], [1, 2]]
ALPHA = (2.0 * 4) ** 0.25
EPS = 1e-6
ATT_SCALE = 96 ** -0.5
NB = 16
T = 2048


class Prog:
    def __init__(self, nc):
        self.nc = nc
        self.q = {e: [] for e in ENGS}
        self.cnt = {e: 0 for e in ENGS}
        self.seen = {e: {} for e in ENGS}
        self.lastw = {}
        self.readers = {}
        self.dma_use = [0] * NDMASEM
        self.dma_rr = 0
        self.pending_nosig = {e: False for e in ENGS}

    def _need(self, eng, tickets):
        out = []
        best = {}
        for t in tickets:
            if t is None:
                continue
            k, v = t
            if v > best.get(k, 0):
                best[k] = v
        for k, v in best.items():
            if self.seen[eng].get(k, 0) >= v:
                continue
            self.seen[eng][k] = v
            out.append((k, v))
        return out

    def _rtix(self, x):
        d = self.lastw.get(x)
        return list(d.items()) if d else []

    def _deps(self, r, w):
        ts = []
        for x in r:
            ts.extend(self._rtix(x))
        for x in w:
            ts.extend(self._rtix(x))
            ts.extend(self.readers.get(x, ()))
        return ts

    def _commit(self, ticket, r, w):
        k, v = ticket
        for x in w:
            d = self.lastw.setdefault(x, {})
            if d.get(k, 0) < v:
                d[k] = v
            self.readers[x] = []
        for x in r:
            self.readers.setdefault(x, []).append(ticket)

    def op(self, eng, fn, r=(), w=(), sig=True, pe_acc=False):
        ts = self._deps(r, w)
        if pe_acc:
            ts = [t for t in ts if t[0] != eng]
        else:
            raw = set()
            for x in r:
                raw.update(self._rtix(x))
            ts = [t for t in ts if (t[0] != eng or t in raw)]
        waits = self._need(eng, ts)
        ticket = (eng, self.cnt[eng] + 1)
        if sig:
            self.cnt[eng] += 1
            self.pending_nosig[eng] = False
        else:
            self.pending_nosig[eng] = True
        self.q[eng].append((waits, fn, ("e", eng) if sig else None))
        self._commit(ticket, r, w)
        return ticket

    def dma(self, eng, fn, r=(), w=(), inc=16):
        ts = self._deps(r, w)
        idx = self.dma_rr
        self.dma_rr = (self.dma_rr + 1) % NDMASEM
        use = self.dma_use[idx]
        if use > 0:
            ts.append((("d", idx), use))
        self.dma_use[idx] = use + inc
        ticket = (("d", idx), use + inc)
        waits = self._need(eng, ts)
        self.q[eng].append((waits, fn, ("d", idx, inc)))
        self._commit(ticket, r, w)
        return ticket

    def wait_all(self, eng, tickets):
        waits = self._need(eng, tickets)
        if waits:
            self.q[eng].append((waits, None, None))

    def barrier(self):
        for e in ENGS:
            assert not self.pending_nosig[e]
        ts = [(e, self.cnt[e]) for e in ENGS if self.cnt[e] > 0]
        ts += [(("d", i), self.dma_use[i]) for i in range(NDMASEM) if self.dma_use[i] > 0]
        for e in ENGS:
            self.wait_all(e, ts)

    def emit(self, stack):
        nc = self.nc
        for e in ENGS:
            assert not self.pending_nosig[e], e
        esem = {e: stack.enter_context(nc.semaphore("se_" + e)) for e in ENGS}
        dsem = [stack.enter_context(nc.semaphore("sd_%d" % i)) for i in range(NDMASEM)]

        def semof(k):
            return esem[k] if isinstance(k, str) else dsem[k[1]]

        block = stack.enter_context(nc.Block())

        def mk(ename):
            def body(engine):
                for waits, fn, sg in self.q[ename]:
                    for k, v in waits:
                        engine.wait_ge(semof(k), v)
                    if fn is None:
                        continue
                    ins = fn(engine)
                    if sg is None:
                        continue
                    if sg[0] == "e":
                        ins.then_inc(esem[sg[1]], 1)
                    else:
                        ins.then_inc(dsem[sg[1]], sg[2])
            return body

        for e in ENGS:
            if self.q[e]:
                getattr(block, e)(mk(e))


def build(kinds, stop_after=None):
    nl = len(kinds)
    nd = kinds.count("d")
    nm = kinds.count("m")
    nc = bass.Bass("TRN2", target_bir_lowering=False)

    def DI(name, shape, dt=F32):
        return nc.dram_tensor(name, shape, dt, kind="ExternalInput").ap()

    x_d = DI("x", [NB, 128, 1024])
    p_d = DI("p", [nl, NB, 128, 256])
    pos_d = DI("pos", [1, T], I32)
    invf_d = DI("invf", [32, 1])
    sgn_d = DI("sgn", [32, 1])
    ident_d = DI("ident", [128, 128])
    tril_d = DI("tril", [128, 128])
    mask_d = DI("maskT", [128, 1024])
    negs_d = DI("negs", [128, 66])
    w_in_d = DI("w_in", [nl, 1024, 1440])
    sg_v_g_d = DI("sg_v_g", [nl, 512])
    sg_v_b_d = DI("sg_v_b", [nl, 512])
    sg_w_s_d = DI("sg_w_s", [nl, 8, 128, 128])
    sg_b_s_d = DI("sg_b_s", [nl, 1024])
    q_norm_g_d = DI("q_norm_g", [nl, 256])
    kv_norm_g_d = DI("kv_norm_g", [nl, 128])
    w_uq_d = DI("w_uq", [nl, 256, 768])
    w_ukv_d = DI("w_ukv", [nl, 128, 1024])
    out_g_d = DI("out_g", [nl, 1024])
    w_o_d = DI("w_o", [nl, 1024, 1024])
    ln1_g_d = DI("ln1_g", [nl, 1024])
    ln1_b_d = DI("ln1_b", [nl, 1024])
    ln2_g_d = DI("ln2_g", [nl, 1024])
    ln2_b_d = DI("ln2_b", [nl, 1024])
    ple_w_g_d = DI("ple_w_g", [nl, 1024, 1024])
    ple_b_g_d = DI("ple_b_g", [nl, 1024])
    ple_w_p_d = DI("ple_w_p", [nl, 256, 1024])
    if nd:
        ffn_w1_d = DI("ffn_w1", [nd, 1024, 2816])
        ffn_w3_d = DI("ffn_w3", [nd, 1024, 2816])
        ffn_w2_d = DI("ffn_w2", [nd, 2816, 1024])
    if nm:
        moe_w_r_d = DI("moe_w_r", [nm, 1024, 8])
        moe_w1_d = DI("moe_w1", [nm, 8, 1024, 3584])
        moe_w3_d = DI("moe_w3", [nm, 8, 1024, 3584])
        moe_w2_d = DI("moe_w2", [nm, 8, 3584, 1024])
    y_d = nc.dram_tensor("y", [NB, 128, 1024], F32, kind="ExternalOutput").ap()
    dbg_d = nc.dram_tensor("dbg", [128, 8192], BF16, kind="ExternalOutput").ap() if stop_after == "mixer_raw" else None
    ag_in = [nc.dram_tensor("ag_in%d" % l, [160, T], BF16).ap() for l in range(nl)]
    ag_out = [nc.dram_tensor("ag_out%d" % l, [320, T], BF16).ap() for l in range(nl)]

    st = contextlib.ExitStack()
    with st:
        def SB(name, shape, dt=F32):
            return st.enter_context(nc.sbuf_tensor(name, shape, dt))

        ps = st.enter_context(nc.psum_tensor("ps", [128, 4096], F32))

        def bank(b, lo=0, hi=512, p0=0, p1=128):
            return ps[p0:p1, b * 512 + lo:b * 512 + hi]

        def bankb(b, lo=0, hi=512, p0=0, p1=128):
            v = ps[p0:p1, b * 512:(b + 1) * 512].bitcast(BF16)
            return v[:, lo:hi]

        X = SB("X", [128, NB, 1024])
        WB = [SB("WB0", [128, 12288], BF16), SB("WB1", [128, 12288], BF16)]
        R1 = SB("R1", [128, 20480], BF16)
        R2 = SB("R2", [128, 8192], BF16)
        CS = SB("CS", [32, 2, T], BF16)
        LNG = SB("LNG", [128, 1024]); LNB = SB("LNB", [128, 1024])
        TA = SB("TA", [128, 1024])
        ident_f = SB("ident_f", [128, 128]); ident_b = SB("ident_b", [128, 128], BF16)
        tril_f = SB("tril_f", [128, 128])
        maskT = SB("maskTs", [128, 4, 256], BF16)
        ones_b = SB("ones_b", [128, 128], BF16); ones_f = SB("ones_f", [128, 128])
        NEGS = SB("NEGS", [128, 66], BF16)
        OG = SB("OG", [128, 8]); QG = SB("QG", [128, 2]); KG = SB("KG", [128, 1])
        invf = SB("invf_s", [32, 1]); sgn = SB("sgn_s", [32, 1])
        bsr = SB("bsr", [1, 1024], BF16); bgr = SB("bgr", [1, 1024], BF16)
        wsT = SB("wsT", [128, 8, 128], BF16)
        WKpad = SB("WKpad", [128, 128], BF16)
        WQpad = SB("WQpad", [128, 2, 128], BF16); WQsw = SB("WQsw", [128, 2, 32], BF16)
        WLsw = SB("WLsw", [128, 8, 32], BF16)
        xTt0 = SB("xTt0", [128, 8, 128], BF16)
        xTt = [xTt0, xTt0]
        sm = SB("sm", [128, 64])
        mstat = SB("mstat", [128, 16])
        Mst = SB("Mst", [128, 33], BF16)
        comb = SB("comb", [128, NB, 8])
        ssq = SB("ssq", [128, NB])
        wr_f = SB("wr_f", [128, 8, 8])
        wr_h = SB("wr_h", [128, 8, 8], BF16); wr_l = SB("wr_l", [128, 8, 8], BF16)
        lgt = SB("lgt", [128, 4, 8])
        stt = SB("stt", [128, 4, 6])
        aTt = SB("aTt", [128, 4, 128], BF16)
        cqn = SB("cqn", [128, 384], BF16)
        pblk = SB("pblk", [128, 256], BF16); pTt = SB("pTt", [128, 2, 128], BF16)
        krt = SB("krt", [32, 256])

        CKV = R1[:, 0:4096]
        CQT = R1[:, 4096:8192].rearrange("p (c t) -> p c t", c=2)
        KT = R1[:, 8192:12288]
        VA = R1[:, 12288:16384].rearrange("p (k c) -> p k c", k=32)
        QTg = [R1[:, 16384:16896], R1[:, 16896:17408]]
        PT = [R1[:, 17408:17920], R1[:, 17920:18432], R1[:, 19968:20480]]
        LAT = R1[:, 12288:14336]
        KRl = R1[0:32, 14336:16384]
        sqb = R1[:, 18432:18944]
        BT = R1[:, 0:16384].rearrange("p (c t) -> p c t", c=8)
        MT = R2[:, :].rearrange("p (c t) -> p c t", c=4)
        GT = R2[:, :].rearrange("p (c t) -> p c t", c=4)
        TBr = WB[1][:, 0:2048].bitcast(F32)
        bcs = WB[1][:, 2048:3072].bitcast(F32)
        KT2 = WB[1][:, 4096:8192]
        VA2 = WB[1][:, 8192:12288].rearrange("p (k c) -> p k c", k=32)
        KTs = [KT, KT2]
        VAs = [VA, VA2]
        WKpads = [WKpad[:], WB[1][:, 320:448]]
        WQpads = [WQpad[:], WB[1][:, 0:256].rearrange("p (c f) -> p c f", c=2)]
        WQsws = [WQsw[:], WB[1][:, 256:320].rearrange("p (c f) -> p c f", c=2)]
        rl = WB[1][:, 3072:4096].bitcast(F32)
        silu_t = [TA[:, 0:512], TA[:, 512:1024]]
        SGG = R2[:, 0:1024].bitcast(F32)
        SGB = R2[:, 1024:2048].bitcast(F32)
        abf = R1[:, 18944:19456]
        vnb = R1[:, 19456:19968]
        junk = R1[:, 19968:20480]

        p = Prog(nc)

        def V(fn, r, w, **k):
            return p.op("vector", fn, r=r, w=w, **k)

        def A(fn, r, w, **k):
            return p.op("scalar", fn, r=r, w=w, **k)

        def G(fn, r, w, **k):
            return p.op("gpsimd", fn, r=r, w=w, **k)

        def MM(out, lhsT, rhs, start, stop, r, w, sig=None):
            if sig is None:
                sig = stop
            return p.op("tensor", lambda e: e.matmul(out, lhsT=lhsT, rhs=rhs, start=start, stop=stop),
                        r=r, w=w, sig=sig, pe_acc=not start)

        def TR(out, in_, idt, r, w, sig=True):
            return p.op("tensor", lambda e: e.transpose(out, in_, idt), r=r, w=w, sig=sig)

        def D(eng, out, in_, r, w, **kw):
            return p.dma(eng, lambda e: e.dma_start(out=out, in_=in_, **kw), r=r, w=w)

        D("sync", ident_f[:], ident_d, [], ["ident_f"])
        D("gpsimd", ident_b[:], ident_d, [], ["ident_b"])
        D("sync", tril_f[:], tril_d, [], ["tril_f"])
        D("gpsimd", maskT[:].rearrange("p a b -> p (a b)"), mask_d, [], ["maskT"])
        D("sync", invf[:], invf_d, [], ["invf"])
        D("gpsimd", NEGS[:], negs_d, [], ["NEGS"])
        D("sync", sgn[:], sgn_d, [], ["sgn"])
        G(lambda e: e.memset(ones_b[:], 1.0), [], ["ones_b"])
        G(lambda e: e.memset(ones_f[:], 1.0), [], ["ones_f"])
        G(lambda e: e.memset(WKpad[:], 0.0), [], [("WKpad", 0)])
        G(lambda e: e.memset(WQpad[:], 0.0), [], [("WQpad", 0)])
        G(lambda e: e.memset(Mst[:], 0.0), [], ["Mst"])
        for jb in range(4):
            D("sync", X[:, jb * 4:(jb + 1) * 4, :], x_d[jb * 4:(jb + 1) * 4].rearrange("j p d -> p j d"),
              [], [("X", j) for j in range(jb * 4, jb * 4 + 4)])
        kf = R2[0:32, 0:2048].bitcast(F32)
        tt = R2[0:32, 2048:4096].bitcast(F32)
        uu = R2[0:32, 4096:6144].bitcast(F32)
        ti = TA[0:32, :].bitcast(I32)
        tf = R2[0:32, 6144:8192].bitcast(F32)
        SK = ["setup"]
        for hh in range(2):
            D("sync", ti, pos_d[:, hh * 1024:(hh + 1) * 1024].partition_broadcast(32), SK, SK)
            V(lambda e: e.tensor_copy(out=tf, in_=ti), SK + ["invf", "sgn"], SK)
            for which in range(2):
                dst = CS[:, which, hh * 1024:(hh + 1) * 1024]
                V(lambda e, which=which: e.tensor_scalar(out=uu, in0=tf, scalar1=invf[:, 0:1], scalar2=(0.25 if which == 0 else 0.0),
                                                           op0=ALU.mult, op1=ALU.add), SK, SK)
                V(lambda e: e.tensor_copy(out=ti, in_=uu), SK, SK)
                V(lambda e: e.tensor_copy(out=kf, in_=ti), SK, SK)
                V(lambda e: e.tensor_tensor(out=uu, in0=uu, in1=kf, op=ALU.subtract), SK, SK)
                V(lambda e: e.tensor_scalar(out=tt, in0=uu, scalar1=0.5, scalar2=None, op0=ALU.is_ge), SK, SK)
                V(lambda e: e.tensor_tensor(out=uu, in0=uu, in1=tt, op=ALU.subtract), SK, SK)
                A(lambda e: e.activation(out=uu, in_=uu, func=AF.Sin, scale=2 * math.pi), SK, SK)
                if which == 1:
                    V(lambda e, dst=dst: e.tensor_scalar(out=dst, in0=uu, scalar1=sgn[:, 0:1], scalar2=None, op0=ALU.mult), SK, SK)
                else:
                    V(lambda e, dst=dst: e.tensor_copy(out=dst, in_=uu), SK, SK)
        p.barrier()

        def rstd_from(ssum_ap, n, out_ap, key):
            A(lambda e: e.activation(out=out_ap, in_=ssum_ap, func=AF.Sqrt, bias=EPS, scale=1.0 / n), [key], [key])
            V(lambda e: e.reciprocal(out=out_ap, in_=out_ap), [key], [key])

        def layernorm_block(i, src, gt, bt, keyg, tmp=None, tk="TA", pi=0):
            xk = ("X", i)
            if tmp is None:
                tmp = TA[:]
            so = 32 * pi
            sk = "stt%d" % pi
            k1, k2, k3 = "lnmv%d" % pi, "lnr%d" % pi, "lnn%d" % pi
            V(lambda e: e.bn_stats(out=stt[:, 2 * pi, :], in_=src[:, 0:512]), [xk], [sk])
            V(lambda e: e.bn_stats(out=stt[:, 2 * pi + 1, :], in_=src[:, 512:1024]), [xk], [sk])
            V(lambda e: e.bn_aggr(out=sm[:, so:so + 2], in_=stt[:, 2 * pi:2 * pi + 2, :]), [sk], [k1])
            A(lambda e: e.activation(out=sm[:, so + 2:so + 3], in_=sm[:, so + 1:so + 2], func=AF.Sqrt, bias=EPS, scale=1.0), [k1], [k2])
            V(lambda e: e.reciprocal(out=sm[:, so + 2:so + 3], in_=sm[:, so + 2:so + 3]), [k2], [k2])
            V(lambda e: e.tensor_scalar(out=sm[:, so + 3:so + 4], in0=sm[:, so:so + 1], scalar1=-1.0, scalar2=sm[:, so + 2:so + 3], op0=ALU.mult, op1=ALU.mult),
              [k1, k2], [k3])
            A(lambda e: e.activation(out=tmp, in_=src, func=AF.Identity, bias=sm[:, so + 3:so + 4], scale=sm[:, so + 2:so + 3]), [xk, k2, k3], [tk])
            V(lambda e: e.tensor_tensor(out=tmp, in0=tmp, in1=gt[:], op=ALU.mult), [tk, keyg], [tk])
            G(lambda e: e.tensor_tensor(out=X[:, i, :], in0=tmp, in1=bt[:], op=ALU.add), [tk, keyg], [xk])

        def transpose_block(i, dst, dkey, also_f32=None):
            xk = ("X", i)
            for kc in range(8):
                TR(bank(kc // 4, (kc % 4) * 128, (kc % 4 + 1) * 128), X[:, i, kc * 128:(kc + 1) * 128], ident_f[:],
                   [xk, "ident_f"], ["B%d" % (kc // 4)], sig=(kc % 4 == 3))
            A(lambda e: e.activation(out=dst, in_=ps[:, 0:1024].rearrange("p (c t) -> p c t", c=8), func=AF.Copy), ["B0", "B1"], [dkey])
            if also_f32 is not None:
                V(lambda e: e.tensor_copy(out=also_f32, in_=ps[:, 0:1024]), ["B0", "B1"], ["WB1"])

        idx_d = 0
        idx_m = 0
        for l, kind in enumerate(kinds):
            last = (l == nl - 1)
            WLAT = WB[0][:, 0:3328].rearrange("p (c f) -> p c f", c=8)
            WUQ = WB[0][:, 3328:4864].rearrange("p (c f) -> p c f", c=2)
            WUKV = WB[0][:, 4864:5888]
            WOM = WB[0][:, 5888:9984].rearrange("p (c f) -> p c f", c=4)
            WUV = WB[1][:, 0:8192].rearrange("p (c f) -> p c f", c=8)
            WOA = WB[1][:, 8192:12288].rearrange("p (c f) -> p c f", c=4)
            D("gpsimd", WLAT, w_in_d[l, :, 1024:1440].rearrange("(c p) f -> p c f", p=128), [], ["WB0"])
            for hh in range(2):
                D("gpsimd", WUV[:, hh * 4:(hh + 1) * 4, :], w_in_d[l, hh * 512:(hh + 1) * 512, 0:1024].rearrange("(c p) f -> p c f", p=128), [], ["WB1"])
            D("sync", OG[:], out_g_d[l].rearrange("(c p) -> p c", p=128), [], ["OG"], allow_slow_non_contiguous=True)
            D("sync", QG[:], q_norm_g_d[l].rearrange("(c p) -> p c", p=128), [], ["QG"], allow_slow_non_contiguous=True)
            D("sync", KG[:], kv_norm_g_d[l].rearrange("(c p) -> p c", p=128), [], ["KG"], allow_slow_non_contiguous=True)
            D("sync", LNG[:], ln1_g_d[l:l + 1, :].partition_broadcast(128), [], ["LN"])
            D("sync", LNB[:], ln1_b_d[l:l + 1, :].partition_broadcast(128), [], ["LN"])
            D("sync", SGG[:], sg_v_g_d[l:l + 1, :].partition_broadcast(128), [], ["SG"])
            D("sync", SGB[:], sg_v_b_d[l:l + 1, :].partition_broadcast(128), [], ["SG"])
            D("gpsimd", bsr[:], sg_b_s_d[l:l + 1, :], [], ["bsr"])
            D("gpsimd", bgr[:], ple_b_g_d[l:l + 1, :], [], ["bgr"])
            stg = [TA, TA]
            si = 0

            def fold(dst, src_ap, ncols, gcol, gkey, wkey):
                nonlocal si
                t_ = stg[si % 2]
                tk = "TA"
                si += 1
                D("sync", t_[:, 0:ncols], src_ap, [], [tk])
                V(lambda e: e.tensor_scalar(out=dst, in0=t_[:, 0:ncols], scalar1=gcol, scalar2=None, op0=ALU.mult), [tk, gkey], [wkey])

            for c in range(2):
                fold(WUQ[:, c, :], w_uq_d[l, c * 128:(c + 1) * 128, :], 768, QG[:, c:c + 1], "QG", "WB0")
            fold(WUKV, w_ukv_d[l], 1024, KG[:, 0:1], "KG", "WB0")
            for c in range(4):
                fold(WOA[:, c, :], w_o_d[l, c * 128:(c + 1) * 128, :], 1024, OG[:, c:c + 1], "OG", "WB1")
            for c in range(4):
                fold(WOM[:, c, :], w_o_d[l, 512 + c * 128:512 + (c + 1) * 128, :], 1024, OG[:, 4 + c:5 + c], "OG", "WB0")
            wsn = TA[:].rearrange("p (g s) -> p g s", g=8)
            D("sync", wsn, sg_w_s_d[l].rearrange("g t s -> t g s"), [], ["TA"])
            V(lambda e: e.tensor_tensor(out=wsn, in0=wsn, in1=tril_f[:].unsqueeze(1).to_broadcast([128, 8, 128]), op=ALU.mult), ["TA", "tril_f"], ["TA"])
            for g in range(8):
                TR(bank(g // 4, (g % 4) * 128, (g % 4 + 1) * 128), wsn[:, g, :], ident_f[:], ["TA", "ident_f"], ["B%d" % (g // 4)], sig=(g % 4 == 3))
            A(lambda e: e.activation(out=wsT[:].rearrange("p g t -> p (g t)"), in_=ps[:, 0:1024], func=AF.Copy), ["B0", "B1"], ["wsT"])
            V(lambda e: e.tensor_copy(out=WLsw[:, :, 0:16], in_=WLAT[:, :, 400:416]), ["WB0"], ["WLsw"])
            V(lambda e: e.tensor_copy(out=WLsw[:, :, 16:32], in_=WLAT[:, :, 384:400]), ["WB0"], ["WLsw"])

            TAp = [TA[:], R1[:, 0:2048].bitcast(F32)]
            xTp2 = [xTt0[:], R1[:, 2048:3072].rearrange("p (c t) -> p c t", c=8)]
            aTp = [aTt[:], R1[:, 3072:3584].rearrange("p (c t) -> p c t", c=4)]
            cqp = [cqn[:], R1[:, 3584:3968]]
            abp = [abf, R1[:, 8192:8704]]
            vnp = [vnb, R1[:, 8704:9216]]
            krp = [krt[:], R1[0:32, 9216:9728].bitcast(F32)]
            def a12_stage(i, which):
                xk = ("X", i)
                pi = i % 2
                so = 32 * pi
                xt = xTp2[pi]; xtk = ("xTt", pi)
                TA_ = TAp[pi]; tak = "TA" if pi == 0 else "TA1"
                cq_ = cqp[pi]; cqk = "cqn%d" % pi
                ab_ = abp[pi]; abk = "abf%d" % pi
                vn_ = vnp[pi]; vnk = "vnb%d" % pi
                aT_ = aTp[pi]; aTk = "aTt%d" % pi
                kr_ = krp[pi]
                ksq, kskv, ksa = "sq%d" % pi, "skv%d" % pi, "sa%d" % pi
                k1, k2, k3, skk = "lnmv%d" % pi, "lnr%d" % pi, "lnn%d" % pi, "stt%d" % pi
                if which == 1:
                    transpose_block(i, xt, xtk)
                    A(lambda e, i=i: e.activation(out=X[:, i, :], in_=X[:, i, :], func=AF.Copy, scale=ALPHA), [xk], [xk])
                    for kc in range(8):
                        MM(bank(2, 0, 384), xt[:, kc, :], WLAT[:, kc, 0:384], kc == 0, kc == 7, [xtk, "WB0"], ["B2"])
                    for kc in range(8):
                        MM(bank(3, 0, 128, 0, 32), WLAT[:, kc, 384:416], xt[:, kc, :], kc == 0, kc == 7, [xtk, "WB0"], ["B3"], sig=False)
                    for kc in range(8):
                        MM(bank(3, 128, 256, 0, 32), WLsw[:, kc, :], xt[:, kc, :], kc == 0, kc == 7, [xtk, "WLsw"], ["B3"])
                    for hh in range(2):
                        for kc in range(8):
                            MM(bank(5 + hh), xt[:, kc, :], WUV[:, kc, hh * 512:(hh + 1) * 512], kc == 0, kc == 7, [xtk, "WB1"], ["B%d" % (5 + hh)])
                elif which == 2:
                    A(lambda e, so=so: e.activation(out=junk[:, 0:256], in_=bank(2, 0, 256), func=AF.Square, accum_out=sm[:, so + 8:so + 9]), ["B2"], ["junk", ksq])
                    A(lambda e, so=so: e.activation(out=junk[:, 0:128], in_=bank(2, 256, 384), func=AF.Square, accum_out=sm[:, so + 9:so + 10]), ["B2"], ["junk", kskv])
                    rstd_from(sm[:, so + 8:so + 9], 256.0, sm[:, so + 8:so + 9], ksq)
                    rstd_from(sm[:, so + 9:so + 10], 128.0, sm[:, so + 9:so + 10], kskv)
                    V(lambda e, so=so, cq_=cq_: e.tensor_scalar(out=cq_[:, 0:256], in0=bank(2, 0, 256), scalar1=sm[:, so + 8:so + 9], scalar2=None, op0=ALU.mult), ["B2", ksq], [cqk])
                    V(lambda e, so=so, cq_=cq_: e.tensor_scalar(out=cq_[:, 256:384], in0=bank(2, 256, 384), scalar1=sm[:, so + 9:so + 10], scalar2=None, op0=ALU.mult), ["B2", kskv], [cqk])
                    for c in range(3):
                        TR(bankb(4, c * 128, (c + 1) * 128), cq_[:, c * 128:(c + 1) * 128], ident_b[:], [cqk, "ident_b"], ["B4"], sig=(c == 2))
                    A(lambda e, i=i: e.activation(out=CQT[:, :, i * 128:(i + 1) * 128], in_=bankb(4, 0, 256).rearrange("p (c t) -> p c t", c=2), func=AF.Copy),
                      ["B4"], [("CQT", i)])
                    V(lambda e, i=i: e.tensor_copy(out=LAT[:, i * 128:(i + 1) * 128], in_=bankb(4, 256, 384)), ["B4"], ["LAT"])
                    V(lambda e, i=i, kr_=kr_: e.tensor_tensor(out=kr_[:, 0:128], in0=bank(3, 0, 128, 0, 32), in1=CS[:, 0, i * 128:(i + 1) * 128], op=ALU.mult),
                      ["B3"], ["krt0_%d" % pi])
                    V(lambda e, i=i, kr_=kr_: e.tensor_tensor(out=kr_[:, 128:256], in0=bank(3, 128, 256, 0, 32), in1=CS[:, 1, i * 128:(i + 1) * 128], op=ALU.mult),
                      ["B3"], ["krt1_%d" % pi])
                    V(lambda e, i=i, kr_=kr_: e.tensor_tensor(out=KRl[:, i * 128:(i + 1) * 128], in0=kr_[:, 0:128], in1=kr_[:, 128:256], op=ALU.add),
                      ["krt0_%d" % pi, "krt1_%d" % pi], ["KRl"])
                    A(lambda e, TA_=TA_: e.activation(out=TA_, in_=ps[:, 5 * 512:7 * 512], func=AF.Gelu), ["B5", "B6"], [tak])
                elif which == 3:
                    V(lambda e, TA_=TA_, pi=pi: e.bn_stats(out=stt[:, 2 * pi, :], in_=TA_[:, 512:1024]), [tak], [skk])
                    V(lambda e, so=so, pi=pi: e.bn_aggr(out=sm[:, so:so + 2], in_=stt[:, 2 * pi:2 * pi + 1, :]), [skk], [k1])
                    A(lambda e, so=so: e.activation(out=sm[:, so + 2:so + 3], in_=sm[:, so + 1:so + 2], func=AF.Sqrt, bias=EPS, scale=1.0), [k1], [k2])
                    V(lambda e, so=so: e.reciprocal(out=sm[:, so + 2:so + 3], in_=sm[:, so + 2:so + 3]), [k2], [k2])
                    V(lambda e, so=so: e.tensor_scalar(out=sm[:, so + 3:so + 4], in0=sm[:, so:so + 1], scalar1=-1.0, scalar2=sm[:, so + 2:so + 3], op0=ALU.mult, op1=ALU.mult), [k1, k2], [k3])
                    A(lambda e, so=so, TA_=TA_: e.activation(out=TA_[:, 512:1024], in_=TA_[:, 512:1024], func=AF.Identity, bias=sm[:, so + 3:so + 4], scale=sm[:, so + 2:so + 3]), [tak, k2, k3], [tak])
                    V(lambda e, TA_=TA_: e.tensor_tensor(out=TA_[:, 512:1024], in0=TA_[:, 512:1024], in1=SGG[:], op=ALU.mult), [tak, "SG"], [tak])
                    V(lambda e, TA_=TA_, vn_=vn_: e.tensor_tensor(out=vn_[:], in0=TA_[:, 512:1024], in1=SGB[:], op=ALU.add), [tak, "SG"], [vnk])
                elif which == 4:
                    for g in range(8):
                        MM(bank(7, g * 64, (g + 1) * 64), wsT[:, g, :], vn_[:, g * 64:(g + 1) * 64], True, False, ["wsT", vnk], ["B7"], sig=False)
                        MM(bank(7, g * 64, (g + 1) * 64), bsr[0:1, g * 128:(g + 1) * 128], ones_b[0:1, 0:64], False, True, ["bsr", "ones_b"], ["B7"], sig=(g == 7))
                    V(lambda e, TA_=TA_, ab_=ab_: e.tensor_tensor(out=ab_[:], in0=TA_[:, 0:512], in1=bank(7), op=ALU.mult), [tak, "B7"], [abk])
                    A(lambda e, so=so, ab_=ab_: e.activation(out=junk[:], in_=ab_[:], func=AF.Square, accum_out=sm[:, so + 10:so + 11]), [abk], ["junk", ksa])
                    rstd_from(sm[:, so + 10:so + 11], 512.0, sm[:, so + 10:so + 11], ksa)
                    for c in range(4):
                        TR(bankb(4, 512 + c * 128, 512 + (c + 1) * 128), ab_[:, c * 128:(c + 1) * 128], ident_b[:], [abk, "ident_b"], ["B4b"], sig=(c == 3))
                    A(lambda e, aT_=aT_: e.activation(out=aT_, in_=bankb(4, 512, 1024).rearrange("p (c t) -> p c t", c=4), func=AF.Copy), ["B4b"], [aTk])
                    for hh in range(2):
                        for c in range(4):
                            MM(bank(5 + hh), aT_[:, c, :], WOA[:, c, hh * 512:(hh + 1) * 512], c == 0, c == 3, [aTk, "WB1"], ["B%d" % (5 + hh)])
                    V(lambda e, i=i, so=so: e.scalar_tensor_tensor(out=X[:, i, :], in0=ps[:, 5 * 512:7 * 512], scalar=sm[:, so + 10:so + 11], in1=X[:, i, :], op0=ALU.mult, op1=ALU.add),
                      ["B5", "B6", ksa, xk], [xk])


            for it in range(NB + 1):
                if it < NB:
                    a12_stage(it, 1)
                if it >= 1:
                    a12_stage(it - 1, 3)
                if it < NB:
                    a12_stage(it, 2)
                if it >= 1:
                    a12_stage(it - 1, 4)

            D("sync", ag_in[l][0:128, :], LAT, ["LAT"], ["ag_in"])
            D("sync", ag_in[l][128:160, :], KRl, ["KRl"], ["ag_in"])
            p.dma("gpsimd", lambda e, l=l: e.collective_compute("AllGather", ALU.bypass, replica_groups=[[0, 1], [2, 3], [4, 5], [6, 7]],
                                                                ins=[ag_in[l].opt()], outs=[ag_out[l].opt()]), r=["ag_in"], w=["ag_out"], inc=1)
            p.barrier()
            for hk in range(2):
                D("sync", CKV[:, hk * T:(hk + 1) * T], ag_out[l][hk * 160:hk * 160 + 128, :], ["ag_out"], [("CKV", hk)])
                D("sync", KT[0:32, hk * T:(hk + 1) * T], ag_out[l][hk * 160 + 128:hk * 160 + 160, :], ["ag_out"], [("KTr", hk)])
                D("sync", KT2[0:32, hk * T:(hk + 1) * T], ag_out[l][hk * 160 + 128:hk * 160 + 160, :], ["ag_out"], [("KTr", hk)])
            p.barrier()
            for KTx in (KT, KT2):
                G(lambda e, KTx=KTx: e.memset(KTx[32:64, :], 0.0), [], ["KTc"])
                G(lambda e, KTx=KTx: e.memset(KTx[32:33, :], 1.0), [], ["KTc"])
            G(lambda e: e.memset(WKpads[1], 0.0), [], [("WKpad", 1)])
            G(lambda e: e.memset(WQpads[1], 0.0), [], [("WQpad", 1)])
            for gq in range(2):
                G(lambda e, gq=gq: e.memset(QTg[gq][32:64, :], 0.0), [], [("QT", gq)])
            p.barrier()
            ckv_keys = [("CKV", 0), ("CKV", 1)]
            ktr_keys = [("KTr", 0), ("KTr", 1), "KTc"]

            def pro_pads(h):
                hp = h % 2
                V(lambda e: e.tensor_copy(out=WKpads[hp][:, 64:128], in_=WUKV[:, h * 128:h * 128 + 64]), ["WB0"], [("WKpad", hp)])
                V(lambda e: e.tensor_copy(out=WQpads[hp][:, :, 0:32], in_=WUQ[:, :, h * 96 + 64:h * 96 + 96]), ["WB0"], [("WQpad", hp)])
                V(lambda e: e.tensor_copy(out=WQpads[hp][:, :, 64:128], in_=WUQ[:, :, h * 96:h * 96 + 64]), ["WB0"], [("WQpad", hp)])
                V(lambda e: e.tensor_copy(out=WQsws[hp][:, :, 0:16], in_=WUQ[:, :, h * 96 + 80:h * 96 + 96]), ["WB0"], [("WQsw", hp)])
                V(lambda e: e.tensor_copy(out=WQsws[hp][:, :, 16:32], in_=WUQ[:, :, h * 96 + 64:h * 96 + 80]), ["WB0"], [("WQsw", hp)])

            def pro_k(h, c):
                hp = h % 2
                KTx = KTs[hp]
                b_ = c % 2
                MM(bank(b_), WKpads[hp], CKV[:, c * 512:(c + 1) * 512], True, True, [("WKpad", hp), ("CKV", c // 4)], ["B%d" % b_])
                if c % 2 == 0:
                    A(lambda e: e.activation(out=KTx[64:128, c * 512:(c + 1) * 512], in_=bank(b_, 0, 512, 64, 128), func=AF.Copy), ["B%d" % b_], [("KTn", hp, c)])
                else:
                    V(lambda e: e.tensor_copy(out=KTx[64:128, c * 512:(c + 1) * 512], in_=bank(b_, 0, 512, 64, 128)), ["B%d" % b_], [("KTn", hp, c)])

            def pro_v(h, q4):
                hp = h % 2
                VAx = VAs[hp]
                voff = 0 if hp == 0 else 64
                b_ = q4 % 2
                for kk in range(8):
                    kb = q4 * 8 + kk
                    MM(bank(b_, kk * 64, (kk + 1) * 64), CKV[:, kb * 128:(kb + 1) * 128], WUKV[:, h * 128 + 64:h * 128 + 128], True, True,
                       [("CKV", kb // 16), "WB0"], ["B%d" % b_], sig=(kk == 7))
                if q4 % 2 == 0:
                    A(lambda e: e.activation(out=VAx[:, q4 * 8:(q4 + 1) * 8, voff:voff + 64],
                                             in_=bank(b_).rearrange("p (k c) -> p k c", k=8), func=AF.Copy), ["B%d" % b_], [("VA", hp)])
                else:
                    V(lambda e: e.tensor_copy(out=VAx[:, q4 * 8:(q4 + 1) * 8, voff:voff + 64],
                                              in_=bank(b_).rearrange("p (k c) -> p k c", k=8)), ["B%d" % b_], [("VA", hp)])
                if q4 == 3:
                    onescol = 64 if hp == 0 else 0
                    G(lambda e: e.memset(VAx[:, :, onescol:onescol + 1], 1.0), [("VA", hp)], [("VA", hp)])

            def prologue(h):
                pro_pads(h)
                for c in range(8):
                    pro_k(h, c)
                for q4 in range(4):
                    pro_v(h, q4)

            kalls = [ktr_keys + [("KTn", hp_, c) for c in range(8)] for hp_ in range(2)]

            def qbuild(h, Gq, u):
                qbuild_a(h, Gq, u)
                qbuild_b(h, Gq, u)

            def qbuild_a(h, Gq, u):
                qb = u % 2
                QT_ = QTg[qb]
                qk = ("QT", qb)
                cqk = [("CQT", Gq * 4 + t_) for t_ in range(4)]
                for c in range(2):
                    MM(bank(0), WQpads[h % 2][:, c, :], CQT[:, c, Gq * 512:(Gq + 1) * 512], c == 0, c == 1, [("WQpad", h % 2)] + cqk, ["B0"])
                for c in range(2):
                    MM(bank(1, 0, 512, 0, 32), WQsws[h % 2][:, c, :], CQT[:, c, Gq * 512:(Gq + 1) * 512], c == 0, c == 1, [("WQsw", h % 2)] + cqk, ["B1"])
                V(lambda e: e.tensor_copy(out=QT_[64:128, :], in_=bank(0, 0, 512, 64, 128)), ["B0"], [qk])
                G(lambda e: e.memset(QT_[32:33, :], 0.0), [], [qk])
                V(lambda e: e.tensor_tensor(out=TA[0:32, 0:512], in0=bank(0, 0, 512, 0, 32), in1=CS[:, 0, Gq * 512:(Gq + 1) * 512], op=ALU.mult), ["B0"], ["TA"])
                V(lambda e: e.tensor_tensor(out=TA[0:32, 512:1024], in0=bank(1, 0, 512, 0, 32), in1=CS[:, 1, Gq * 512:(Gq + 1) * 512], op=ALU.mult), ["B1"], ["TA"])
                V(lambda e: e.tensor_tensor(out=QT_[0:32, :], in0=TA[0:32, 0:512], in1=TA[0:32, 512:1024], op=ALU.add), ["TA"], [qk])
                V(lambda e: e.tensor_tensor(out=abf[:], in0=QT_[:, :], in1=KTs[h % 2][:, Gq * 512:(Gq + 1) * 512], op=ALU.mult), [qk] + kalls[h % 2], ["abf"])
                G(lambda e: e.tensor_tensor(out=vnb[:], in0=QT_[:, :], in1=KTs[h % 2][:, T + Gq * 512:T + (Gq + 1) * 512], op=ALU.mult), [qk] + kalls[h % 2], ["vnb"])

            def qbuild_b(h, Gq, u):
                qb = u % 2
                QT_ = QTg[qb]
                qk = ("QT", qb)
                MM(bank(2, 0, 512, 0, 33), NEGS[:, 0:33], abf[:], True, False, ["NEGS", "abf"], ["B2"], sig=False)
                MM(bank(2, 0, 512, 0, 33), NEGS[:, 33:66], vnb[:], False, True, ["NEGS", "vnb"], ["B2"], sig=True)
                A(lambda e: e.activation(out=QT_[32:33, :], in_=bank(2, 0, 512, 32, 33), func=AF.Copy), ["B2"], [qk])

            def tail(h, Gq, u):
                tail_a(h, Gq, u)
                tail_b(h, Gq, u)

            def tail_a(h, Gq, u):
                par = h % 2
                bo = 7 if u % 2 == 0 else 3
                lrow = 64 if par == 0 else 0
                V(lambda e: e.reciprocal(out=rl[lrow:lrow + 1, :], in_=bank(bo, 0, 512, lrow, lrow + 1)), ["B%d" % bo], ["WB1"])

            def tail_b(h, Gq, u):
                par = h % 2
                bo = 7 if u % 2 == 0 else 3
                bok = "B%d" % bo
                Mv = 65 if par == 0 else 128
                orow0 = 0 if par == 0 else 64
                lrow = 64 if par == 0 else 0
                MM(bank(2, 0, 512, 0, Mv if par else 64), ones_f[lrow:lrow + 1, 0:(128 if par else 64)], rl[lrow:lrow + 1, :], True, True, ["WB1", "ones_f"], ["B2"])
                V(lambda e: e.tensor_copy(out=bcs[orow0:orow0 + 64, :], in_=bank(2, 0, 512, orow0, orow0 + 64)), ["B2"], ["WB1"])
                V(lambda e: e.tensor_tensor(out=MT[orow0:orow0 + 64, h // 2, Gq * 512:(Gq + 1) * 512],
                                            in0=bank(bo, 0, 512, orow0, orow0 + 64), in1=bcs[orow0:orow0 + 64, :], op=ALU.mult),
                  [bok, "WB1"], [("MT", Gq)])
                V(lambda e: e.tensor_tensor(out=sqb[orow0:orow0 + 64, :], in0=MT[orow0:orow0 + 64, h // 2, Gq * 512:(Gq + 1) * 512],
                                            in1=MT[orow0:orow0 + 64, h // 2, Gq * 512:(Gq + 1) * 512], op=ALU.mult),
                  [("MT", Gq)], ["sqb"])
                for tb in range(4):
                    col = Gq * 4 + tb
                    MM(bank(4, h * 16 + col, h * 16 + col + 1), sqb[orow0:orow0 + 64, tb * 128:(tb + 1) * 128], ones_b[orow0:orow0 + 64, 0:1], True, True,
                       ["sqb", "ones_b"], ["B4s"], sig=(tb == 3))

            def pass2(h, Gq, u, hooks):
                par = h % 2
                qb = u % 2
                QT_ = QTg[qb]
                qk = ("QT", qb)
                bo = 7 if u % 2 == 0 else 3
                bok = "B%d" % bo
                Mv = 65 if par == 0 else 128
                kbl = []
                for hk in range(2):
                    for jb in range(4 * Gq):
                        kbl.append((hk * 16 + jb, 0, None))
                for hk in range(2):
                    for d_ in range(2):
                        kbl.append((hk * 16 + 4 * Gq + d_, 0, (0, hk * 2 + d_)))
                for hk in range(2):
                    for d_ in range(2):
                        kbl.append((hk * 16 + 4 * Gq + 2 + d_, 256, (256, hk * 2 + d_)))
                nk = len(kbl)

                def issue_S(ki):
                    kb, qlo, msk = kbl[ki]
                    b_ = 5 + (ki % 2)
                    MM(bank(b_, qlo, 512), KTs[par][:, kb * 128:(kb + 1) * 128], QT_[:, qlo:512], True, msk is None, [qk] + kalls[par], ["B%d" % b_])
                    if msk is not None:
                        mlo, kbq = msk
                        MM(bank(b_, mlo, mlo + 256), ident_b[:], maskT[:, kbq, :], False, True, ["ident_b", "maskT"], ["B%d" % b_])

                issue_S(0)
                issue_S(1)
                for ki, (kb, qlo, msk) in enumerate(kbl):
                    b_ = 5 + (ki % 2)
                    pt = PT[ki % 3]
                    ptk = ("PT", ki % 3)
                    A(lambda e, b_=b_, qlo=qlo, pt=pt: e.activation(out=pt[:, qlo:512], in_=bank(b_, qlo, 512), func=AF.Exp, scale=ATT_SCALE), ["B%d" % b_], [ptk])
                    if ki + 2 < nk:
                        issue_S(ki + 2)
                    if qlo == 0:
                        MM(bank(bo, 0, 512, 0, Mv), VAs[par][:, kb, 0:Mv], pt[:, 0:512], ki == 0, ki == nk - 1, [("VA", par), ptk], [bok], sig=True)
                    else:
                        MM(bank(bo, 256, 512, 0, Mv), VAs[par][:, kb, 0:Mv], pt[:, 256:512], False, ki == nk - 1, [("VA", par), ptk], [bok], sig=True)
                    for fn in hooks.get(ki, ()):
                        fn()

            units = [(h, Gq) for h in range(8) for Gq in range(4)]
            prev = None
            for u, (h, Gq) in enumerate(units):
                hooks = {}
                if u == 0:
                    prologue(0)
                    qbuild(0, 0, 0)
                if h + 1 < 8 and Gq == 2:
                    hooks.setdefault(7, []).append(lambda h=h: pro_pads(h + 1))
                    for c in range(8):
                        hooks.setdefault(8 + c, []).append(lambda h=h, c=c: pro_k(h + 1, c))
                if h + 1 < 8 and Gq == 3:
                    for q4 in range(4):
                        hooks.setdefault(8 + 4 * q4, []).append(lambda h=h, q4=q4: pro_v(h + 1, q4))
                if u + 1 < len(units):
                    nh, nG = units[u + 1]
                    hooks.setdefault(0, []).append(lambda nh=nh, nG=nG, u=u: qbuild_a(nh, nG, u + 1))
                    hooks.setdefault(4, []).append(lambda nh=nh, nG=nG, u=u: qbuild_b(nh, nG, u + 1))
                if prev is not None:
                    ph, pg, pu = prev
                    hooks.setdefault(1, []).append(lambda ph=ph, pg=pg, pu=pu: tail_a(ph, pg, pu))
                    hooks.setdefault(6, []).append(lambda ph=ph, pg=pg, pu=pu: tail_b(ph, pg, pu))
                pass2(h, Gq, u, hooks)
                prev = (h, Gq, u)
            tail(*prev)
            V(lambda e: e.reduce_sum(out=ssq[:], in_=bank(4, 0, 128).rearrange("p (h c) -> p c h", h=8), axis=AX.X), ["B4s"], ["ssq"])
            p.barrier()
            if dbg_d is not None:
                D("sync", dbg_d, R2[:, :], [], ["dbg"])
                p.barrier()

            if kind == "m":
                D("sync", wr_f[:], moe_w_r_d[idx_m].rearrange("(c p) e -> p c e", p=128), [], ["wr_f"])
                V(lambda e: e.tensor_copy(out=wr_h[:], in_=wr_f[:]), ["wr_f"], ["wr_h"])
                V(lambda e: e.tensor_tensor(out=wr_l[:], in0=wr_f[:], in1=wr_h[:], op=ALU.subtract), ["wr_f", "wr_h"], ["wr_l"])
            LN4t = [TA[:], R1[:, 16384:18432].bitcast(F32)]
            LN4k = ["TA", "TA1"]
            xlop = [xTt0[:], R1[:, 18432:19456].rearrange("p (c t) -> p c t", c=8)]
            def a4_stage(i, which):
                xk = ("X", i)
                pi = i % 2
                y0 = 2 if pi == 0 else 6
                smc = 11 + 32 * pi
                smk = "sm_%d" % pi
                if which == 1:
                    A(lambda e, i=i, smc=smc: e.activation(out=sm[:, smc:smc + 1], in_=ssq[:, i:i + 1], func=AF.Sqrt, bias=EPS, scale=1.0 / 512.0), ["ssq"], [smk])
                    V(lambda e, smc=smc: e.reciprocal(out=sm[:, smc:smc + 1], in_=sm[:, smc:smc + 1]), [smk], [smk])
                    for hh in range(2):
                        for c in range(4):
                            MM(bank(y0 + hh), MT[:, c, i * 128:(i + 1) * 128], WOM[:, c, hh * 512:(hh + 1) * 512], c == 0, c == 3, [("MT", i // 4), "WB0"], ["B%d" % (y0 + hh)])
                    if stop_after != "mixer_a":
                        V(lambda e, i=i, y0=y0, smc=smc: e.scalar_tensor_tensor(out=X[:, i, :], in0=ps[:, y0 * 512:(y0 + 2) * 512], scalar=sm[:, smc:smc + 1], in1=X[:, i, :], op0=ALU.mult, op1=ALU.add),
                          ["B%d" % y0, "B%d" % (y0 + 1), smk, xk], [xk])
                elif which == 2:
                    layernorm_block(i, X[:, i, :], LNG, LNB, "LN", tmp=LN4t[pi], tk=LN4k[pi], pi=pi)
                else:
                    transpose_block(i, BT[:, :, i * 128:(i + 1) * 128], ("BT", i), also_f32=None)
                    A(lambda e, i=i: e.activation(out=X[:, i, :], in_=X[:, i, :], func=AF.Copy, scale=ALPHA), [xk], [xk])
                    if kind == "m":
                        xlo = xlop[pi]
                        V(lambda e, i=i, xlo=xlo: e.tensor_tensor(out=xlo, in0=ps[:, 0:1024].rearrange("p (c t) -> p c t", c=8), in1=BT[:, :, i * 128:(i + 1) * 128], op=ALU.subtract),
                          ["B0", "B1", ("BT", i)], [("xTt", pi)])
                        for pi_, (lt, lk, wt, wk) in enumerate([("hi", None, wr_h, "wr_h"), ("lo", None, wr_h, "wr_h"), ("hi", None, wr_l, "wr_l")]):
                            for kc in range(8):
                                lhs = BT[:, kc, i * 128:(i + 1) * 128] if lt == "hi" else xlo[:, kc, :]
                                lkey = ("BT", i) if lt == "hi" else ("xTt", pi)
                                MM(bank(4, 0, 8), lhs, wt[:, kc, :], (pi_ == 0 and kc == 0), (pi_ == 2 and kc == 7), [lkey, wk], ["B4"])
                        V(lambda e: e.tensor_copy(out=lgt[:, 0, :], in_=bank(4, 0, 8)), ["B4"], ["lg0"])
                        V(lambda e: e.reduce_max(out=sm[:, 20:21], in_=lgt[:, 0, :], axis=AX.X), ["lg0"], ["m1"])
                        V(lambda e: e.tensor_scalar(out=lgt[:, 1, :], in0=lgt[:, 0, :], scalar1=sm[:, 20:21], scalar2=None, op0=ALU.is_ge), ["lg0", "m1"], ["lg1"])
                        V(lambda e: e.scalar_tensor_tensor(out=lgt[:, 2, :], in0=lgt[:, 1, :], scalar=-1e30, in1=lgt[:, 0, :], op0=ALU.mult, op1=ALU.add), ["lg1", "lg0"], ["lg2"])
                        V(lambda e: e.reduce_max(out=sm[:, 21:22], in_=lgt[:, 2, :], axis=AX.X), ["lg2"], ["m2"])
                        V(lambda e: e.tensor_scalar(out=lgt[:, 1, :], in0=lgt[:, 0, :], scalar1=sm[:, 21:22], scalar2=None, op0=ALU.is_ge), ["lg0", "m2", "lg2"], ["lg1b"])
                        V(lambda e: e.tensor_scalar(out=sm[:, 22:23], in0=sm[:, 20:21], scalar1=-1.0, scalar2=None, op0=ALU.mult), ["m1"], ["nm1"])
                        A(lambda e: e.activation(out=lgt[:, 3, :], in_=lgt[:, 0, :], func=AF.Exp, bias=sm[:, 22:23], scale=1.0), ["lg0", "nm1"], ["lg3"])
                        V(lambda e: e.tensor_tensor(out=lgt[:, 3, :], in0=lgt[:, 3, :], in1=lgt[:, 1, :], op=ALU.mult), ["lg3", "lg1b"], ["lg3b"])
                        V(lambda e: e.reduce_sum(out=sm[:, 23:24], in_=lgt[:, 3, :], axis=AX.X), ["lg3b"], ["sw"])
                        V(lambda e: e.reciprocal(out=sm[:, 23:24], in_=sm[:, 23:24]), ["sw"], ["sw"])
                        V(lambda e, i=i: e.tensor_scalar(out=comb[:, i, :], in0=lgt[:, 3, :], scalar1=sm[:, 23:24], scalar2=None, op0=ALU.mult), ["lg3b", "sw"], [("comb", i)])


            if stop_after in ("mixer_a", "mixer_raw"):
                for i in range(NB):
                    a4_stage(i, 1)
            elif stop_after == "mixer":
                for i in range(NB):
                    a4_stage(i, 1)
                    a4_stage(i, 2)
            else:
                for it in range(NB + 1):
                    if it < NB:
                        a4_stage(it, 1)
                    if it >= 1:
                        a4_stage(it - 1, 2)
                        a4_stage(it - 1, 3)

            if stop_after not in ("mixer", "mixer_a", "mixer_raw"):
                p.barrier()
                stages = []
                if kind == "d":
                    f0 = 0
                    while f0 < 2816:
                        fw = min(512, 2816 - f0)
                        stages.append((None, f0, fw))
                        f0 += fw
                else:
                    for e_ in range(8):
                        for f0 in range(0, 3584, 512):
                            stages.append((e_, f0, 512))

                def load_stage(si_):
                    e_, f0, fw = stages[si_]
                    slot = WB[si_ % 2]
                    sk = "WB%d" % (si_ % 2)
                    W1s = slot[:, 0:4096].rearrange("p (c f) -> p c f", c=8)
                    W3s = slot[:, 4096:8192].rearrange("p (c f) -> p c f", c=8)
                    W2s = slot[:, 8192:12288].rearrange("p (c f) -> p c f", c=4)
                    if e_ is None:
                        s1 = ffn_w1_d[idx_d]; s3 = ffn_w3_d[idx_d]; s2 = ffn_w2_d[idx_d]
                    else:
                        s1 = moe_w1_d[idx_m, e_]; s3 = moe_w3_d[idx_m, e_]; s2 = moe_w2_d[idx_m, e_]
                    for hh in range(2):
                        D("gpsimd", W1s[:, hh * 4:(hh + 1) * 4, 0:fw], s1[hh * 512:(hh + 1) * 512, f0:f0 + fw].rearrange("(c p) f -> p c f", p=128), [], [sk])
                        D("gpsimd", W3s[:, hh * 4:(hh + 1) * 4, 0:fw], s3[hh * 512:(hh + 1) * 512, f0:f0 + fw].rearrange("(c p) f -> p c f", p=128), [], [sk])
                    D("gpsimd", W2s[:, 0:fw // 128, :], s2[f0:f0 + fw, :].rearrange("(c p) d -> p c d", p=128), [], [sk])

                load_stage(0)
                for si_ in range(len(stages)):
                    if si_ + 1 < len(stages):
                        load_stage(si_ + 1)
                    if si_ == len(stages) - 1:
                        assert si_ % 2 == 1
                        D("sync", LNG[:], ln2_g_d[l:l + 1, :].partition_broadcast(128), [], ["LN"])
                        D("sync", LNB[:], ln2_b_d[l:l + 1, :].partition_broadcast(128), [], ["LN"])
                        WGp = WB[0][:, 0:8192].rearrange("p (c f) -> p c f", c=8)
                        WPp = WB[0][:, 8192:10240].rearrange("p (c f) -> p c f", c=2)
                        for hh in range(2):
                            D("gpsimd", WGp[:, hh * 4:(hh + 1) * 4, :], ple_w_g_d[l, hh * 512:(hh + 1) * 512, :].rearrange("(c p) f -> p c f", p=128), [], ["WB0"])
                        D("gpsimd", WPp, ple_w_p_d[l].rearrange("(c p) f -> p c f", p=128), [], ["WB0"])
                    e_, f0, fw = stages[si_]
                    nfc = fw // 128
                    slot = WB[si_ % 2]
                    sk = "WB%d" % (si_ % 2)
                    W1s = slot[:, 0:4096].rearrange("p (c f) -> p c f", c=8)
                    W3s = slot[:, 4096:8192].rearrange("p (c f) -> p c f", c=8)
                    W2s = slot[:, 8192:12288].rearrange("p (c f) -> p c f", c=4)
                    def ffn_up(tg):
                        btk = [("BT", tg * 4 + t_) for t_ in range(4)]
                        for fc in range(nfc):
                            u_ = (tg * nfc + fc) % 2
                            b1, b3 = 2 * u_, 2 * u_ + 1
                            for kc in range(8):
                                MM(bank(b1), W1s[:, kc, fc * 128:(fc + 1) * 128], BT[:, kc, tg * 512:(tg + 1) * 512], kc == 0, kc == 7, [sk] + btk, ["B%d" % b1])
                            for kc in range(8):
                                MM(bank(b3), W3s[:, kc, fc * 128:(fc + 1) * 128], BT[:, kc, tg * 512:(tg + 1) * 512], kc == 0, kc == 7, [sk] + btk, ["B%d" % b3])
                            stl = silu_t[u_]
                            A(lambda e, stl=stl, b1=b1: e.activation(out=stl[:], in_=bank(b1), func=AF.Silu), ["B%d" % b1], [("silu", u_)])
                            V(lambda e, stl=stl, b3=b3, fc=fc, tg=tg: e.tensor_tensor(out=GT[:, fc, tg * 512:(tg + 1) * 512], in0=stl[:], in1=bank(b3), op=ALU.mult),
                              [("silu", u_), "B%d" % b3], [("GT", tg)])

                    def ffn_down(tg):
                        for t_ in range(4):
                            tb = tg * 4 + t_
                            bo = 4 + 2 * (tb % 2)
                            for hh in range(2):
                                for fc in range(nfc):
                                    MM(bank(bo + hh), GT[:, fc, tb * 128:(tb + 1) * 128], W2s[:, fc, hh * 512:(hh + 1) * 512], fc == 0, fc == nfc - 1,
                                       [("GT", tg), sk], ["B%d" % (bo + hh)])
                            xk = ("X", tb)
                            if e_ is None:
                                V(lambda e, tb=tb, bo=bo: e.tensor_tensor(out=X[:, tb, :], in0=X[:, tb, :], in1=ps[:, bo * 512:(bo + 2) * 512], op=ALU.add),
                                  [xk, "B%d" % bo, "B%d" % (bo + 1)], [xk])
                            else:
                                V(lambda e, tb=tb, bo=bo, e_=e_: e.scalar_tensor_tensor(out=X[:, tb, :], in0=ps[:, bo * 512:(bo + 2) * 512], scalar=comb[:, tb, e_:e_ + 1],
                                                                                        in1=X[:, tb, :], op0=ALU.mult, op1=ALU.add),
                                  [xk, "B%d" % bo, "B%d" % (bo + 1), ("comb", tb)], [xk])

                    ffn_up(0)
                    for tg in range(4):
                        if tg + 1 < 4:
                            ffn_up(tg + 1)
                        ffn_down(tg)
                p.barrier()

                WG = WB[0][:, 0:8192].rearrange("p (c f) -> p c f", c=8)
                WP = WB[0][:, 8192:10240].rearrange("p (c f) -> p c f", c=2)
                LNt = [TA[:], R1[:, 0:2048].bitcast(F32)]
                LNk = ["TA", "TA1"]
                GTt = [R1[:, 2048:4096].bitcast(F32), R1[:, 4096:6144].bitcast(F32)]
                xTp = [xTt0[:], R1[:, 6144:7168].rearrange("p (c t) -> p c t", c=8)]
                pbp = [pblk[:], R1[:, 7168:7424]]
                pTp = [pTt[:], R1[:, 7424:7680].rearrange("p (c t) -> p c t", c=2)]
                def ple_stage(i, which):
                    xk = ("X", i)
                    pi = i % 2
                    xt = xTp[pi]
                    xtk = ("xTt", pi)
                    gt_ = GTt[pi]
                    gk = "GTt%d" % pi
                    pb_ = pbp[pi]
                    pT_ = pTp[pi]
                    g0 = 2 if pi == 0 else 6
                    if which == 1:
                        layernorm_block(i, X[:, i, :], LNG, LNB, "LN", tmp=LNt[pi], tk=LNk[pi], pi=pi)
                        D("gpsimd", pb_, p_d[l, i], [], ["pblk%d" % pi])
                    elif which == 2:
                        transpose_block(i, xt, xtk)
                        for c in range(2):
                            TR(bankb(4, c * 128, (c + 1) * 128), pb_[:, c * 128:(c + 1) * 128], ident_b[:], ["pblk%d" % pi, "ident_b"], ["B4"], sig=(c == 1))
                        V(lambda e, pT_=pT_: e.tensor_copy(out=pT_, in_=bankb(4, 0, 256).rearrange("p (c t) -> p c t", c=2)), ["B4"], ["pTt%d" % pi])
                        for hh in range(2):
                            for kc in range(8):
                                MM(bank(g0 + hh), xt[:, kc, :], WG[:, kc, hh * 512:(hh + 1) * 512], kc == 0, False, [xtk, "WB0"], ["B%d" % (g0 + hh)], sig=False)
                            MM(bank(g0 + hh), ones_b[0:1, 0:128], bgr[0:1, hh * 512:(hh + 1) * 512], False, True, ["ones_b", "bgr"], ["B%d" % (g0 + hh)], sig=True)
                        for hh in range(2):
                            for c in range(2):
                                MM(bank(4 + hh), pT_[:, c, :], WP[:, c, hh * 512:(hh + 1) * 512], c == 0, c == 1, ["pTt%d" % pi, "WB0"], ["B%d" % (4 + hh)])
                    else:
                        A(lambda e, gt_=gt_, g0=g0: e.activation(out=gt_, in_=ps[:, g0 * 512:(g0 + 2) * 512], func=AF.Sigmoid), ["B%d" % g0, "B%d" % (g0 + 1)], [gk])
                        V(lambda e, gt_=gt_: e.tensor_tensor(out=gt_, in0=gt_, in1=ps[:, 4 * 512:6 * 512], op=ALU.mult), [gk, "B4", "B5"], [gk])
                        G(lambda e, i=i, gt_=gt_: e.tensor_tensor(out=X[:, i, :], in0=X[:, i, :], in1=gt_, op=ALU.add), [gk, xk], [xk])


                if stop_after == "ffn":
                    for i in range(NB):
                        layernorm_block(i, X[:, i, :], LNG, LNB, "LN", tmp=LNt[i % 2], tk=LNk[i % 2], pi=i % 2)
                else:
                    for it in range(NB + 1):
                        if it < NB:
                            ple_stage(it, 1)
                        if it >= 1:
                            ple_stage(it - 1, 2)
                            ple_stage(it - 1, 3)
                p.barrier()
            if kind == "d":
                idx_d += 1
            else:
                idx_m += 1

        outk = []
        for jb in range(4):
            t_ = D("sync", y_d[jb * 4:(jb + 1) * 4].rearrange("j p d -> p j d"), X[:, jb * 4:(jb + 1) * 4, :],
                   [("X", j) for j in range(jb * 4, jb * 4 + 4)], [("y", jb)])
            outk.append(t_)
        p.wait_all("sync", outk)
        p.emit(st)
    return nc


_W_LAYER = ["w_in", "sg_v_g", "sg_v_b", "sg_w_s", "sg_b_s", "q_norm_g", "kv_norm_g", "w_uq", "w_ukv", "out_g", "w_o",
            "ln1_g", "ln1_b", "ln2_g", "ln2_b", "ple_w_g", "ple_b_g", "ple_w_p"]
_W_DENSE = ["ffn_w1", "ffn_w3", "ffn_w2"]
_W_MOE = ["moe_w_r", "moe_w1", "moe_w3", "moe_w2"]


def _gb(hf, j):
    return 4 * (j // 2) + GMAP[hf][j % 2]


def _consts(hf):
    inv_freq = 1.0 / (10000.0 ** (np.arange(0, 32, 2, dtype=np.float32) / 32.0))
    invf = (np.concatenate([inv_freq, inv_freq]) / (2 * np.pi)).astype(np.float32).reshape(32, 1)
    sgn = np.concatenate([-np.ones(16, np.float32), np.ones(16, np.float32)]).reshape(32, 1)
    ident = np.eye(128, dtype=np.float32)
    tril = np.tril(np.ones((128, 128), np.float32))
    m = np.full((128, 4, 2, 128), -30000.0, np.float32)
    ii = np.arange(128)
    diag = (ii[:, None] <= ii[None, :]).astype(np.float32)
    for r in range(2):
        qg = GMAP[hf][r]
        for kb in range(4):
            kg = GMAP[kb // 2][kb % 2]
            if kg < qg:
                m[:, kb, r, :] = 0.0
            elif kg == qg:
                m[:, kb, r, :] = (diag - 1.0) * 30000.0
    negs = np.zeros((128, 66), np.float32)
    negs[:, 32 + 33 * hf] = -1.0
    return {"invf": invf, "sgn": sgn, "ident": ident, "tril": tril, "maskT": m.reshape(128, 1024), "negs": negs}


def _run(kinds, layer_ids, xin, inputs, stop_after=None):
    nc = build(kinds, stop_after=stop_after)
    p = inputs["p"]
    positions = inputs["positions"]
    d_ids = [li // 2 for li in layer_ids if li % 2 == 0]
    m_ids = [li // 2 for li in layer_ids if li % 2 == 1]
    shared = {}
    for k in _W_LAYER:
        a = np.ascontiguousarray(inputs[k][layer_ids])
        if k == "sg_b_s":
            a = a.reshape(len(layer_ids), 1024)
        shared[k] = a
    if d_ids:
        for k in _W_DENSE:
            shared[k] = np.ascontiguousarray(inputs[k][d_ids])
    if m_ids:
        for k in _W_MOE:
            shared[k] = np.ascontiguousarray(inputs[k][m_ids])
    maps = []
    for c in range(8):
        b, hf = c // 2, c % 2
        blks = [_gb(hf, j) for j in range(NB)]
        pc = np.stack([np.stack([p[li, b, g * 128:(g + 1) * 128, :] for g in blks]) for li in layer_ids])
        pos = np.concatenate([positions[b, g * 128:(g + 1) * 128] for g in blks]).astype(np.int32).reshape(1, T)
        m = {"x": xin[c], "p": np.ascontiguousarray(pc), "pos": pos}
        m.update(_consts(hf))
        m.update(shared)
        maps.append(m)
    res = run_bass_kernel_spmd(nc, maps, core_ids=list(range(8)))
    global DBG
    DBG = [res.results[c].get("dbg") for c in range(8)]
    return [np.asarray(res.results[c]["y"]) for c in range(8)]


def _shard_x(x):
    out = []
    for c in range(8):
        b, hf = c // 2, c % 2
        out.append(np.ascontiguousarray(np.stack([x[b, _gb(hf, j) * 128:(_gb(hf, j) + 1) * 128, :] for j in range(NB)])))
    return out


def _unshard(ys, shape):
    out = np.zeros(shape, np.float32)
    for c in range(8):
        b, hf = c // 2, c % 2
        for j in range(NB):
            g = _gb(hf, j)
            out[b, g * 128:(g + 1) * 128, :] = ys[c][j]
    return out


FUSED = True
DBG = None


def kernel(**inputs):
    inputs = {k: np.asarray(v) for k, v in inputs.items()}
    x = inputs["x"].astype(np.float32)
    xs = _shard_x(x)
    if FUSED:
        ys = _run(["d", "m", "d", "m"], [0, 1, 2, 3], xs, inputs)
    else:
        ys = xs
        for li in range(4):
            ys = _run(["d" if li % 2 == 0 else "m"], [li], ys, inputs)
    return _unshard(ys, x.shape)
```

```python
import contextlib
import math
import numpy as np
import concourse.bass as bass
import concourse.mybir as mybir
from concourse.bass_utils import run_bass_kernel_spmd

F32 = mybir.dt.float32
BF16 = mybir.dt.bfloat16
I32 = mybir.dt.int32
AF = mybir.ActivationFunctionType
ALU = mybir.AluOpType
AX = mybir.AxisListType

ENGS = ["sync", "scalar", "vector", "gpsimd", "tensor"]
NDMASEM = 40
GMAP = [[0, 3], [1, 2]]
ALPHA = (2.0 * 4) ** 0.25
EPS = 1e-6
ATT_SCALE = 96 ** -0.5
NB = 16
T = 2048


class Prog:
    def __init__(self, nc):
        self.nc = nc
        self.q = {e: [] for e in ENGS}
        self.cnt = {e: 0 for e in ENGS}
        self.seen = {e: {} for e in ENGS}
        self.lastw = {}
        self.readers = {}
        self.dma_use = [0] * NDMASEM
        self.dma_rr = 0
        self.pending_nosig = {e: False for e in ENGS}

    def _need(self, eng, tickets):
        out = []
        best = {}
        for t in tickets:
            if t is None:
                continue
            k, v = t
            if v > best.get(k, 0):
                best[k] = v
        for k, v in best.items():
            if self.seen[eng].get(k, 0) >= v:
                continue
            self.seen[eng][k] = v
            out.append((k, v))
        return out

    def _rtix(self, x):
        d = self.lastw.get(x)
        return list(d.items()) if d else []

    def _deps(self, r, w):
        ts = []
        for x in r:
            ts.extend(self._rtix(x))
        for x in w:
            ts.extend(self._rtix(x))
            ts.extend(self.readers.get(x, ()))
        return ts

    def _commit(self, ticket, r, w):
        k, v = ticket
        for x in w:
            d = self.lastw.setdefault(x, {})
            if d.get(k, 0) < v:
                d[k] = v
            self.readers[x] = []
        for x in r:
            self.readers.setdefault(x, []).append(ticket)

    def op(self, eng, fn, r=(), w=(), sig=True, pe_acc=False):
        ts = self._deps(r, w)
        if pe_acc:
            ts = [t for t in ts if t[0] != eng]
        else:
            raw = set()
            for x in r:
                raw.update(self._rtix(x))
            ts = [t for t in ts if (t[0] != eng or t in raw)]
        waits = self._need(eng, ts)
        ticket = (eng, self.cnt[eng] + 1)
        if sig:
            self.cnt[eng] += 1
            self.pending_nosig[eng] = False
        else:
            self.pending_nosig[eng] = True
        self.q[eng].append((waits, fn, ("e", eng) if sig else None))
        self._commit(ticket, r, w)
        return ticket

    def dma(self, eng, fn, r=(), w=(), inc=16):
        ts = self._deps(r, w)
        idx = self.dma_rr
        self.dma_rr = (self.dma_rr + 1) % NDMASEM
        use = self.dma_use[idx]
        if use > 0:
            ts.append((("d", idx), use))
        self.dma_use[idx] = use + inc
        ticket = (("d", idx), use + inc)
        waits = self._need(eng, ts)
        self.q[eng].append((waits, fn, ("d", idx, inc)))
        self._commit(ticket, r, w)
        return ticket

    def wait_all(self, eng, tickets):
        waits = self._need(eng, tickets)
        if waits:
            self.q[eng].append((waits, None, None))

    def barrier(self):
        for e in ENGS:
            assert not self.pending_nosig[e]
        ts = [(e, self.cnt[e]) for e in ENGS if self.cnt[e] > 0]
        ts += [(("d", i), self.dma_use[i]) for i in range(NDMASEM) if self.dma_use[i] > 0]
        for e in ENGS:
            self.wait_all(e, ts)

    def emit(self, stack):
        nc = self.nc
        for e in ENGS:
            assert not self.pending_nosig[e], e
        esem = {e: stack.enter_context(nc.semaphore("se_" + e)) for e in ENGS}
        dsem = [stack.enter_context(nc.semaphore("sd_%d" % i)) for i in range(NDMASEM)]

        def semof(k):
            return esem[k] if isinstance(k, str) else dsem[k[1]]

        block = stack.enter_context(nc.Block())

        def mk(ename):
            def body(engine):
                for waits, fn, sg in self.q[ename]:
                    for k, v in waits:
                        engine.wait_ge(semof(k), v)
                    if fn is None:
                        continue
                    ins = fn(engine)
                    if sg is None:
                        continue
                    if sg[0] == "e":
                        ins.then_inc(esem[sg[1]], 1)
                    else:
                        ins.then_inc(dsem[sg[1]], sg[2])
            return body

        for e in ENGS:
            if self.q[e]:
                getattr(block, e)(mk(e))


def build(kinds, stop_after=None):
    nl = len(kinds)
    nd = kinds.count("d")
    nm = kinds.count("m")
    nc = bass.Bass("TRN2", target_bir_lowering=False)

    def DI(name, shape, dt=F32):
        return nc.dram_tensor(name, shape, dt, kind="ExternalInput").ap()

    x_d = DI("x", [NB, 128, 1024])
    p_d = DI("p", [nl, NB, 128, 256])
    pos_d = DI("pos", [1, T], I32)
    invf_d = DI("invf", [32, 1])
    sgn_d = DI("sgn", [32, 1])
    ident_d = DI("ident", [128, 128])
    tril_d = DI("tril", [128, 128])
    mask_d = DI("maskT", [128, 1024])
    negs_d = DI("negs", [128, 66])
    w_in_d = DI("w_in", [nl, 1024, 1440])
    sg_v_g_d = DI("sg_v_g", [nl, 512])
    sg_v_b_d = DI("sg_v_b", [nl, 512])
    sg_w_s_d = DI("sg_w_s", [nl, 8, 128, 128])
    sg_b_s_d = DI("sg_b_s", [nl, 1024])
    q_norm_g_d = DI("q_norm_g", [nl, 256])
    kv_norm_g_d = DI("kv_norm_g", [nl, 128])
    w_uq_d = DI("w_uq", [nl, 256, 768])
    w_ukv_d = DI("w_ukv", [nl, 128, 1024])
    out_g_d = DI("out_g", [nl, 1024])
    w_o_d = DI("w_o", [nl, 1024, 1024])
    ln1_g_d = DI("ln1_g", [nl, 1024])
    ln1_b_d = DI("ln1_b", [nl, 1024])
    ln2_g_d = DI("ln2_g", [nl, 1024])
    ln2_b_d = DI("ln2_b", [nl, 1024])
    ple_w_g_d = DI("ple_w_g", [nl, 1024, 1024])
    ple_b_g_d = DI("ple_b_g", [nl, 1024])
    ple_w_p_d = DI("ple_w_p", [nl, 256, 1024])
    if nd:
        ffn_w1_d = DI("ffn_w1", [nd, 1024, 2816])
        ffn_w3_d = DI("ffn_w3", [nd, 1024, 2816])
        ffn_w2_d = DI("ffn_w2", [nd, 2816, 1024])
    if nm:
        moe_w_r_d = DI("moe_w_r", [nm, 1024, 8])
        moe_w1_d = DI("moe_w1", [nm, 8, 1024, 3584])
        moe_w3_d = DI("moe_w3", [nm, 8, 1024, 3584])
        moe_w2_d = DI("moe_w2", [nm, 8, 3584, 1024])
    y_d = nc.dram_tensor("y", [NB, 128, 1024], F32, kind="ExternalOutput").ap()
    dbg_d = nc.dram_tensor("dbg", [128, 8192], BF16, kind="ExternalOutput").ap() if stop_after == "mixer_raw" else None
    ag_in = [nc.dram_tensor("ag_in%d" % l, [160, T], BF16).ap() for l in range(nl)]
    ag_out = [nc.dram_tensor("ag_out%d" % l, [320, T], BF16).ap() for l in range(nl)]

    st = contextlib.ExitStack()
    with st:
        def SB(name, shape, dt=F32):
            return st.enter_context(nc.sbuf_tensor(name, shape, dt))

        ps = st.enter_context(nc.psum_tensor("ps", [128, 4096], F32))

        def bank(b, lo=0, hi=512, p0=0, p1=128):
            return ps[p0:p1, b * 512 + lo:b * 512 + hi]

        def bankb(b, lo=0, hi=512, p0=0, p1=128):
            v = ps[p0:p1, b * 512:(b + 1) * 512].bitcast(BF16)
            return v[:, lo:hi]

        X = SB("X", [128, NB, 1024])
        WB = [SB("WB0", [128, 12288], BF16), SB("WB1", [128, 12288], BF16)]
        R1 = SB("R1", [128, 20480], BF16)
        R2 = SB("R2", [128, 8192], BF16)
        CS = SB("CS", [32, 2, T], BF16)
        LNG = SB("LNG", [128, 1024]); LNB = SB("LNB", [128, 1024])
        TA = SB("TA", [128, 1024])
        ident_f = SB("ident_f", [128, 128]); ident_b = SB("ident_b", [128, 128], BF16)
        tril_f = SB("tril_f", [128, 128])
        maskT = SB("maskTs", [128, 4, 256], BF16)
        ones_b = SB("ones_b", [128, 128], BF16); ones_f = SB("ones_f", [128, 128])
        NEGS = SB("NEGS", [128, 66], BF16)
        OG = SB("OG", [128, 8]); QG = SB("QG", [128, 2]); KG = SB("KG", [128, 1])
        invf = SB("invf_s", [32, 1]); sgn = SB("sgn_s", [32, 1])
        bsr = SB("bsr", [1, 1024], BF16); bgr = SB("bgr", [1, 1024], BF16)
        wsT = SB("wsT", [128, 8, 128], BF16)
        WKpad = SB("WKpad", [128, 128], BF16)
        WQpad = SB("WQpad", [128, 2, 128], BF16); WQsw = SB("WQsw", [128, 2, 32], BF16)
        WLsw = SB("WLsw", [128, 8, 32], BF16)
        xTt0 = SB("xTt0", [128, 8, 128], BF16)
        xTt = [xTt0, xTt0]
        sm = SB("sm", [128, 64])
        mstat = SB("mstat", [128, 16])
        Mst = SB("Mst", [128, 33], BF16)
        comb = SB("comb", [128, NB, 8])
        ssq = SB("ssq", [128, NB])
        wr_f = SB("wr_f", [128, 8, 8])
        wr_h = SB("wr_h", [128, 8, 8], BF16); wr_l = SB("wr_l", [128, 8, 8], BF16)
        lgt = SB("lgt", [128, 4, 8])
        stt = SB("stt", [128, 4, 6])
        aTt = SB("aTt", [128, 4, 128], BF16)
        cqn = SB("cqn", [128, 384], BF16)
        pblk = SB("pblk", [128, 256], BF16); pTt = SB("pTt", [128, 2, 128], BF16)
        krt = SB("krt", [32, 256])

        CKV = R1[:, 0:4096]
        CQT = R1[:, 4096:8192].rearrange("p (c t) -> p c t", c=2)
        KT = R1[:, 8192:12288]
        VA = R1[:, 12288:16384].rearrange("p (k c) -> p k c", k=32)
        QTg = [R1[:, 16384:16896], R1[:, 16896:17408]]
        PT = [R1[:, 17408:17920], R1[:, 17920:18432], R1[:, 19968:20480]]
        LAT = R1[:, 12288:14336]
        KRl = R1[0:32, 14336:16384]
        sqb = R1[:, 18432:18944]
        BT = R1[:, 0:16384].rearrange("p (c t) -> p c t", c=8)
        MT = R2[:, :].rearrange("p (c t) -> p c t", c=4)
        GT = R2[:, :].rearrange("p (c t) -> p c t", c=4)
        TBr = WB[1][:, 0:2048].bitcast(F32)
        bcs = WB[1][:, 2048:3072].bitcast(F32)
        rl = WB[1][:, 3072:4096].bitcast(F32)
        silu_t = [TA[:, 0:512], TA[:, 512:1024]]
        SGG = R2[:, 0:1024].bitcast(F32)
        SGB = R2[:, 1024:2048].bitcast(F32)
        abf = R1[:, 18944:19456]
        vnb = R1[:, 19456:19968]
        junk = R1[:, 19968:20480]

        p = Prog(nc)

        def V(fn, r, w, **k):
            return p.op("vector", fn, r=r, w=w, **k)

        def A(fn, r, w, **k):
            return p.op("scalar", fn, r=r, w=w, **k)

        def G(fn, r, w, **k):
            return p.op("gpsimd", fn, r=r, w=w, **k)

        def MM(out, lhsT, rhs, start, stop, r, w, sig=None):
            if sig is None:
                sig = stop
            return p.op("tensor", lambda e: e.matmul(out, lhsT=lhsT, rhs=rhs, start=start, stop=stop),
                        r=r, w=w, sig=sig, pe_acc=not start)

        def TR(out, in_, idt, r, w, sig=True):
            return p.op("tensor", lambda e: e.transpose(out, in_, idt), r=r, w=w, sig=sig)

        def D(eng, out, in_, r, w, **kw):
            return p.dma(eng, lambda e: e.dma_start(out=out, in_=in_, **kw), r=r, w=w)

        D("sync", ident_f[:], ident_d, [], ["ident_f"])
        D("gpsimd", ident_b[:], ident_d, [], ["ident_b"])
        D("sync", tril_f[:], tril_d, [], ["tril_f"])
        D("gpsimd", maskT[:].rearrange("p a b -> p (a b)"), mask_d, [], ["maskT"])
        D("sync", invf[:], invf_d, [], ["invf"])
        D("gpsimd", NEGS[:], negs_d, [], ["NEGS"])
        D("sync", sgn[:], sgn_d, [], ["sgn"])
        G(lambda e: e.memset(ones_b[:], 1.0), [], ["ones_b"])
        G(lambda e: e.memset(ones_f[:], 1.0), [], ["ones_f"])
        G(lambda e: e.memset(WKpad[:], 0.0), [], ["WKpad"])
        G(lambda e: e.memset(WQpad[:], 0.0), [], ["WQpad"])
        G(lambda e: e.memset(Mst[:], 0.0), [], ["Mst"])
        for jb in range(4):
            D("sync", X[:, jb * 4:(jb + 1) * 4, :], x_d[jb * 4:(jb + 1) * 4].rearrange("j p d -> p j d"),
              [], [("X", j) for j in range(jb * 4, jb * 4 + 4)])
        kf = R2[0:32, 0:2048].bitcast(F32)
        tt = R2[0:32, 2048:4096].bitcast(F32)
        uu = R2[0:32, 4096:6144].bitcast(F32)
        ti = TA[0:32, :].bitcast(I32)
        tf = R2[0:32, 6144:8192].bitcast(F32)
        SK = ["setup"]
        for hh in range(2):
            D("sync", ti, pos_d[:, hh * 1024:(hh + 1) * 1024].partition_broadcast(32), SK, SK)
            V(lambda e: e.tensor_copy(out=tf, in_=ti), SK + ["invf", "sgn"], SK)
            for which in range(2):
                dst = CS[:, which, hh * 1024:(hh + 1) * 1024]
                V(lambda e, which=which: e.tensor_scalar(out=uu, in0=tf, scalar1=invf[:, 0:1], scalar2=(0.25 if which == 0 else 0.0),
                                                           op0=ALU.mult, op1=ALU.add), SK, SK)
                V(lambda e: e.tensor_copy(out=ti, in_=uu), SK, SK)
                V(lambda e: e.tensor_copy(out=kf, in_=ti), SK, SK)
                V(lambda e: e.tensor_tensor(out=uu, in0=uu, in1=kf, op=ALU.subtract), SK, SK)
                V(lambda e: e.tensor_scalar(out=tt, in0=uu, scalar1=0.5, scalar2=None, op0=ALU.is_ge), SK, SK)
                V(lambda e: e.tensor_tensor(out=uu, in0=uu, in1=tt, op=ALU.subtract), SK, SK)
                A(lambda e: e.activation(out=uu, in_=uu, func=AF.Sin, scale=2 * math.pi), SK, SK)
                if which == 1:
                    V(lambda e, dst=dst: e.tensor_scalar(out=dst, in0=uu, scalar1=sgn[:, 0:1], scalar2=None, op0=ALU.mult), SK, SK)
                else:
                    V(lambda e, dst=dst: e.tensor_copy(out=dst, in_=uu), SK, SK)
        p.barrier()

        def rstd_from(ssum_ap, n, out_ap, key):
            A(lambda e: e.activation(out=out_ap, in_=ssum_ap, func=AF.Sqrt, bias=EPS, scale=1.0 / n), [key], [key])
            V(lambda e: e.reciprocal(out=out_ap, in_=out_ap), [key], [key])

        def layernorm_block(i, src, gt, bt, keyg, tmp=None, tk="TA", pi=0):
            xk = ("X", i)
            if tmp is None:
                tmp = TA[:]
            so = 32 * pi
            sk = "stt%d" % pi
            k1, k2, k3 = "lnmv%d" % pi, "lnr%d" % pi, "lnn%d" % pi
            V(lambda e: e.bn_stats(out=stt[:, 2 * pi, :], in_=src[:, 0:512]), [xk], [sk])
            V(lambda e: e.bn_stats(out=stt[:, 2 * pi + 1, :], in_=src[:, 512:1024]), [xk], [sk])
            V(lambda e: e.bn_aggr(out=sm[:, so:so + 2], in_=stt[:, 2 * pi:2 * pi + 2, :]), [sk], [k1])
            A(lambda e: e.activation(out=sm[:, so + 2:so + 3], in_=sm[:, so + 1:so + 2], func=AF.Sqrt, bias=EPS, scale=1.0), [k1], [k2])
            V(lambda e: e.reciprocal(out=sm[:, so + 2:so + 3], in_=sm[:, so + 2:so + 3]), [k2], [k2])
            V(lambda e: e.tensor_scalar(out=sm[:, so + 3:so + 4], in0=sm[:, so:so + 1], scalar1=-1.0, scalar2=sm[:, so + 2:so + 3], op0=ALU.mult, op1=ALU.mult),
              [k1, k2], [k3])
            A(lambda e: e.activation(out=tmp, in_=src, func=AF.Identity, bias=sm[:, so + 3:so + 4], scale=sm[:, so + 2:so + 3]), [xk, k2, k3], [tk])
            V(lambda e: e.tensor_tensor(out=tmp, in0=tmp, in1=gt[:], op=ALU.mult), [tk, keyg], [tk])
            G(lambda e: e.tensor_tensor(out=X[:, i, :], in0=tmp, in1=bt[:], op=ALU.add), [tk, keyg], [xk])

        def transpose_block(i, dst, dkey, also_f32=None):
            xk = ("X", i)
            for kc in range(8):
                TR(bank(kc // 4, (kc % 4) * 128, (kc % 4 + 1) * 128), X[:, i, kc * 128:(kc + 1) * 128], ident_f[:],
                   [xk, "ident_f"], ["B%d" % (kc // 4)], sig=(kc % 4 == 3))
            A(lambda e: e.activation(out=dst, in_=ps[:, 0:1024].rearrange("p (c t) -> p c t", c=8), func=AF.Copy), ["B0", "B1"], [dkey])
            if also_f32 is not None:
                V(lambda e: e.tensor_copy(out=also_f32, in_=ps[:, 0:1024]), ["B0", "B1"], ["WB1"])

        idx_d = 0
        idx_m = 0
        for l, kind in enumerate(kinds):
            last = (l == nl - 1)
            WLAT = WB[0][:, 0:3328].rearrange("p (c f) -> p c f", c=8)
            WUQ = WB[0][:, 3328:4864].rearrange("p (c f) -> p c f", c=2)
            WUKV = WB[0][:, 4864:5888]
            WOM = WB[0][:, 5888:9984].rearrange("p (c f) -> p c f", c=4)
            WUV = WB[1][:, 0:8192].rearrange("p (c f) -> p c f", c=8)
            WOA = WB[1][:, 8192:12288].rearrange("p (c f) -> p c f", c=4)
            D("gpsimd", WLAT, w_in_d[l, :, 1024:1440].rearrange("(c p) f -> p c f", p=128), [], ["WB0"])
            for hh in range(2):
                D("gpsimd", WUV[:, hh * 4:(hh + 1) * 4, :], w_in_d[l, hh * 512:(hh + 1) * 512, 0:1024].rearrange("(c p) f -> p c f", p=128), [], ["WB1"])
            D("sync", OG[:], out_g_d[l].rearrange("(c p) -> p c", p=128), [], ["OG"], allow_slow_non_contiguous=True)
            D("sync", QG[:], q_norm_g_d[l].rearrange("(c p) -> p c", p=128), [], ["QG"], allow_slow_non_contiguous=True)
            D("sync", KG[:], kv_norm_g_d[l].rearrange("(c p) -> p c", p=128), [], ["KG"], allow_slow_non_contiguous=True)
            D("sync", LNG[:], ln1_g_d[l:l + 1, :].partition_broadcast(128), [], ["LN"])
            D("sync", LNB[:], ln1_b_d[l:l + 1, :].partition_broadcast(128), [], ["LN"])
            D("sync", SGG[:], sg_v_g_d[l:l + 1, :].partition_broadcast(128), [], ["SG"])
            D("sync", SGB[:], sg_v_b_d[l:l + 1, :].partition_broadcast(128), [], ["SG"])
            D("gpsimd", bsr[:], sg_b_s_d[l:l + 1, :], [], ["bsr"])
            D("gpsimd", bgr[:], ple_b_g_d[l:l + 1, :], [], ["bgr"])
            stg = [TA, TA]
            si = 0

            def fold(dst, src_ap, ncols, gcol, gkey, wkey):
                nonlocal si
                t_ = stg[si % 2]
                tk = "TA"
                si += 1
                D("sync", t_[:, 0:ncols], src_ap, [], [tk])
                V(lambda e: e.tensor_scalar(out=dst, in0=t_[:, 0:ncols], scalar1=gcol, scalar2=None, op0=ALU.mult), [tk, gkey], [wkey])

            for c in range(2):
                fold(WUQ[:, c, :], w_uq_d[l, c * 128:(c + 1) * 128, :], 768, QG[:, c:c + 1], "QG", "WB0")
            fold(WUKV, w_ukv_d[l], 1024, KG[:, 0:1], "KG", "WB0")
            for c in range(4):
                fold(WOA[:, c, :], w_o_d[l, c * 128:(c + 1) * 128, :], 1024, OG[:, c:c + 1], "OG", "WB1")
            for c in range(4):
                fold(WOM[:, c, :], w_o_d[l, 512 + c * 128:512 + (c + 1) * 128, :], 1024, OG[:, 4 + c:5 + c], "OG", "WB0")
            wsn = TA[:].rearrange("p (g s) -> p g s", g=8)
            D("sync", wsn, sg_w_s_d[l].rearrange("g t s -> t g s"), [], ["TA"])
            V(lambda e: e.tensor_tensor(out=wsn, in0=wsn, in1=tril_f[:].unsqueeze(1).to_broadcast([128, 8, 128]), op=ALU.mult), ["TA", "tril_f"], ["TA"])
            for g in range(8):
                TR(bank(g // 4, (g % 4) * 128, (g % 4 + 1) * 128), wsn[:, g, :], ident_f[:], ["TA", "ident_f"], ["B%d" % (g // 4)], sig=(g % 4 == 3))
            A(lambda e: e.activation(out=wsT[:].rearrange("p g t -> p (g t)"), in_=ps[:, 0:1024], func=AF.Copy), ["B0", "B1"], ["wsT"])
            V(lambda e: e.tensor_copy(out=WLsw[:, :, 0:16], in_=WLAT[:, :, 400:416]), ["WB0"], ["WLsw"])
            V(lambda e: e.tensor_copy(out=WLsw[:, :, 16:32], in_=WLAT[:, :, 384:400]), ["WB0"], ["WLsw"])

            TAp = [TA[:], R1[:, 0:2048].bitcast(F32)]
            xTp2 = [xTt0[:], R1[:, 2048:3072].rearrange("p (c t) -> p c t", c=8)]
            aTp = [aTt[:], R1[:, 3072:3584].rearrange("p (c t) -> p c t", c=4)]
            cqp = [cqn[:], R1[:, 3584:3968]]
            abp = [abf, R1[:, 8192:8704]]
            vnp = [vnb, R1[:, 8704:9216]]
            krp = [krt[:], R1[0:32, 9216:9728].bitcast(F32)]
            def a12_stage(i, which):
                xk = ("X", i)
                pi = i % 2
                so = 32 * pi
                xt = xTp2[pi]; xtk = ("xTt", pi)
                TA_ = TAp[pi]; tak = "TA" if pi == 0 else "TA1"
                cq_ = cqp[pi]; cqk = "cqn%d" % pi
                ab_ = abp[pi]; abk = "abf%d" % pi
                vn_ = vnp[pi]; vnk = "vnb%d" % pi
                aT_ = aTp[pi]; aTk = "aTt%d" % pi
                kr_ = krp[pi]
                ksq, kskv, ksa = "sq%d" % pi, "skv%d" % pi, "sa%d" % pi
                k1, k2, k3, skk = "lnmv%d" % pi, "lnr%d" % pi, "lnn%d" % pi, "stt%d" % pi
                if which == 1:
                    transpose_block(i, xt, xtk)
                    A(lambda e, i=i: e.activation(out=X[:, i, :], in_=X[:, i, :], func=AF.Copy, scale=ALPHA), [xk], [xk])
                    for kc in range(8):
                        MM(bank(2, 0, 384), xt[:, kc, :], WLAT[:, kc, 0:384], kc == 0, kc == 7, [xtk, "WB0"], ["B2"])
                    for kc in range(8):
                        MM(bank(3, 0, 128, 0, 32), WLAT[:, kc, 384:416], xt[:, kc, :], kc == 0, kc == 7, [xtk, "WB0"], ["B3"], sig=False)
                    for kc in range(8):
                        MM(bank(3, 128, 256, 0, 32), WLsw[:, kc, :], xt[:, kc, :], kc == 0, kc == 7, [xtk, "WLsw"], ["B3"])
                    for hh in range(2):
                        for kc in range(8):
                            MM(bank(5 + hh), xt[:, kc, :], WUV[:, kc, hh * 512:(hh + 1) * 512], kc == 0, kc == 7, [xtk, "WB1"], ["B%d" % (5 + hh)])
                elif which == 2:
                    A(lambda e, so=so: e.activation(out=junk[:, 0:256], in_=bank(2, 0, 256), func=AF.Square, accum_out=sm[:, so + 8:so + 9]), ["B2"], ["junk", ksq])
                    A(lambda e, so=so: e.activation(out=junk[:, 0:128], in_=bank(2, 256, 384), func=AF.Square, accum_out=sm[:, so + 9:so + 10]), ["B2"], ["junk", kskv])
                    rstd_from(sm[:, so + 8:so + 9], 256.0, sm[:, so + 8:so + 9], ksq)
                    rstd_from(sm[:, so + 9:so + 10], 128.0, sm[:, so + 9:so + 10], kskv)
                    V(lambda e, so=so, cq_=cq_: e.tensor_scalar(out=cq_[:, 0:256], in0=bank(2, 0, 256), scalar1=sm[:, so + 8:so + 9], scalar2=None, op0=ALU.mult), ["B2", ksq], [cqk])
                    V(lambda e, so=so, cq_=cq_: e.tensor_scalar(out=cq_[:, 256:384], in0=bank(2, 256, 384), scalar1=sm[:, so + 9:so + 10], scalar2=None, op0=ALU.mult), ["B2", kskv], [cqk])
                    for c in range(3):
                        TR(bankb(4, c * 128, (c + 1) * 128), cq_[:, c * 128:(c + 1) * 128], ident_b[:], [cqk, "ident_b"], ["B4"], sig=(c == 2))
                    A(lambda e, i=i: e.activation(out=CQT[:, :, i * 128:(i + 1) * 128], in_=bankb(4, 0, 256).rearrange("p (c t) -> p c t", c=2), func=AF.Copy),
                      ["B4"], [("CQT", i)])
                    V(lambda e, i=i: e.tensor_copy(out=LAT[:, i * 128:(i + 1) * 128], in_=bankb(4, 256, 384)), ["B4"], ["LAT"])
                    V(lambda e, i=i, kr_=kr_: e.tensor_tensor(out=kr_[:, 0:128], in0=bank(3, 0, 128, 0, 32), in1=CS[:, 0, i * 128:(i + 1) * 128], op=ALU.mult),
                      ["B3"], ["krt0_%d" % pi])
                    V(lambda e, i=i, kr_=kr_: e.tensor_tensor(out=kr_[:, 128:256], in0=bank(3, 128, 256, 0, 32), in1=CS[:, 1, i * 128:(i + 1) * 128], op=ALU.mult),
                      ["B3"], ["krt1_%d" % pi])
                    V(lambda e, i=i, kr_=kr_: e.tensor_tensor(out=KRl[:, i * 128:(i + 1) * 128], in0=kr_[:, 0:128], in1=kr_[:, 128:256], op=ALU.add),
                      ["krt0_%d" % pi, "krt1_%d" % pi], ["KRl"])
                    A(lambda e, TA_=TA_: e.activation(out=TA_, in_=ps[:, 5 * 512:7 * 512], func=AF.Gelu), ["B5", "B6"], [tak])
                elif which == 3:
                    V(lambda e, TA_=TA_, pi=pi: e.bn_stats(out=stt[:, 2 * pi, :], in_=TA_[:, 512:1024]), [tak], [skk])
                    V(lambda e, so=so, pi=pi: e.bn_aggr(out=sm[:, so:so + 2], in_=stt[:, 2 * pi:2 * pi + 1, :]), [skk], [k1])
                    A(lambda e, so=so: e.activation(out=sm[:, so + 2:so + 3], in_=sm[:, so + 1:so + 2], func=AF.Sqrt, bias=EPS, scale=1.0), [k1], [k2])
                    V(lambda e, so=so: e.reciprocal(out=sm[:, so + 2:so + 3], in_=sm[:, so + 2:so + 3]), [k2], [k2])
                    V(lambda e, so=so: e.tensor_scalar(out=sm[:, so + 3:so + 4], in0=sm[:, so:so + 1], scalar1=-1.0, scalar2=sm[:, so + 2:so + 3], op0=ALU.mult, op1=ALU.mult), [k1, k2], [k3])
                    A(lambda e, so=so, TA_=TA_: e.activation(out=TA_[:, 512:1024], in_=TA_[:, 512:1024], func=AF.Identity, bias=sm[:, so + 3:so + 4], scale=sm[:, so + 2:so + 3]), [tak, k2, k3], [tak])
                    V(lambda e, TA_=TA_: e.tensor_tensor(out=TA_[:, 512:1024], in0=TA_[:, 512:1024], in1=SGG[:], op=ALU.mult), [tak, "SG"], [tak])
                    V(lambda e, TA_=TA_, vn_=vn_: e.tensor_tensor(out=vn_[:], in0=TA_[:, 512:1024], in1=SGB[:], op=ALU.add), [tak, "SG"], [vnk])
                elif which == 4:
                    for g in range(8):
                        MM(bank(7, g * 64, (g + 1) * 64), wsT[:, g, :], vn_[:, g * 64:(g + 1) * 64], True, False, ["wsT", vnk], ["B7"], sig=False)
                        MM(bank(7, g * 64, (g + 1) * 64), bsr[0:1, g * 128:(g + 1) * 128], ones_b[0:1, 0:64], False, True, ["bsr", "ones_b"], ["B7"], sig=(g == 7))
                    V(lambda e, TA_=TA_, ab_=ab_: e.tensor_tensor(out=ab_[:], in0=TA_[:, 0:512], in1=bank(7), op=ALU.mult), [tak, "B7"], [abk])
                    A(lambda e, so=so, ab_=ab_: e.activation(out=junk[:], in_=ab_[:], func=AF.Square, accum_out=sm[:, so + 10:so + 11]), [abk], ["junk", ksa])
                    rstd_from(sm[:, so + 10:so + 11], 512.0, sm[:, so + 10:so + 11], ksa)
                    for c in range(4):
                        TR(bankb(4, 512 + c * 128, 512 + (c + 1) * 128), ab_[:, c * 128:(c + 1) * 128], ident_b[:], [abk, "ident_b"], ["B4b"], sig=(c == 3))
                    A(lambda e, aT_=aT_: e.activation(out=aT_, in_=bankb(4, 512, 1024).rearrange("p (c t) -> p c t", c=4), func=AF.Copy), ["B4b"], [aTk])
                    for hh in range(2):
                        for c in range(4):
                            MM(bank(5 + hh), aT_[:, c, :], WOA[:, c, hh * 512:(hh + 1) * 512], c == 0, c == 3, [aTk, "WB1"], ["B%d" % (5 + hh)])
                    V(lambda e, i=i, so=so: e.scalar_tensor_tensor(out=X[:, i, :], in0=ps[:, 5 * 512:7 * 512], scalar=sm[:, so + 10:so + 11], in1=X[:, i, :], op0=ALU.mult, op1=ALU.add),
                      ["B5", "B6", ksa, xk], [xk])


            for it in range(NB + 1):
                if it < NB:
                    a12_stage(it, 1)
                if it >= 1:
                    a12_stage(it - 1, 3)
                if it < NB:
                    a12_stage(it, 2)
                if it >= 1:
                    a12_stage(it - 1, 4)

            D("sync", ag_in[l][0:128, :], LAT, ["LAT"], ["ag_in"])
            D("sync", ag_in[l][128:160, :], KRl, ["KRl"], ["ag_in"])
            p.dma("gpsimd", lambda e, l=l: e.collective_compute("AllGather", ALU.bypass, replica_groups=[[0, 1], [2, 3], [4, 5], [6, 7]],
                                                                ins=[ag_in[l].opt()], outs=[ag_out[l].opt()]), r=["ag_in"], w=["ag_out"], inc=1)
            p.barrier()
            for hk in range(2):
                D("sync", CKV[:, hk * T:(hk + 1) * T], ag_out[l][hk * 160:hk * 160 + 128, :], ["ag_out"], [("CKV", hk)])
                D("sync", KT[0:32, hk * T:(hk + 1) * T], ag_out[l][hk * 160 + 128:hk * 160 + 160, :], ["ag_out"], [("KTr", hk)])
            G(lambda e: e.memset(KT[32:64, :], 0.0), [], ["KTc"])
            G(lambda e: e.memset(KT[32:33, :], 1.0), [], ["KTc"])
            for gq in range(2):
                G(lambda e, gq=gq: e.memset(QTg[gq][32:64, :], 0.0), [], [("QT", gq)])
            ckv_keys = [("CKV", 0), ("CKV", 1)]
            ktr_keys = [("KTr", 0), ("KTr", 1), "KTc"]

            def prologue(h):
                par = h % 2
                V(lambda e: e.tensor_copy(out=WKpad[:, 64:128], in_=WUKV[:, h * 128:h * 128 + 64]), ["WB0"], ["WKpad"])
                V(lambda e: e.tensor_copy(out=WQpad[:, :, 0:32], in_=WUQ[:, :, h * 96 + 64:h * 96 + 96]), ["WB0"], ["WQpad"])
                V(lambda e: e.tensor_copy(out=WQpad[:, :, 64:128], in_=WUQ[:, :, h * 96:h * 96 + 64]), ["WB0"], ["WQpad"])
                V(lambda e: e.tensor_copy(out=WQsw[:, :, 0:16], in_=WUQ[:, :, h * 96 + 80:h * 96 + 96]), ["WB0"], ["WQsw"])
                V(lambda e: e.tensor_copy(out=WQsw[:, :, 16:32], in_=WUQ[:, :, h * 96 + 64:h * 96 + 80]), ["WB0"], ["WQsw"])
                for c in range(8):
                    b_ = c % 2
                    MM(bank(b_), WKpad[:], CKV[:, c * 512:(c + 1) * 512], True, True, ["WKpad", ("CKV", c // 4)], ["B%d" % b_])
                    if c % 2 == 0:
                        A(lambda e, c=c, b_=b_: e.activation(out=KT[64:128, c * 512:(c + 1) * 512], in_=bank(b_, 0, 512, 64, 128), func=AF.Copy), ["B%d" % b_], [("KTn", c)])
                    else:
                        V(lambda e, c=c, b_=b_: e.tensor_copy(out=KT[64:128, c * 512:(c + 1) * 512], in_=bank(b_, 0, 512, 64, 128)), ["B%d" % b_], [("KTn", c)])
                voff = 0 if par == 0 else 64
                onescol = 64 if par == 0 else 0
                for q4 in range(4):
                    b_ = q4 % 2
                    for kk in range(8):
                        kb = q4 * 8 + kk
                        MM(bank(b_, kk * 64, (kk + 1) * 64), CKV[:, kb * 128:(kb + 1) * 128], WUKV[:, h * 128 + 64:h * 128 + 128], True, True,
                           [("CKV", kb // 16), "WB0"], ["B%d" % b_], sig=(kk == 7))
                    if q4 % 2 == 0:
                        A(lambda e, q4=q4, b_=b_: e.activation(out=VA[:, q4 * 8:(q4 + 1) * 8, voff:voff + 64],
                                                               in_=bank(b_).rearrange("p (k c) -> p k c", k=8), func=AF.Copy), ["B%d" % b_], ["VA"])
                    else:
                        V(lambda e, q4=q4, b_=b_: e.tensor_copy(out=VA[:, q4 * 8:(q4 + 1) * 8, voff:voff + 64],
                                                                in_=bank(b_).rearrange("p (k c) -> p k c", k=8)), ["B%d" % b_], ["VA"])
                G(lambda e: e.memset(VA[:, :, onescol:onescol + 1], 1.0), ["VA"], ["VA"])

            ktn_keys = [("KTn", c) for c in range(8)]
            kall = ktr_keys + ktn_keys

            def qbuild(h, Gq, u):
                qbuild_a(h, Gq, u)
                qbuild_b(h, Gq, u)

            def qbuild_a(h, Gq, u):
                qb = u % 2
                QT_ = QTg[qb]
                qk = ("QT", qb)
                cqk = [("CQT", Gq * 4 + t_) for t_ in range(4)]
                for c in range(2):
                    MM(bank(0), WQpad[:, c, :], CQT[:, c, Gq * 512:(Gq + 1) * 512], c == 0, c == 1, ["WQpad"] + cqk, ["B0"])
                for c in range(2):
                    MM(bank(1, 0, 512, 0, 32), WQsw[:, c, :], CQT[:, c, Gq * 512:(Gq + 1) * 512], c == 0, c == 1, ["WQsw"] + cqk, ["B1"])
                V(lambda e: e.tensor_copy(out=QT_[64:128, :], in_=bank(0, 0, 512, 64, 128)), ["B0"], [qk])
                G(lambda e: e.memset(QT_[32:33, :], 0.0), [], [qk])
                V(lambda e: e.tensor_tensor(out=TA[0:32, 0:512], in0=bank(0, 0, 512, 0, 32), in1=CS[:, 0, Gq * 512:(Gq + 1) * 512], op=ALU.mult), ["B0"], ["TA"])
                V(lambda e: e.tensor_tensor(out=TA[0:32, 512:1024], in0=bank(1, 0, 512, 0, 32), in1=CS[:, 1, Gq * 512:(Gq + 1) * 512], op=ALU.mult), ["B1"], ["TA"])
                V(lambda e: e.tensor_tensor(out=QT_[0:32, :], in0=TA[0:32, 0:512], in1=TA[0:32, 512:1024], op=ALU.add), ["TA"], [qk])
                V(lambda e: e.tensor_tensor(out=abf[:], in0=QT_[:, :], in1=KT[:, Gq * 512:(Gq + 1) * 512], op=ALU.mult), [qk] + kall, ["abf"])
                G(lambda e: e.tensor_tensor(out=vnb[:], in0=QT_[:, :], in1=KT[:, T + Gq * 512:T + (Gq + 1) * 512], op=ALU.mult), [qk] + kall, ["vnb"])

            def qbuild_b(h, Gq, u):
                qb = u % 2
                QT_ = QTg[qb]
                qk = ("QT", qb)
                MM(bank(2, 0, 512, 0, 33), NEGS[:, 0:33], abf[:], True, False, ["NEGS", "abf"], ["B2"], sig=False)
                MM(bank(2, 0, 512, 0, 33), NEGS[:, 33:66], vnb[:], False, True, ["NEGS", "vnb"], ["B2"], sig=True)
                A(lambda e: e.activation(out=QT_[32:33, :], in_=bank(2, 0, 512, 32, 33), func=AF.Copy), ["B2"], [qk])

            def tail(h, Gq, u):
                tail_a(h, Gq, u)
                tail_b(h, Gq, u)

            def tail_a(h, Gq, u):
                par = h % 2
                bo = 7 if u % 2 == 0 else 3
                lrow = 64 if par == 0 else 0
                V(lambda e: e.reciprocal(out=rl[lrow:lrow + 1, :], in_=bank(bo, 0, 512, lrow, lrow + 1)), ["B%d" % bo], ["WB1"])

            def tail_b(h, Gq, u):
                par = h % 2
                bo = 7 if u % 2 == 0 else 3
                bok = "B%d" % bo
                Mv = 65 if par == 0 else 128
                orow0 = 0 if par == 0 else 64
                lrow = 64 if par == 0 else 0
                MM(bank(2, 0, 512, 0, Mv if par else 64), ones_f[lrow:lrow + 1, 0:(128 if par else 64)], rl[lrow:lrow + 1, :], True, True, ["WB1", "ones_f"], ["B2"])
                V(lambda e: e.tensor_copy(out=bcs[orow0:orow0 + 64, :], in_=bank(2, 0, 512, orow0, orow0 + 64)), ["B2"], ["WB1"])
                V(lambda e: e.tensor_tensor(out=MT[orow0:orow0 + 64, h // 2, Gq * 512:(Gq + 1) * 512],
                                            in0=bank(bo, 0, 512, orow0, orow0 + 64), in1=bcs[orow0:orow0 + 64, :], op=ALU.mult),
                  [bok, "WB1"], [("MT", Gq)])
                V(lambda e: e.tensor_tensor(out=sqb[orow0:orow0 + 64, :], in0=MT[orow0:orow0 + 64, h // 2, Gq * 512:(Gq + 1) * 512],
                                            in1=MT[orow0:orow0 + 64, h // 2, Gq * 512:(Gq + 1) * 512], op=ALU.mult),
                  [("MT", Gq)], ["sqb"])
                for tb in range(4):
                    col = Gq * 4 + tb
                    MM(bank(4, h * 16 + col, h * 16 + col + 1), sqb[orow0:orow0 + 64, tb * 128:(tb + 1) * 128], ones_b[orow0:orow0 + 64, 0:1], True, True,
                       ["sqb", "ones_b"], ["B4s"], sig=(tb == 3))

            def pass2(h, Gq, u, hooks):
                par = h % 2
                qb = u % 2
                QT_ = QTg[qb]
                qk = ("QT", qb)
                bo = 7 if u % 2 == 0 else 3
                bok = "B%d" % bo
                Mv = 65 if par == 0 else 128
                kbl = []
                for hk in range(2):
                    for jb in range(4 * Gq):
                        kbl.append((hk * 16 + jb, 0, None))
                for hk in range(2):
                    for d_ in range(2):
                        kbl.append((hk * 16 + 4 * Gq + d_, 0, (0, hk * 2 + d_)))
                for hk in range(2):
                    for d_ in range(2):
                        kbl.append((hk * 16 + 4 * Gq + 2 + d_, 256, (256, hk * 2 + d_)))
                nk = len(kbl)

                def issue_S(ki):
                    kb, qlo, msk = kbl[ki]
                    b_ = 5 + (ki % 2)
                    MM(bank(b_, qlo, 512), KT[:, kb * 128:(kb + 1) * 128], QT_[:, qlo:512], True, msk is None, [qk] + kall, ["B%d" % b_])
                    if msk is not None:
                        mlo, kbq = msk
                        MM(bank(b_, mlo, mlo + 256), ident_b[:], maskT[:, kbq, :], False, True, ["ident_b", "maskT"], ["B%d" % b_])

                issue_S(0)
                issue_S(1)
                for ki, (kb, qlo, msk) in enumerate(kbl):
                    b_ = 5 + (ki % 2)
                    pt = PT[ki % 3]
                    ptk = ("PT", ki % 3)
                    A(lambda e, b_=b_, qlo=qlo, pt=pt: e.activation(out=pt[:, qlo:512], in_=bank(b_, qlo, 512), func=AF.Exp, scale=ATT_SCALE), ["B%d" % b_], [ptk])
                    if ki + 2 < nk:
                        issue_S(ki + 2)
                    if qlo == 0:
                        MM(bank(bo, 0, 512, 0, Mv), VA[:, kb, 0:Mv], pt[:, 0:512], ki == 0, ki == nk - 1, ["VA", ptk], [bok], sig=True)
                    else:
                        MM(bank(bo, 256, 512, 0, Mv), VA[:, kb, 0:Mv], pt[:, 256:512], False, ki == nk - 1, ["VA", ptk], [bok], sig=True)
                    for fn in hooks.get(ki, ()):
                        fn()

            units = [(h, Gq) for h in range(8) for Gq in range(4)]
            prev = None
            for u, (h, Gq) in enumerate(units):
                hooks = {}
                if Gq == 0:
                    prologue(h)
                    qbuild(h, 0, u)
                if Gq < 3:
                    hooks.setdefault(0, []).append(lambda h=h, Gq=Gq, u=u: qbuild_a(h, Gq + 1, u + 1))
                    hooks.setdefault(4, []).append(lambda h=h, Gq=Gq, u=u: qbuild_b(h, Gq + 1, u + 1))
                if prev is not None:
                    ph, pg, pu = prev
                    hooks.setdefault(1, []).append(lambda ph=ph, pg=pg, pu=pu: tail_a(ph, pg, pu))
                    hooks.setdefault(6, []).append(lambda ph=ph, pg=pg, pu=pu: tail_b(ph, pg, pu))
                pass2(h, Gq, u, hooks)
                prev = (h, Gq, u)
            tail(*prev)
            V(lambda e: e.reduce_sum(out=ssq[:], in_=bank(4, 0, 128).rearrange("p (h c) -> p c h", h=8), axis=AX.X), ["B4s"], ["ssq"])
            p.barrier()
            if dbg_d is not None:
                D("sync", dbg_d, R2[:, :], [], ["dbg"])
                p.barrier()

            if kind == "m":
                D("sync", wr_f[:], moe_w_r_d[idx_m].rearrange("(c p) e -> p c e", p=128), [], ["wr_f"])
                V(lambda e: e.tensor_copy(out=wr_h[:], in_=wr_f[:]), ["wr_f"], ["wr_h"])
                V(lambda e: e.tensor_tensor(out=wr_l[:], in0=wr_f[:], in1=wr_h[:], op=ALU.subtract), ["wr_f", "wr_h"], ["wr_l"])
            LN4t = [TA[:], R1[:, 16384:18432].bitcast(F32)]
            LN4k = ["TA", "TA1"]
            xlop = [xTt0[:], R1[:, 18432:19456].rearrange("p (c t) -> p c t", c=8)]
            def a4_stage(i, which):
                xk = ("X", i)
                pi = i % 2
                y0 = 2 if pi == 0 else 6
                smc = 11 + 32 * pi
                smk = "sm_%d" % pi
                if which == 1:
                    A(lambda e, i=i, smc=smc: e.activation(out=sm[:, smc:smc + 1], in_=ssq[:, i:i + 1], func=AF.Sqrt, bias=EPS, scale=1.0 / 512.0), ["ssq"], [smk])
                    V(lambda e, smc=smc: e.reciprocal(out=sm[:, smc:smc + 1], in_=sm[:, smc:smc + 1]), [smk], [smk])
                    for hh in range(2):
                        for c in range(4):
                            MM(bank(y0 + hh), MT[:, c, i * 128:(i + 1) * 128], WOM[:, c, hh * 512:(hh + 1) * 512], c == 0, c == 3, [("MT", i // 4), "WB0"], ["B%d" % (y0 + hh)])
                    if stop_after != "mixer_a":
                        V(lambda e, i=i, y0=y0, smc=smc: e.scalar_tensor_tensor(out=X[:, i, :], in0=ps[:, y0 * 512:(y0 + 2) * 512], scalar=sm[:, smc:smc + 1], in1=X[:, i, :], op0=ALU.mult, op1=ALU.add),
                          ["B%d" % y0, "B%d" % (y0 + 1), smk, xk], [xk])
                elif which == 2:
                    layernorm_block(i, X[:, i, :], LNG, LNB, "LN", tmp=LN4t[pi], tk=LN4k[pi], pi=pi)
                else:
                    transpose_block(i, BT[:, :, i * 128:(i + 1) * 128], ("BT", i), also_f32=None)
                    A(lambda e, i=i: e.activation(out=X[:, i, :], in_=X[:, i, :], func=AF.Copy, scale=ALPHA), [xk], [xk])
                    if kind == "m":
                        xlo = xlop[pi]
                        V(lambda e, i=i, xlo=xlo: e.tensor_tensor(out=xlo, in0=ps[:, 0:1024].rearrange("p (c t) -> p c t", c=8), in1=BT[:, :, i * 128:(i + 1) * 128], op=ALU.subtract),
                          ["B0", "B1", ("BT", i)], [("xTt", pi)])
                        for pi_, (lt, lk, wt, wk) in enumerate([("hi", None, wr_h, "wr_h"), ("lo", None, wr_h, "wr_h"), ("hi", None, wr_l, "wr_l")]):
                            for kc in range(8):
                                lhs = BT[:, kc, i * 128:(i + 1) * 128] if lt == "hi" else xlo[:, kc, :]
                                lkey = ("BT", i) if lt == "hi" else ("xTt", pi)
                                MM(bank(4, 0, 8), lhs, wt[:, kc, :], (pi_ == 0 and kc == 0), (pi_ == 2 and kc == 7), [lkey, wk], ["B4"])
                        V(lambda e: e.tensor_copy(out=lgt[:, 0, :], in_=bank(4, 0, 8)), ["B4"], ["lg0"])
                        V(lambda e: e.reduce_max(out=sm[:, 20:21], in_=lgt[:, 0, :], axis=AX.X), ["lg0"], ["m1"])
                        V(lambda e: e.tensor_scalar(out=lgt[:, 1, :], in0=lgt[:, 0, :], scalar1=sm[:, 20:21], scalar2=None, op0=ALU.is_ge), ["lg0", "m1"], ["lg1"])
                        V(lambda e: e.scalar_tensor_tensor(out=lgt[:, 2, :], in0=lgt[:, 1, :], scalar=-1e30, in1=lgt[:, 0, :], op0=ALU.mult, op1=ALU.add), ["lg1", "lg0"], ["lg2"])
                        V(lambda e: e.reduce_max(out=sm[:, 21:22], in_=lgt[:, 2, :], axis=AX.X), ["lg2"], ["m2"])
                        V(lambda e: e.tensor_scalar(out=lgt[:, 1, :], in0=lgt[:, 0, :], scalar1=sm[:, 21:22], scalar2=None, op0=ALU.is_ge), ["lg0", "m2", "lg2"], ["lg1b"])
                        V(lambda e: e.tensor_scalar(out=sm[:, 22:23], in0=sm[:, 20:21], scalar1=-1.0, scalar2=None, op0=ALU.mult), ["m1"], ["nm1"])
                        A(lambda e: e.activation(out=lgt[:, 3, :], in_=lgt[:, 0, :], func=AF.Exp, bias=sm[:, 22:23], scale=1.0), ["lg0", "nm1"], ["lg3"])
                        V(lambda e: e.tensor_tensor(out=lgt[:, 3, :], in0=lgt[:, 3, :], in1=lgt[:, 1, :], op=ALU.mult), ["lg3", "lg1b"], ["lg3b"])
                        V(lambda e: e.reduce_sum(out=sm[:, 23:24], in_=lgt[:, 3, :], axis=AX.X), ["lg3b"], ["sw"])
                        V(lambda e: e.reciprocal(out=sm[:, 23:24], in_=sm[:, 23:24]), ["sw"], ["sw"])
                        V(lambda e, i=i: e.tensor_scalar(out=comb[:, i, :], in0=lgt[:, 3, :], scalar1=sm[:, 23:24], scalar2=None, op0=ALU.mult), ["lg3b", "sw"], [("comb", i)])


            if stop_after in ("mixer_a", "mixer_raw"):
                for i in range(NB):
                    a4_stage(i, 1)
            elif stop_after == "mixer":
                for i in range(NB):
                    a4_stage(i, 1)
                    a4_stage(i, 2)
            else:
                for it in range(NB + 1):
                    if it < NB:
                        a4_stage(it, 1)
                    if it >= 1:
                        a4_stage(it - 1, 2)
                        a4_stage(it - 1, 3)

            if stop_after not in ("mixer", "mixer_a", "mixer_raw"):
                p.barrier()
                stages = []
                if kind == "d":
                    f0 = 0
                    while f0 < 2816:
                        fw = min(512, 2816 - f0)
                        stages.append((None, f0, fw))
                        f0 += fw
                else:
                    for e_ in range(8):
                        for f0 in range(0, 3584, 512):
                            stages.append((e_, f0, 512))

                def load_stage(si_):
                    e_, f0, fw = stages[si_]
                    slot = WB[si_ % 2]
                    sk = "WB%d" % (si_ % 2)
                    W1s = slot[:, 0:4096].rearrange("p (c f) -> p c f", c=8)
                    W3s = slot[:, 4096:8192].rearrange("p (c f) -> p c f", c=8)
                    W2s = slot[:, 8192:12288].rearrange("p (c f) -> p c f", c=4)
                    if e_ is None:
                        s1 = ffn_w1_d[idx_d]; s3 = ffn_w3_d[idx_d]; s2 = ffn_w2_d[idx_d]
                    else:
                        s1 = moe_w1_d[idx_m, e_]; s3 = moe_w3_d[idx_m, e_]; s2 = moe_w2_d[idx_m, e_]
                    for hh in range(2):
                        D("gpsimd", W1s[:, hh * 4:(hh + 1) * 4, 0:fw], s1[hh * 512:(hh + 1) * 512, f0:f0 + fw].rearrange("(c p) f -> p c f", p=128), [], [sk])
                        D("gpsimd", W3s[:, hh * 4:(hh + 1) * 4, 0:fw], s3[hh * 512:(hh + 1) * 512, f0:f0 + fw].rearrange("(c p) f -> p c f", p=128), [], [sk])
                    D("gpsimd", W2s[:, 0:fw // 128, :], s2[f0:f0 + fw, :].rearrange("(c p) d -> p c d", p=128), [], [sk])

                load_stage(0)
                for si_ in range(len(stages)):
                    if si_ + 1 < len(stages):
                        load_stage(si_ + 1)
                    if si_ == len(stages) - 1:
                        assert si_ % 2 == 1
                        D("sync", LNG[:], ln2_g_d[l:l + 1, :].partition_broadcast(128), [], ["LN"])
                        D("sync", LNB[:], ln2_b_d[l:l + 1, :].partition_broadcast(128), [], ["LN"])
                        WGp = WB[0][:, 0:8192].rearrange("p (c f) -> p c f", c=8)
                        WPp = WB[0][:, 8192:10240].rearrange("p (c f) -> p c f", c=2)
                        for hh in range(2):
                            D("gpsimd", WGp[:, hh * 4:(hh + 1) * 4, :], ple_w_g_d[l, hh * 512:(hh + 1) * 512, :].rearrange("(c p) f -> p c f", p=128), [], ["WB0"])
                        D("gpsimd", WPp, ple_w_p_d[l].rearrange("(c p) f -> p c f", p=128), [], ["WB0"])
                    e_, f0, fw = stages[si_]
                    nfc = fw // 128
                    slot = WB[si_ % 2]
                    sk = "WB%d" % (si_ % 2)
                    W1s = slot[:, 0:4096].rearrange("p (c f) -> p c f", c=8)
                    W3s = slot[:, 4096:8192].rearrange("p (c f) -> p c f", c=8)
                    W2s = slot[:, 8192:12288].rearrange("p (c f) -> p c f", c=4)
                    def ffn_up(tg):
                        btk = [("BT", tg * 4 + t_) for t_ in range(4)]
                        for fc in range(nfc):
                            u_ = (tg * nfc + fc) % 2
                            b1, b3 = 2 * u_, 2 * u_ + 1
                            for kc in range(8):
                                MM(bank(b1), W1s[:, kc, fc * 128:(fc + 1) * 128], BT[:, kc, tg * 512:(tg + 1) * 512], kc == 0, kc == 7, [sk] + btk, ["B%d" % b1])
                            for kc in range(8):
                                MM(bank(b3), W3s[:, kc, fc * 128:(fc + 1) * 128], BT[:, kc, tg * 512:(tg + 1) * 512], kc == 0, kc == 7, [sk] + btk, ["B%d" % b3])
                            stl = silu_t[u_]
                            A(lambda e, stl=stl, b1=b1: e.activation(out=stl[:], in_=bank(b1), func=AF.Silu), ["B%d" % b1], [("silu", u_)])
                            V(lambda e, stl=stl, b3=b3, fc=fc, tg=tg: e.tensor_tensor(out=GT[:, fc, tg * 512:(tg + 1) * 512], in0=stl[:], in1=bank(b3), op=ALU.mult),
                              [("silu", u_), "B%d" % b3], [("GT", tg)])

                    def ffn_down(tg):
                        for t_ in range(4):
                            tb = tg * 4 + t_
                            bo = 4 + 2 * (tb % 2)
                            for hh in range(2):
                                for fc in range(nfc):
                                    MM(bank(bo + hh), GT[:, fc, tb * 128:(tb + 1) * 128], W2s[:, fc, hh * 512:(hh + 1) * 512], fc == 0, fc == nfc - 1,
                                       [("GT", tg), sk], ["B%d" % (bo + hh)])
                            xk = ("X", tb)
                            if e_ is None:
                                V(lambda e, tb=tb, bo=bo: e.tensor_tensor(out=X[:, tb, :], in0=X[:, tb, :], in1=ps[:, bo * 512:(bo + 2) * 512], op=ALU.add),
                                  [xk, "B%d" % bo, "B%d" % (bo + 1)], [xk])
                            else:
                                V(lambda e, tb=tb, bo=bo, e_=e_: e.scalar_tensor_tensor(out=X[:, tb, :], in0=ps[:, bo * 512:(bo + 2) * 512], scalar=comb[:, tb, e_:e_ + 1],
                                                                                        in1=X[:, tb, :], op0=ALU.mult, op1=ALU.add),
                                  [xk, "B%d" % bo, "B%d" % (bo + 1), ("comb", tb)], [xk])

                    ffn_up(0)
                    for tg in range(4):
                        if tg + 1 < 4:
                            ffn_up(tg + 1)
                        ffn_down(tg)
                p.barrier()

                WG = WB[0][:, 0:8192].rearrange("p (c f) -> p c f", c=8)
                WP = WB[0][:, 8192:10240].rearrange("p (c f) -> p c f", c=2)
                LNt = [TA[:], R1[:, 0:2048].bitcast(F32)]
                LNk = ["TA", "TA1"]
                GTt = [R1[:, 2048:4096].bitcast(F32), R1[:, 4096:6144].bitcast(F32)]
                xTp = [xTt0[:], R1[:, 6144:7168].rearrange("p (c t) -> p c t", c=8)]
                pbp = [pblk[:], R1[:, 7168:7424]]
                pTp = [pTt[:], R1[:, 7424:7680].rearrange("p (c t) -> p c t", c=2)]
                def ple_stage(i, which):
                    xk = ("X", i)
                    pi = i % 2
                    xt = xTp[pi]
                    xtk = ("xTt", pi)
                    gt_ = GTt[pi]
                    gk = "GTt%d" % pi
                    pb_ = pbp[pi]
                    pT_ = pTp[pi]
                    g0 = 2 if pi == 0 else 6
                    if which == 1:
                        layernorm_block(i, X[:, i, :], LNG, LNB, "LN", tmp=LNt[pi], tk=LNk[pi], pi=pi)
                        D("gpsimd", pb_, p_d[l, i], [], ["pblk%d" % pi])
                    elif which == 2:
                        transpose_block(i, xt, xtk)
                        for c in range(2):
                            TR(bankb(4, c * 128, (c + 1) * 128), pb_[:, c * 128:(c + 1) * 128], ident_b[:], ["pblk%d" % pi, "ident_b"], ["B4"], sig=(c == 1))
                        V(lambda e, pT_=pT_: e.tensor_copy(out=pT_, in_=bankb(4, 0, 256).rearrange("p (c t) -> p c t", c=2)), ["B4"], ["pTt%d" % pi])
                        for hh in range(2):
                            for kc in range(8):
                                MM(bank(g0 + hh), xt[:, kc, :], WG[:, kc, hh * 512:(hh + 1) * 512], kc == 0, False, [xtk, "WB0"], ["B%d" % (g0 + hh)], sig=False)
                            MM(bank(g0 + hh), ones_b[0:1, 0:128], bgr[0:1, hh * 512:(hh + 1) * 512], False, True, ["ones_b", "bgr"], ["B%d" % (g0 + hh)], sig=True)
                        for hh in range(2):
                            for c in range(2):
                                MM(bank(4 + hh), pT_[:, c, :], WP[:, c, hh * 512:(hh + 1) * 512], c == 0, c == 1, ["pTt%d" % pi, "WB0"], ["B%d" % (4 + hh)])
                    else:
                        A(lambda e, gt_=gt_, g0=g0: e.activation(out=gt_, in_=ps[:, g0 * 512:(g0 + 2) * 512], func=AF.Sigmoid), ["B%d" % g0, "B%d" % (g0 + 1)], [gk])
                        V(lambda e, gt_=gt_: e.tensor_tensor(out=gt_, in0=gt_, in1=ps[:, 4 * 512:6 * 512], op=ALU.mult), [gk, "B4", "B5"], [gk])
                        G(lambda e, i=i, gt_=gt_: e.tensor_tensor(out=X[:, i, :], in0=X[:, i, :], in1=gt_, op=ALU.add), [gk, xk], [xk])


                if stop_after == "ffn":
                    for i in range(NB):
                        layernorm_block(i, X[:, i, :], LNG, LNB, "LN", tmp=LNt[i % 2], tk=LNk[i % 2], pi=i % 2)
                else:
                    for it in range(NB + 1):
                        if it < NB:
                            ple_stage(it, 1)
                        if it >= 1:
                            ple_stage(it - 1, 2)
                            ple_stage(it - 1, 3)
                p.barrier()
            if kind == "d":
                idx_d += 1
            else:
                idx_m += 1

        outk = []
        for jb in range(4):
            t_ = D("sync", y_d[jb * 4:(jb + 1) * 4].rearrange("j p d -> p j d"), X[:, jb * 4:(jb + 1) * 4, :],
                   [("X", j) for j in range(jb * 4, jb * 4 + 4)], [("y", jb)])
            outk.append(t_)
        p.wait_all("sync", outk)
        p.emit(st)
    return nc


_W_LAYER = ["w_in", "sg_v_g", "sg_v_b", "sg_w_s", "sg_b_s", "q_norm_g", "kv_norm_g", "w_uq", "w_ukv", "out_g", "w_o",
            "ln1_g", "ln1_b", "ln2_g", "ln2_b", "ple_w_g", "ple_b_g", "ple_w_p"]
_W_DENSE = ["ffn_w1", "ffn_w3", "ffn_w2"]
_W_MOE = ["moe_w_r", "moe_w1", "moe_w3", "moe_w2"]


def _gb(hf, j):
    return 4 * (j // 2) + GMAP[hf][j % 2]


def _consts(hf):
    inv_freq = 1.0 / (10000.0 ** (np.arange(0, 32, 2, dtype=np.float32) / 32.0))
    invf = (np.concatenate([inv_freq, inv_freq]) / (2 * np.pi)).astype(np.float32).reshape(32, 1)
    sgn = np.concatenate([-np.ones(16, np.float32), np.ones(16, np.float32)]).reshape(32, 1)
    ident = np.eye(128, dtype=np.float32)
    tril = np.tril(np.ones((128, 128), np.float32))
    m = np.full((128, 4, 2, 128), -30000.0, np.float32)
    ii = np.arange(128)
    diag = (ii[:, None] <= ii[None, :]).astype(np.float32)
    for r in range(2):
        qg = GMAP[hf][r]
        for kb in range(4):
            kg = GMAP[kb // 2][kb % 2]
            if kg < qg:
                m[:, kb, r, :] = 0.0
            elif kg == qg:
                m[:, kb, r, :] = (diag - 1.0) * 30000.0
    negs = np.zeros((128, 66), np.float32)
    negs[:, 32 + 33 * hf] = -1.0
    return {"invf": invf, "sgn": sgn, "ident": ident, "tril": tril, "maskT": m.reshape(128, 1024), "negs": negs}


def _run(kinds, layer_ids, xin, inputs, stop_after=None):
    nc = build(kinds, stop_after=stop_after)
    p = inputs["p"]
    positions = inputs["positions"]
    d_ids = [li // 2 for li in layer_ids if li % 2 == 0]
    m_ids = [li // 2 for li in layer_ids if li % 2 == 1]
    shared = {}
    for k in _W_LAYER:
        a = np.ascontiguousarray(inputs[k][layer_ids])
        if k == "sg_b_s":
            a = a.reshape(len(layer_ids), 1024)
        shared[k] = a
    if d_ids:
        for k in _W_DENSE:
            shared[k] = np.ascontiguousarray(inputs[k][d_ids])
    if m_ids:
        for k in _W_MOE:
            shared[k] = np.ascontiguousarray(inputs[k][m_ids])
    maps = []
    for c in range(8):
        b, hf = c // 2, c % 2
        blks = [_gb(hf, j) for j in range(NB)]
        pc = np.stack([np.stack([p[li, b, g * 128:(g + 1) * 128, :] for g in blks]) for li in layer_ids])
        pos = np.concatenate([positions[b, g * 128:(g + 1) * 128] for g in blks]).astype(np.int32).reshape(1, T)
        m = {"x": xin[c], "p": np.ascontiguousarray(pc), "pos": pos}
        m.update(_consts(hf))
        m.update(shared)
        maps.append(m)
    res = run_bass_kernel_spmd(nc, maps, core_ids=list(range(8)))
    global DBG
    DBG = [res.results[c].get("dbg") for c in range(8)]
    return [np.asarray(res.results[c]["y"]) for c in range(8)]


def _shard_x(x):
    out = []
    for c in range(8):
        b, hf = c // 2, c % 2
        out.append(np.ascontiguousarray(np.stack([x[b, _gb(hf, j) * 128:(_gb(hf, j) + 1) * 128, :] for j in range(NB)])))
    return out


def _unshard(ys, shape):
    out = np.zeros(shape, np.float32)
    for c in range(8):
        b, hf = c // 2, c % 2
        for j in range(NB):
            g = _gb(hf, j)
            out[b, g * 128:(g + 1) * 128, :] = ys[c][j]
    return out


FUSED = True
DBG = None


def kernel(**inputs):
    inputs = {k: np.asarray(v) for k, v in inputs.items()}
    x = inputs["x"].astype(np.float32)
    xs = _shard_x(x)
    if FUSED:
        ys = _run(["d", "m", "d", "m"], [0, 1, 2, 3], xs, inputs)
    else:
        ys = xs
        for li in range(4):
            ys = _run(["d" if li % 2 == 0 else "m"], [li], ys, inputs)
    return _unshard(ys, x.shape)
```
